# Optimizing a Trainium2 kernel written in Bass

```python
import numpy as np
import jax, jax.numpy as jnp
from jax import lax

D_MODEL = 2048
BATCH = 2
SEQ = 16384
DEPTH = 2

HEAD_DIM = 128
ROPE_THETA = 10000.0
NORM_EPS = 1e-6
NEG_INF = -1e30
BIG = 1e9
Q_BLOCK = 128
NSA_HEADS = 4
MLA_HEADS = 6
DIL_HEADS = 6
MIX_WIDTH = (NSA_HEADS + MLA_HEADS + DIL_HEADS) * HEAD_DIM
CMP_LEN = 32
CMP_STRIDE = 16
CMP_HIDDEN = 128
SLC_BLOCK = 64
SLC_TOPK = 16
NSA_WINDOW = 512
Q_LORA = 512
KV_LORA = 512
QK_NOPE = 128
QK_ROPE = 64
V_HEAD = 128
DIL_PATTERNS = ((128, 1), (512, 4), (2048, 16))
MEM_LEN = 256
XATTN_HEADS = 4
XATTN_DIM = XATTN_HEADS * HEAD_DIM
D_FF = 4 * D_MODEL
NSA_Q_W = NSA_HEADS * HEAD_DIM
NSA_KV_W = 6 * HEAD_DIM
NSA_GATE_W = 3 * NSA_HEADS
DIL_W = 3 * DIL_HEADS * HEAD_DIM
IN_SPLITS = (NSA_Q_W, NSA_KV_W, NSA_GATE_W, Q_LORA, KV_LORA, QK_ROPE, DIL_W)
D_IN = NSA_Q_W + NSA_KV_W + NSA_GATE_W + Q_LORA + KV_LORA + QK_ROPE + DIL_W

kernel_name = 'hybrid_nsa_mla_dilated_trunk'


def rms_norm(x, g):
    xf = x.astype(jnp.float32)
    y = xf * lax.rsqrt(jnp.mean(xf * xf, axis=-1, keepdims=True) + NORM_EPS)
    return (y * g.astype(jnp.float32)).astype(x.dtype)


def rope_tables(pos, dim):
    inv = ROPE_THETA ** (-jnp.arange(0, dim, 2, dtype=jnp.float32) / dim)
    ang = pos.astype(jnp.float32)[..., None] * inv
    return jnp.cos(ang), jnp.sin(ang)


def apply_rope(x, cos, sin):
    xf = x.astype(jnp.float32)
    x1, x2 = jnp.split(xf, 2, axis=-1)
    c, s = cos[:, None], sin[:, None]
    return jnp.concatenate([x1 * c - x2 * s, x2 * c + x1 * s], axis=-1).astype(x.dtype)


def heads(t, n):
    B, S, _ = t.shape
    return t.reshape(B, S, n, -1).transpose(0, 2, 1, 3)


def merge_heads(t):
    B, H, S, d = t.shape
    return t.transpose(0, 2, 1, 3).reshape(B, S, H * d)


def banded_attention(q, k, v, window, block=Q_BLOCK):
    L, dh = q.shape[-2], q.shape[-1]
    nb = -(-L // block)
    Lp = nb * block
    n_prev = -(-window // block)

    def to_blocks(t):
        t = jnp.pad(t, [(0, 0)] * (t.ndim - 2) + [(0, Lp - L), (0, 0)])
        return t.reshape(t.shape[:-2] + (nb, block, dh))

    def band(t):
        tp = jnp.pad(t, [(0, 0)] * (t.ndim - 3) + [(n_prev, 0), (0, 0), (0, 0)])
        return jnp.concatenate([tp[..., j:j + nb, :, :] for j in range(n_prev + 1)], axis=-2)

    qb = to_blocks(q)
    kband = band(to_blocks(k))
    vband = band(to_blocks(v))
    qpos = jnp.arange(Lp).reshape(nb, block)[:, :, None]
    kpos = (jnp.arange(nb)[:, None] - n_prev) * block + jnp.arange((n_prev + 1) * block)[None, :]
    dist = qpos - kpos[:, None, :]
    mask = (dist >= 0) & (dist <= window) & (kpos[:, None, :] >= 0)
    s = jnp.einsum('...nqd,...nkd->...nqk', qb, kband, preferred_element_type=jnp.float32) * dh ** -0.5
    s = jnp.where(mask, s, NEG_INF)
    lse = jax.nn.logsumexp(s, axis=-1)
    p = jnp.exp(s - lse[..., None])
    o = jnp.einsum('...nqk,...nkd->...nqd', p.astype(v.dtype), vband)
    o = o.reshape(o.shape[:-3] + (Lp, dh))[..., :L, :]
    lse = lse.reshape(lse.shape[:-2] + (Lp,))[..., :L]
    return o, lse


def causal_attention_blocked(q, k, v):
    B, H, S, dq = q.shape
    nb = S // Q_BLOCK
    qb = q.reshape(B, H, nb, Q_BLOCK, dq).transpose(2, 0, 1, 3, 4)
    kpos = jnp.arange(S)

    def one(args):
        qi, b = args
        s = jnp.einsum('bhqd,bhkd->bhqk', qi, k, preferred_element_type=jnp.float32) * dq ** -0.5
        tq = b * Q_BLOCK + jnp.arange(Q_BLOCK)
        s = jnp.where(kpos[None, :] <= tq[:, None], s, NEG_INF)
        p = jax.nn.softmax(s, axis=-1)
        return jnp.einsum('bhqk,bhkd->bhqd', p.astype(v.dtype), v)

    o = lax.map(one, (qb, jnp.arange(nb)))
    return o.transpose(1, 2, 0, 3, 4).reshape(B, H, S, v.shape[-1])


def nsa_attention(q, kv, gate_logits, positions, cos, sin, cmp_pos_emb, w_ck1, w_ck2, w_cv1, w_cv2):
    B, H, S, dh = q.shape
    k_cr, v_cr, k_sr, v_sr, k_wr, v_wr = jnp.split(kv, 6, axis=-1)

    n_cmp = (S - CMP_LEN) // CMP_STRIDE + 1
    cmp_idx = np.arange(n_cmp)[:, None] * CMP_STRIDE + np.arange(CMP_LEN)[None, :]
    cmp_end = cmp_idx[:, -1]

    def compress(t, w1, w2):
        blk = t[:, cmp_idx] + cmp_pos_emb
        return jax.nn.gelu(blk.reshape(B, n_cmp, CMP_LEN * dh) @ w1) @ w2

    cos_c, sin_c = rope_tables(positions[:, cmp_end], dh)
    k_c = apply_rope(compress(k_cr, w_ck1, w_ck2)[:, None], cos_c, sin_c)[:, 0]
    v_c = compress(v_cr, w_cv1, w_cv2)

    n_slc = S // SLC_BLOCK
    n_sel = min(SLC_TOPK, n_slc)
    k_s = apply_rope(k_sr[:, None], cos, sin)[:, 0].reshape(B, n_slc, SLC_BLOCK, dh)
    v_s = v_sr.reshape(B, n_slc, SLC_BLOCK, dh)
    slc_start = np.arange(n_slc) * SLC_BLOCK
    overlap = ((cmp_idx[:, 0, None] < slc_start[None, :] + SLC_BLOCK) &
               (cmp_end[:, None] >= slc_start[None, :])).astype(np.float32)
    cmp_end_j = jnp.asarray(cmp_end)
    blk_ids = jnp.arange(n_slc)
    bidx = jnp.arange(B)[:, None, None]
    scale = dh ** -0.5
    nqb = S // Q_BLOCK
    qb = q.reshape(B, H, nqb, Q_BLOCK, dh).transpose(2, 0, 1, 3, 4)

    def one(args):
        qi, b = args
        tq = b * Q_BLOCK + jnp.arange(Q_BLOCK)
        s = jnp.einsum('bhqd,bcd->bhqc', qi, k_c, preferred_element_type=jnp.float32) * scale
        cmask = cmp_end_j[None, :] <= tq[:, None]
        p = jnp.where(cmask, jax.nn.softmax(jnp.where(cmask, s, NEG_INF), axis=-1), 0.0)
        o_c = jnp.einsum('bhqc,bcd->bhqd', p.astype(v_c.dtype), v_c)
        imp = jnp.einsum('bhqc,cj->bqj', p, overlap)
        cur = tq // SLC_BLOCK
        forced = (blk_ids[None, :] == 0) | (blk_ids[None, :] == cur[:, None]) | (blk_ids[None, :] == cur[:, None] - 1)
        valid = blk_ids[None, :] <= cur[:, None]
        imp = jnp.where(forced, BIG, jnp.where(valid, imp, -BIG))
        _, sel = lax.top_k(imp, n_sel)
        ks = k_s[bidx, sel]
        vs = v_s[bidx, sel]
        tok = sel[..., None] * SLC_BLOCK + jnp.arange(SLC_BLOCK)
        smask = (tok <= tq[None, :, None, None]).reshape(B, 1, Q_BLOCK, n_sel * SLC_BLOCK)
        s2 = jnp.einsum('bhqd,bqnkd->bhqnk', qi, ks, preferred_element_type=jnp.float32)
        s2 = jnp.where(smask, s2.reshape(B, H, Q_BLOCK, n_sel * SLC_BLOCK) * scale, NEG_INF)
        p2 = jax.nn.softmax(s2, axis=-1).reshape(B, H, Q_BLOCK, n_sel, SLC_BLOCK)
        o_s = jnp.einsum('bhqnk,bqnkd->bhqd', p2.astype(vs.dtype), vs)
        return o_c, o_s

    o_c, o_s = lax.map(one, (qb, jnp.arange(nqb)))
    o_c = o_c.transpose(1, 2, 0, 3, 4).reshape(B, H, S, dh)
    o_s = o_s.transpose(1, 2, 0, 3, 4).reshape(B, H, S, dh)

    k_w = jnp.broadcast_to(apply_rope(k_wr[:, None], cos, sin), (B, H, S, dh))
    v_w = jnp.broadcast_to(v_wr[:, None], (B, H, S, dh))
    o_w, _ = banded_attention(q, k_w, v_w, NSA_WINDOW)

    g = jax.nn.sigmoid(gate_logits).reshape(B, S, H, 3).transpose(0, 2, 1, 3)
    return g[..., 0:1] * o_c + g[..., 1:2] * o_s + g[..., 2:3] * o_w


def dilated_attention(q, k, v):
    B, H, S, dh = q.shape
    outs, lses = [], []
    for window, dil in DIL_PATTERNS:
        L = S // dil

        def sub(t):
            return t.reshape(B, H, L, dil, dh).transpose(0, 1, 3, 2, 4)

        o, lse = banded_attention(sub(q), sub(k), sub(v), window // dil)
        outs.append(o.transpose(0, 1, 3, 2, 4).reshape(B, H, S, dh))
        lses.append(lse.transpose(0, 1, 3, 2).reshape(B, H, S))
    w = jax.nn.softmax(jnp.stack(lses, axis=0), axis=0)
    o = w[0][..., None] * outs[0].astype(jnp.float32) + w[1][..., None] * outs[1].astype(jnp.float32) \
        + w[2][..., None] * outs[2].astype(jnp.float32)
    return o.astype(q.dtype)


def hybrid_mixer(h, positions, cos, sin, cos_r, sin_r, w_in, cmp_pos_emb, w_cmp_k1, w_cmp_k2, w_cmp_v1,
                 w_cmp_v2, g_q_lora, g_kv_lora, w_uq, w_ukv, w_out):
    proj = h @ w_in
    cuts = [int(c) for c in np.cumsum(IN_SPLITS)[:-1]]
    nsa_q, nsa_kv, nsa_g, cq, ckv, kr, dil_qkv = jnp.split(proj, cuts, axis=-1)

    q_a = apply_rope(heads(nsa_q, NSA_HEADS), cos, sin)
    o_a = nsa_attention(q_a, nsa_kv, nsa_g, positions, cos, sin, cmp_pos_emb, w_cmp_k1, w_cmp_k2, w_cmp_v1, w_cmp_v2)

    qm = heads(rms_norm(cq, g_q_lora) @ w_uq, MLA_HEADS)
    q_pe = apply_rope(qm[..., QK_NOPE:], cos_r, sin_r)
    kvm = heads(rms_norm(ckv, g_kv_lora) @ w_ukv, MLA_HEADS)
    k_nope, v_m = kvm[..., :QK_NOPE], kvm[..., QK_NOPE:]
    k_pe = apply_rope(kr[:, None], cos_r, sin_r)
    q_m = jnp.concatenate([qm[..., :QK_NOPE], q_pe], axis=-1)
    k_m = jnp.concatenate([k_nope, jnp.broadcast_to(k_pe, k_nope.shape[:-1] + (QK_ROPE,))], axis=-1)
    o_b = causal_attention_blocked(q_m, k_m, v_m)

    dq, dk, dv = jnp.split(dil_qkv, 3, axis=-1)
    o_c = dilated_attention(apply_rope(heads(dq, DIL_HEADS), cos, sin),
                            apply_rope(heads(dk, DIL_HEADS), cos, sin),
                            heads(dv, DIL_HEADS))

    o = jnp.concatenate([o_a, o_b, o_c], axis=1)
    return merge_heads(o) @ w_out


def memory_cross_attention(h, mem_n, w_xq, w_xkv, w_xo):
    q = heads(h @ w_xq, XATTN_HEADS)
    k, v = jnp.split(mem_n @ w_xkv, 2, axis=-1)
    k, v = heads(k, XATTN_HEADS), heads(v, XATTN_HEADS)
    s = jnp.einsum('bhqd,bhkd->bhqk', q, k, preferred_element_type=jnp.float32) * HEAD_DIM ** -0.5
    p = jax.nn.softmax(s, axis=-1)
    o = jnp.einsum('bhqk,bhkd->bhqd', p.astype(v.dtype), v)
    return merge_heads(o) @ w_xo


def squared_relu_mlp(h, w_up, w_down):
    return jnp.square(jax.nn.relu(h @ w_up)) @ w_down


def setup_inputs(seed: int = 0) -> dict:
    key = jax.random.key(seed)
    keys = list(jax.random.split(key, 32))
    it = iter(keys)

    def w(shape, fan_in):
        return jax.random.normal(next(it), shape, jnp.float32) * fan_in ** -0.5

    def gain(n):
        return 1.0 + 0.05 * jax.random.normal(next(it), (DEPTH, n), jnp.float32)

    x = jax.random.normal(next(it), (BATCH, SEQ, D_MODEL), jnp.float32)
    mem = jax.random.normal(next(it), (BATCH, MEM_LEN, D_MODEL), jnp.float32)
    offset = jax.random.randint(next(it), (BATCH, 1), 0, 4096, dtype=jnp.int32)
    positions = offset + jnp.arange(SEQ, dtype=jnp.int32)[None, :]
    return {
        'x': x,
        'mem': mem,
        'positions': positions,
        'g_mix_pre': gain(D_MODEL),
        'w_in': w((DEPTH, D_MODEL, D_IN), D_MODEL),
        'cmp_pos_emb': w((DEPTH, CMP_LEN, HEAD_DIM), 4),
        'w_cmp_k1': w((DEPTH, CMP_LEN * HEAD_DIM, CMP_HIDDEN), CMP_LEN * HEAD_DIM),
        'w_cmp_k2': w((DEPTH, CMP_HIDDEN, HEAD_DIM), CMP_HIDDEN),
        'w_cmp_v1': w((DEPTH, CMP_LEN * HEAD_DIM, CMP_HIDDEN), CMP_LEN * HEAD_DIM),
        'w_cmp_v2': w((DEPTH, CMP_HIDDEN, HEAD_DIM), CMP_HIDDEN),
        'g_q_lora': gain(Q_LORA),
        'g_kv_lora': gain(KV_LORA),
        'w_uq': w((DEPTH, Q_LORA, MLA_HEADS * (QK_NOPE + QK_ROPE)), Q_LORA),
        'w_ukv': w((DEPTH, KV_LORA, MLA_HEADS * (QK_NOPE + V_HEAD)), KV_LORA),
        'w_out': w((DEPTH, MIX_WIDTH, D_MODEL), MIX_WIDTH),
        'g_mix_post': gain(D_MODEL),
        'g_mem_pre': gain(D_MODEL),
        'g_mem_kv': gain(D_MODEL),
        'w_xq': w((DEPTH, D_MODEL, XATTN_DIM), D_MODEL),
        'w_xkv': w((DEPTH, D_MODEL, 2 * XATTN_DIM), D_MODEL),
        'w_xo': w((DEPTH, XATTN_DIM, D_MODEL), XATTN_DIM),
        'g_mem_post': gain(D_MODEL),
        'g_mlp_pre': gain(D_MODEL),
        'w_up': w((DEPTH, D_MODEL, D_FF), D_MODEL),
        'w_down': w((DEPTH, D_FF, D_MODEL), D_FF),
        'g_mlp_post': gain(D_MODEL),
    }


def reference(x, mem, positions, g_mix_pre, w_in, cmp_pos_emb, w_cmp_k1, w_cmp_k2, w_cmp_v1, w_cmp_v2,
              g_q_lora, g_kv_lora, w_uq, w_ukv, w_out, g_mix_post, g_mem_pre, g_mem_kv, w_xq, w_xkv, w_xo,
              g_mem_post, g_mlp_pre, w_up, w_down, g_mlp_post):
    cos, sin = rope_tables(positions, HEAD_DIM)
    cos_r, sin_r = rope_tables(positions, QK_ROPE)
    h = x
    for l in range(DEPTH):
        y = hybrid_mixer(rms_norm(h, g_mix_pre[l]), positions, cos, sin, cos_r, sin_r, w_in[l], cmp_pos_emb[l],
                         w_cmp_k1[l], w_cmp_k2[l], w_cmp_v1[l], w_cmp_v2[l], g_q_lora[l], g_kv_lora[l],
                         w_uq[l], w_ukv[l], w_out[l])
        h = h + rms_norm(y, g_mix_post[l])
        y = memory_cross_attention(rms_norm(h, g_mem_pre[l]), rms_norm(mem, g_mem_kv[l]), w_xq[l], w_xkv[l], w_xo[l])
        h = h + rms_norm(y, g_mem_post[l])
        y = squared_relu_mlp(rms_norm(h, g_mlp_pre[l]), w_up[l], w_down[l])
        h = h + rms_norm(y, g_mlp_post[l])
    return h
```

```python
import numpy as np
import ml_dtypes
from contextlib import ExitStack
import concourse.bass as bass
import concourse.mybir as mybir
from concourse.bass_utils import run_bass_kernel_spmd

F32 = mybir.dt.float32
BF16 = mybir.dt.bfloat16
I32 = mybir.dt.int32
ALU = mybir.AluOpType
AF = mybir.ActivationFunctionType
AX = mybir.AxisListType
NPBF = ml_dtypes.bfloat16

NCORES = 8


class T:
    __slots__ = ("name", "w", "rd", "excl")

    def __init__(self, name="", excl=False):
        self.name = name
        self.w = None
        self.rd = {}
        self.excl = excl


def PT():
    return T(excl=True)


class _Rec:
    def __getattr__(self, name):
        def f(*a, **kw):
            self.call = (name, a, kw)
            return self
        return f


class _Eng:
    def __init__(self, name):
        self.name = name
        self.q = []
        self.sem = None
        self.count = 0
        self.known = {}
        self.dsems = []
        self.drr = 0


class Prog:
    def __init__(self, n_dma_sems=20, dma_queues=("sp", "pool", "act")):
        self.nc = bass.Bass("TRN2", target_bir_lowering=False)
        self.es = ExitStack()
        self.sems = []
        self.semval = []
        self.E = {n: _Eng(n) for n in ("pe", "act", "dve", "pool", "sp")}
        for n in ("pe", "act", "dve", "pool"):
            self.E[n].sem = self._newsem("p_" + n)
        for qn in dma_queues:
            for i in range(n_dma_sems):
                self.E[qn].dsems.append(self._newsem("d_%s%d" % (qn, i)))
        self.out_tokens = []
        self.n_inst = 0

    def _newsem(self, name):
        h = self.es.enter_context(self.nc.semaphore(name))
        self.sems.append(h)
        self.semval.append(0)
        return len(self.sems) - 1

    def dram(self, name, shape, dt, kind):
        return self.nc.dram_tensor(name, list(shape), dt, kind=kind).ap()

    def sb(self, name, shape, dt):
        return self.es.enter_context(self.nc.sbuf_tensor(name, list(shape), dt))

    def ps(self, name, shape, dt=F32):
        return self.es.enter_context(self.nc.psum_tensor(name, list(shape), dt))

    def _deps(self, eng, reads, writes):
        need = {}

        def add(tok):
            if tok is None:
                return
            s, v = tok
            if need.get(s, 0) < v:
                need[s] = v

        for t in reads:
            add(t.w)
        for t in writes:
            add(t.w)
            for s, v in t.rd.items():
                add((s, v))
        out = []
        for s, v in need.items():
            if s == eng.sem and eng.name == "pe":
                continue
            if eng.known.get(s, 0) >= v:
                continue
            eng.known[s] = v
            out.append((s, v))
        return out

    def _emit_waits(self, eng, waits):
        for s, v in waits:
            h = self.sems[s]
            eng.q.append(lambda e, h=h, v=v: e.wait_ge(h, v))

    def op(self, en, fn, reads=(), writes=()):
        eng = self.E[en]
        ex = [t for t in reads if t.excl]
        if ex:
            reads = [t for t in reads if not t.excl]
            writes = list(writes) + ex
        waits = self._deps(eng, reads, writes)
        self._emit_waits(eng, waits)
        eng.count += 1
        c = eng.count
        h = self.sems[eng.sem]
        rec = _Rec()
        fn(rec)
        name, a, kw = rec.call
        eng.q.append(lambda e, name=name, a=a, kw=kw, h=h: getattr(e, name)(*a, **kw).then_inc(h, 1))
        tok = (eng.sem, c)
        for t in reads:
            if t.rd.get(eng.sem, 0) < c:
                t.rd[eng.sem] = c
        for t in writes:
            t.w = tok
            t.rd = {}
        self.n_inst += 1
        return tok

    def dma(self, qn, out, in_, reads=(), writes=(), is_output=False):
        eng = self.E[qn]
        k = eng.drr
        eng.drr = (k + 1) % len(eng.dsems)
        s = eng.dsems[k]
        waits = self._deps(eng, reads, writes)
        prev = self.semval[s]
        if prev > 0 and eng.known.get(s, 0) < prev:
            eng.known[s] = prev
            waits.append((s, prev))
        self._emit_waits(eng, waits)
        self.semval[s] = prev + 16
        v = prev + 16
        h = self.sems[s]
        eng.q.append(lambda e, out=out, in_=in_, h=h: e.dma_start(out=out, in_=in_).then_inc(h, 16))
        tok = (s, v)
        for t in reads:
            if t.rd.get(s, 0) < v:
                t.rd[s] = v
        for t in writes:
            t.w = tok
            t.rd = {}
        if is_output:
            self.out_tokens.append(tok)
        self.n_inst += 1
        return tok

    def finish(self):
        sp = self.E["sp"]
        fin = {}
        for s, v in self.out_tokens:
            fin[s] = max(fin.get(s, 0), v)
        for s in range(len(self.sems)):
            if self.semval[s] > 0:
                fin[s] = max(fin.get(s, 0), self.semval[s])
        for n in ("pe", "act", "dve", "pool"):
            e = self.E[n]
            if e.count:
                fin[e.sem] = e.count
        for s, v in fin.items():
            h = self.sems[s]
            sp.q.append(lambda e, h=h, v=v: e.wait_ge(h, v))
        with self.nc.Block() as block:
            @block.sync
            def _(e):
                for f in self.E["sp"].q:
                    f(e)

            @block.tensor
            def _(e):
                for f in self.E["pe"].q:
                    f(e)

            @block.scalar
            def _(e):
                for f in self.E["act"].q:
                    f(e)

            @block.vector
            def _(e):
                for f in self.E["dve"].q:
                    f(e)

            @block.gpsimd
            def _(e):
                for f in self.E["pool"].q:
                    f(e)
        self.es.close()
        return self.nc


def build_cast(F, CH=4096):
    P = Prog()
    x = P.dram("x", [128, F], F32, "ExternalInput")
    y = P.dram("y", [128, F], BF16, "ExternalOutput")
    NB = 3
    xin = [P.sb("xin%d" % i, [128, CH], F32) for i in range(NB)]
    xo = [P.sb("xo%d" % i, [128, CH], BF16) for i in range(NB)]
    tin = [T() for _ in range(NB)]
    to = [T() for _ in range(NB)]
    engs = ["dve", "pool", "act"]
    nch = (F + CH - 1) // CH
    for c in range(nch):
        b = c % NB
        w = min(CH, F - c * CH)
        P.dma("sp", xin[b][:, :w], x[:, c * CH:c * CH + w], writes=[tin[b]])
        en = engs[c % 3]
        if en == "act":
            P.op(en, lambda e, b=b, w=w: e.copy(xo[b][:, :w], xin[b][:, :w]), reads=[tin[b]], writes=[to[b]])
        else:
            P.op(en, lambda e, b=b, w=w: e.tensor_copy(xo[b][:, :w], xin[b][:, :w]), reads=[tin[b]], writes=[to[b]])
        P.dma("pool", y[:, c * CH:c * CH + w], xo[b][:, :w], reads=[to[b]], is_output=True)
    return P.finish()


def cast_on_device(flat):
    n = flat.size
    per = -(-n // (NCORES * 128))
    per = -(-per // 8) * 8
    pad = np.zeros(NCORES * 128 * per, np.float32)
    pad[:n] = flat
    shards = pad.reshape(NCORES, 128, per)
    nc = build_cast(per)
    res = run_bass_kernel_spmd(nc, [{"x": shards[i]} for i in range(NCORES)], core_ids=list(range(NCORES)))
    out = np.concatenate([np.asarray(r["y"]).reshape(-1) for r in res.results])
    return out[:n]


D = 2048
S = 16384
B = 2
DIN = 4684
EPS = 1e-6
TWO_PI = 6.283185307179586
CW1 = 6.28125
CW2 = TWO_PI - CW1
W_IN_PERM = np.concatenate([np.arange(0, 1280), np.arange(1292, 2316), np.arange(2380, 4684),
                            np.arange(2316, 2380), np.arange(1280, 1292)])
C_NQ, C_KC, C_VC, C_KS, C_VS, C_KW, C_VW = 0, 512, 640, 768, 896, 1024, 1152
C_CQ, C_CKV, C_DQ, C_DK, C_DV, C_KR, C_G = 1280, 1792, 2304, 3072, 3840, 4608, 4672
NFB = 36
NVB = 14


def _sincos(P, ang, sin_out, cos_out, y, ki, kf, r1, t_ang, t_out, t_scr):
    import math
    for which, dst in ((0, sin_out), (1, cos_out)):
        if which == 1:
            P.op("dve", lambda e: e.tensor_scalar(r1, ang, math.pi / 2, None, ALU.add), reads=[t_ang], writes=[t_scr])
            src = r1
        else:
            src = ang
        P.op("dve", lambda e, src=src: e.tensor_scalar(y, src, 1.0 / TWO_PI, None, ALU.mult), reads=[t_ang, t_scr], writes=[t_scr])
        P.op("dve", lambda e: e.tensor_copy(ki, y), reads=[t_scr], writes=[t_scr])
        P.op("dve", lambda e: e.tensor_copy(kf, ki), reads=[t_scr], writes=[t_scr])
        P.op("dve", lambda e, src=src: e.scalar_tensor_tensor(y, kf, -CW1, src, ALU.mult, ALU.add), reads=[t_scr, t_ang], writes=[t_scr])
        P.op("dve", lambda e: e.scalar_tensor_tensor(y, kf, -CW2, y, ALU.mult, ALU.add), reads=[t_scr], writes=[t_scr])
        P.op("dve", lambda e: e.tensor_scalar(y, y, -math.pi, math.pi, ALU.max, ALU.min), reads=[t_scr], writes=[t_scr])
        P.op("act", lambda e, dst=dst: e.activation(dst, y, AF.Sin), reads=[t_scr], writes=[t_out])


def _rope(P, en, src, dst, cos, sin, H, hd, tmp_a, tmp_b, rd, wr, t_tab, t_tmp):
    c = cos.unsqueeze(1).broadcast_to([128, H, hd])
    s = sin.unsqueeze(1).broadcast_to([128, H, hd])
    x1, x2 = src[:, :, 0:hd], src[:, :, hd:2 * hd]
    ta, tb = tmp_a[:, 0:H, 0:hd], tmp_b[:, 0:H, 0:hd]
    P.op(en, lambda e: e.tensor_tensor(ta, x1, c, ALU.mult), reads=rd + [t_tab], writes=[t_tmp])
    P.op(en, lambda e: e.tensor_tensor(tb, x2, s, ALU.mult), reads=rd + [t_tab], writes=[t_tmp])
    P.op(en, lambda e: e.tensor_tensor(dst[:, :, 0:hd], ta, tb, ALU.subtract), reads=[t_tmp], writes=wr)
    P.op(en, lambda e: e.tensor_tensor(ta, x2, c, ALU.mult), reads=rd + [t_tab], writes=[t_tmp])
    P.op(en, lambda e: e.tensor_tensor(tb, x1, s, ALU.mult), reads=rd + [t_tab], writes=[t_tmp])
    P.op(en, lambda e: e.tensor_tensor(dst[:, :, hd:2 * hd], ta, tb, ALU.add), reads=[t_tmp], writes=wr)


def _rstd(P, ss, rstd, n, epsb, rd, wr):
    P.op("act", lambda e: e.activation(rstd, ss, AF.Sqrt, bias=epsb, scale=1.0 / n), reads=rd, writes=wr)
    P.op("dve", lambda e: e.reciprocal(rstd, rstd), reads=wr, writes=wr)


def build_p1(NT=4096, ST=2):
    NTL = NT // 128
    NSUP = NTL // ST
    P = Prog()
    h = P.dram("h", [NT, D], F32, "ExternalInput")
    posT = P.dram("posT", [128, NTL], I32, "ExternalInput")
    inv128 = P.dram("inv128", [128, 64], F32, "ExternalInput")
    inv64 = P.dram("inv64", [128, 32], F32, "ExternalInput")
    g_pre = P.dram("g_pre", [128, D], F32, "ExternalInput")
    w_in = P.dram("w_in", [128, 16, DIN], BF16, "ExternalInput")
    g_q = P.dram("g_q", [128, 512], F32, "ExternalInput")
    g_kv = P.dram("g_kv", [128, 512], F32, "ExternalInput")
    w_uq = P.dram("w_uq", [128, 4, 1152], BF16, "ExternalInput")
    w_ukv = P.dram("w_ukv", [128, 4, 1536], BF16, "ExternalInput")
    ident = P.dram("ident", [128, 128], BF16, "ExternalInput")
    FT = P.dram("FT", [128, NFB, NT], BF16, "ExternalOutput")
    VT = P.dram("VT", [NT, NVB, 129], BF16, "ExternalOutput")
    GT = P.dram("GT", [NT, 12], F32, "ExternalOutput")

    s_g = P.sb("s_g", [128, D], F32); t_g = T()
    s_gq = P.sb("s_gq", [128, 512], F32)
    s_gkv = P.sb("s_gkv", [128, 512], F32)
    s_wuq = P.sb("s_wuq", [128, 4, 1152], BF16)
    s_wukv = P.sb("s_wukv", [128, 4, 1536], BF16)
    s_id = P.sb("s_id", [128, 128], BF16)
    s_pos = P.sb("s_pos", [128, NTL], I32)
    s_posf = P.sb("s_posf", [128, NTL], F32)
    s_inv128 = P.sb("s_inv128", [128, 64], F32)
    s_inv64 = P.sb("s_inv64", [128, 32], F32)
    epsb = P.sb("epsb", [128, 1], F32)
    t_c = T()
    for dst, src in ((s_g, g_pre), (s_gq, g_q), (s_gkv, g_kv), (s_wuq, w_uq), (s_wukv, w_ukv), (s_id, ident),
                     (s_pos, posT), (s_inv128, inv128), (s_inv64, inv64)):
        P.dma("sp", dst[:], src[:], writes=[t_c])
    P.op("dve", lambda e: e.memset(epsb[:], EPS), writes=[t_c])
    P.op("dve", lambda e: e.tensor_copy(s_posf[:], s_pos[:]), reads=[t_c], writes=[t_c])

    cos128 = P.sb("cos128", [128, NTL, 64], F32)
    sin128 = P.sb("sin128", [128, NTL, 64], F32)
    cos64 = P.sb("cos64", [128, NTL, 32], F32)
    sin64 = P.sb("sin64", [128, NTL, 32], F32)
    t_tab = T()
    pj = [P.sb("pj%d" % j, [128, DIN], F32) for j in range(ST)]
    t_pj = [T() for _ in range(ST)]
    s_ki = P.sb("s_ki", [128, NTL * 64], I32)
    t_ang, t_scr = T(), T()
    for hd, inv, co, si in ((64, s_inv128, cos128, sin128), (32, s_inv64, cos64, sin64)):
        n = NTL * hd
        ang = pj[0][:, 0:n].rearrange("p (t d) -> p t d", d=hd)
        yv = pj[0][:, 2048:2048 + n].rearrange("p (t d) -> p t d", d=hd)
        kfv = pj[1][:, 0:n].rearrange("p (t d) -> p t d", d=hd)
        r1v = pj[1][:, 2048:2048 + n].rearrange("p (t d) -> p t d", d=hd)
        kiv = s_ki[:, 0:n].rearrange("p (t d) -> p t d", d=hd)
        pb = s_posf[:].unsqueeze(2).broadcast_to([128, NTL, hd])
        ib = inv[:].unsqueeze(1).broadcast_to([128, NTL, hd])
        P.op("dve", lambda e, ang=ang, pb=pb, ib=ib: e.tensor_tensor(ang, pb, ib, ALU.mult), reads=[t_c, t_scr], writes=[t_ang])
        _sincos(P, ang, si[:], co[:], yv, kiv, kfv, r1v, t_ang, t_tab, t_scr)
    for j in range(ST):
        t_pj[j] = T()
        t_pj[j].rd = dict(t_tab.rd)
    t_pj[0].w = t_scr.w; t_pj[0].rd = dict(t_scr.rd); t_pj[0].rd.update(t_ang.rd)
    if ST > 1:
        t_pj[1].w = t_scr.w; t_pj[1].rd = dict(t_scr.rd)
    if t_ang.w is not None:
        s_, v_ = t_ang.w
        t_pj[0].rd[s_] = max(t_pj[0].rd.get(s_, 0), v_)

    hin = [P.sb("hin%d" % i, [128, D], F32) for i in range(2)]
    t_hin = [T(), T()]
    hn = P.sb("hn", [128, D], BF16); t_hn = T()
    ss = P.sb("ss", [128, 4], F32); t_ss = T()
    hnT = [P.sb("hnT%d" % j, [128, 16, 128], BF16) for j in range(ST)]
    t_hnT = [T() for _ in range(ST)]
    wg = [P.sb("wg%d" % i, [128, 16, 512], BF16) for i in range(2)]
    t_wg = [T(), T()]
    rb = P.sb("rb", [128, NFB, 128], BF16)
    t_rb = [T() for _ in range(9)]
    tsb = P.sb("tsb", [128, NFB, ST * 128], BF16); t_tsb = T()
    vt = P.sb("vt", [128, NVB, 129], BF16); t_vt = T()
    gsb = P.sb("gsb", [128, 12], F32); t_gsb = T()
    cqn = P.sb("cqn", [128, 2, 512], BF16); t_cqn = T()
    cT = P.sb("cT", [128, 2, 4, 128], BF16); t_cT = T()
    tmp_a = P.sb("tmp_a", [128, 12, 64], F32)
    tmp_b = P.sb("tmp_b", [128, 12, 64], F32)
    t_tmp = T()
    tmp_c = P.sb("tmp_c", [128, 1, 64], F32)
    tmp_d = P.sb("tmp_d", [128, 1, 64], F32)
    t_tmp2 = T()
    ps_tr = [P.ps("ps_tr%d" % i, [128, 4, 128], BF16) for i in range(2)]
    t_ps_tr = [PT(), PT()]
    ps_mm = [P.ps("ps_mm%d" % i, [128, 512], F32) for i in range(2)]
    t_ps_mm = [PT(), PT()]
    ps_ml = [P.ps("ps_ml%d" % i, [128, 512], F32) for i in range(2)]
    t_ps_ml = [PT(), PT()]

    P.op("pool", lambda e: e.memset(vt[:], 1.0), writes=[t_vt])
    P.op("pool", lambda e: e.memset(rb[:], 0.0), writes=t_rb)

    ngrp = (DIN + 511) // 512
    trc = [0]
    mmc = [0]
    mlc = [0]
    evc = [0]

    def transposes(src_blocks, dst_fn, rd, wr):
        k = trc[0] % 2
        trc[0] += 1
        for i, sa in enumerate(src_blocks):
            w = sa.shape[1]
            P.op("pe", lambda e, i=i, sa=sa, w=w, k=k: e.transpose(ps_tr[k][0:w, i, :], sa, s_id[:]),
                 reads=rd + [t_c], writes=[t_ps_tr[k]])
        en = "act" if evc[0] % 2 == 0 else "dve"
        evc[0] += 1
        dst, src = dst_fn(ps_tr[k])
        if en == "act":
            P.op("act", lambda e: e.copy(dst, src), reads=[t_ps_tr[k]], writes=wr)
        else:
            P.op("dve", lambda e: e.tensor_copy(dst, src), reads=[t_ps_tr[k]], writes=wr)

    import os
    LEVEL = float(os.environ.get("P1_LEVEL", "9"))
    PEN = "dve" if os.environ.get("P1_NOPOOL") else "pool"
    for st in range(NSUP if LEVEL >= 2 else 0):
        for j in range(ST):
            tl = st * ST + j
            b = tl % 2
            P.dma("sp", hin[b][:], h[tl * 128:(tl + 1) * 128, :], writes=[t_hin[b]])
            P.op("act", lambda e, b=b: e.activation(hn[:], hin[b][:], AF.Square, accum_out=ss[:, 0:1]),
                 reads=[t_hin[b]], writes=[t_hn, t_ss])
            _rstd(P, ss[:, 0:1], ss[:, 1:2], D, epsb[:], [t_ss, t_c], [t_ss])
            P.op("dve", lambda e, b=b: e.scalar_tensor_tensor(hn[:], hin[b][:], ss[:, 1:2], s_g[:], ALU.mult, ALU.mult),
                 reads=[t_hin[b], t_ss, t_c], writes=[t_hn])
            for g4 in range(4):
                transposes([hn[:, (4 * g4 + i) * 128:(4 * g4 + i + 1) * 128] for i in range(4)],
                           lambda pt, j=j, g4=g4: (hnT[j][:, 4 * g4:4 * g4 + 4, :], pt[:]),
                           [t_hn], [t_hnT[j]])
        for g in range(ngrp if LEVEL >= 3 else 0):
            n0 = g * 512
            nw = min(512, DIN - n0)
            wb = (st * ngrp + g) % 2
            P.dma("pool", wg[wb][:, :, 0:nw], w_in[:, :, n0:n0 + nw], writes=[t_wg[wb]])
            for j in range(ST):
                k = mmc[0] % 2
                mmc[0] += 1
                for kc in range(16):
                    P.op("pe", lambda e, k=k, j=j, kc=kc, wb=wb, nw=nw: e.matmul(
                        ps_mm[k][:, 0:nw], hnT[j][:, kc, :], wg[wb][:, kc, 0:nw], start=(kc == 0), stop=(kc == 15)),
                        reads=[t_hnT[j], t_wg[wb]], writes=[t_ps_mm[k]])
                P.op("act", lambda e, k=k, j=j, n0=n0, nw=nw: e.copy(pj[j][:, n0:n0 + nw], ps_mm[k][:, 0:nw]),
                     reads=[t_ps_mm[k]], writes=[t_pj[j]])
        for j in range(ST if LEVEL >= 4 else 0):
            tl = st * ST + j
            pjt = pj[j]
            c128, s128 = cos128[:, tl, :], sin128[:, tl, :]
            c64, s64 = cos64[:, tl, :], sin64[:, tl, :]
            rd = [t_pj[j]]

            def blk(b0, nb):
                return rb[:, b0:b0 + nb, :]

            def colv(c0, nb):
                return pjt[:, c0:c0 + nb * 128].rearrange("p (h d) -> p h d", d=128)
            _rope(P, "dve", colv(C_NQ, 4), blk(0, 4), c128, s128, 4, 64, tmp_a, tmp_b, rd, [t_rb[0]], t_tab, t_tmp)
            _rope(P, PEN, colv(C_KS, 1), blk(6, 1), c128, s128, 1, 64, tmp_c, tmp_d, rd, [t_rb[1]], t_tab, t_tmp2)
            _rope(P, PEN, colv(C_KW, 1), blk(7, 1), c128, s128, 1, 64, tmp_c, tmp_d, rd, [t_rb[1]], t_tab, t_tmp2)
            _rope(P, "dve", colv(C_DQ, 12), blk(20, 12), c128, s128, 12, 64, tmp_a, tmp_b, rd, [t_rb[5], t_rb[6], t_rb[7]], t_tab, t_tmp)
            if LEVEL < 4.2:
                continue
            P.op(PEN, lambda e: e.tensor_copy(blk(4, 2), colv(C_KC, 2)), reads=rd, writes=[t_rb[1]])
            _rope(P, PEN, pjt[:, C_KR:C_KR + 64].unsqueeze(1), rb[:, 35, 0:64].unsqueeze(1), c64, s64, 1, 32,
                  tmp_c, tmp_d, rd, [t_rb[8]], t_tab, t_tmp2)
            P.op(PEN, lambda e: e.tensor_copy(vt[:, 0, 0:128], pjt[:, C_VS:C_VS + 128]), reads=rd, writes=[t_vt])
            P.op(PEN, lambda e: e.tensor_copy(vt[:, 1, 0:128], pjt[:, C_VW:C_VW + 128]), reads=rd, writes=[t_vt])
            P.op(PEN, lambda e: e.tensor_copy(vt[:, 8:14, 0:128], colv(C_DV, 6)), reads=rd, writes=[t_vt])
            P.op("act", lambda e: e.activation(gsb[:], pjt[:, C_G:C_G + 12], AF.Sigmoid), reads=rd, writes=[t_gsb])
            P.dma("pool", GT[tl * 128:(tl + 1) * 128, :], gsb[:], reads=[t_gsb], is_output=True)
            if LEVEL < 4.3:
                continue
            for which, c0, gsbuf in ((0, C_CQ, s_gq), (1, C_CKV, s_gkv)):
                P.op("act", lambda e, which=which, c0=c0: e.activation(cqn[:, which, :], pjt[:, c0:c0 + 512], AF.Square,
                                                                     accum_out=ss[:, 2:3]),
                     reads=rd, writes=[t_cqn, t_ss])
                _rstd(P, ss[:, 2:3], ss[:, 3:4], 512, epsb[:], [t_ss, t_c], [t_ss])
                P.op("dve", lambda e, which=which, c0=c0, gsbuf=gsbuf: e.scalar_tensor_tensor(
                    cqn[:, which, :], pjt[:, c0:c0 + 512], ss[:, 3:4], gsbuf[:], ALU.mult, ALU.mult),
                    reads=rd + [t_ss, t_c], writes=[t_cqn])
                transposes([cqn[:, which, i * 128:(i + 1) * 128] for i in range(4)],
                           lambda pt, which=which: (cT[:, which, :, :], pt[:]), [t_cqn], [t_cT])
            if LEVEL < 4.4:
                continue
            for g in range(3):
                k = mlc[0] % 2
                mlc[0] += 1
                for kc in range(4):
                    P.op("pe", lambda e, k=k, kc=kc, g=g: e.matmul(ps_ml[k][:, 0:384], cT[:, 0, kc, :],
                                                                  s_wuq[:, kc, g * 384:(g + 1) * 384],
                                                                  start=(kc == 0), stop=(kc == 3)),
                         reads=[t_cT, t_c], writes=[t_ps_ml[k]])
                pv = ps_ml[k][:, 0:384].rearrange("p (h d) -> p h d", d=192)
                P.op("act", lambda e, pv=pv, g=g: e.copy(rb[:, 8 + 2 * g:10 + 2 * g, :], pv[:, :, 0:128]),
                     reads=[t_ps_ml[k]], writes=[t_rb[2], t_rb[3]])
                if LEVEL < 4.42:
                    continue
                qpe_dst = rb[:, 32:35, :].rearrange("p b (h d) -> p (b h) d", d=64)[:, 2 * g:2 * g + 2, :]
                _rope(P, "dve", pv[:, :, 128:192], qpe_dst, c64, s64, 2, 32, tmp_a, tmp_b, [t_ps_ml[k]], [t_rb[8]], t_tab, t_tmp)
            if LEVEL < 4.5:
                continue
            for g in range(3):
                k = mlc[0] % 2
                mlc[0] += 1
                for kc in range(4):
                    P.op("pe", lambda e, k=k, kc=kc, g=g: e.matmul(ps_ml[k][:, 0:512], cT[:, 1, kc, :],
                                                                  s_wukv[:, kc, g * 512:(g + 1) * 512],
                                                                  start=(kc == 0), stop=(kc == 3)),
                         reads=[t_cT, t_c], writes=[t_ps_ml[k]])
                pv = ps_ml[k][:, 0:512].rearrange("p (h d) -> p h d", d=256)
                P.op("act", lambda e, pv=pv, g=g: e.copy(rb[:, 14 + 2 * g:16 + 2 * g, :], pv[:, :, 0:128]),
                     reads=[t_ps_ml[k]], writes=[t_rb[3], t_rb[4]])
                P.op("dve", lambda e, pv=pv, g=g: e.tensor_copy(vt[:, 2 + 2 * g:4 + 2 * g, 0:128], pv[:, :, 128:256]),
                     reads=[t_ps_ml[k]], writes=[t_vt])
            P.dma("pool", VT[tl * 128:(tl + 1) * 128, :, :], vt[:], reads=[t_vt], is_output=True)
            for g4 in range(9 if LEVEL >= 5 else 0):
                srcs = [rb[:, 4 * g4 + i, :] for i in range(4)]
                if g4 == 8:
                    srcs[3] = rb[:, 35, 0:64]
                transposes(srcs, lambda pt, j=j, g4=g4: (tsb[:, 4 * g4:4 * g4 + 4, j * 128:(j + 1) * 128], pt[:]),
                           t_rb, [t_tsb])
        if LEVEL >= 6:
            P.dma("pool", FT[:, :, st * ST * 128:(st + 1) * ST * 128], tsb[:], reads=[t_tsb], is_output=True)
    return P.finish()


NEG = -30000.0
SC128 = 128 ** -0.5
SC192 = 192 ** -0.5


def build_A(S=16384):
    import math
    NI = S // 2048
    NTQ = NI * 512
    NCC = S // 2048
    NCMP = S // 16
    P = Prog(n_dma_sems=12, dma_queues=("sp", "pool"))
    di = lambda n, sh, dt=BF16: P.dram(n, sh, dt, "ExternalInput")
    QaT = di("QaT", [128, 4, NTQ]); QnT = di("QnT", [128, 6, NTQ]); QpeT = di("QpeT", [64, 6, NTQ])
    DqT = di("DqT", [128, 6, NTQ]); GTd = di("GT", [NTQ, 12], F32)
    KsT = di("KsT", [128, S]); Vs = di("Vs", [S, 129]); KnT = di("KnT", [128, 6, S]); KpeT = di("KpeT", [64, S])
    Vm = di("Vm", [S, 6, 129]); KcrT = di("KcrT", [128, S]); VcrT = di("VcrT", [128, S])
    KwL = di("KwL", [128, NI, 1024]); VwL = di("VwL", [NI, 1024, 129])
    DkL = di("DkL", [128, NI, 6, 2560]); DvL = di("DvL", [NI, 2560, 6, 129])
    w_ck1 = di("w_ck1", [128, 32, 128]); w_ck2 = di("w_ck2", [128, 128])
    w_cv1 = di("w_cv1", [128, 32, 128]); w_cv2 = di("w_cv2", [128, 128])
    pembT = di("pembT", [128, 32]); cposT = di("cposT", [128, NCC], I32); inv128 = di("inv128", [128, 64], F32)
    caus = di("caus", [128, 4, 2048]); cmask = di("cmask", [128, 4, 128]); fmask = di("fmask", [128, 4, 64], F32)
    wm = di("wm", [128, 5, 128]); dm = di("dm", [128, 17, 128]); ident = di("ident", [128, 128]); I4d = di("I4", [128, 512])
    O = P.dram("O", [NTQ, 2048], BF16, "ExternalOutput")

    t_c = T()
    def cload(name, src, shape, dt=BF16):
        t = P.sb(name, shape, dt)
        P.dma("sp", t[:], src[:], writes=[t_c])
        return t
    s_caus = cload("s_caus", caus, [128, 4, 2048]); s_cmask = cload("s_cmask", cmask, [128, 4, 128])
    s_fmask = cload("s_fmask", fmask, [128, 4, 64], F32); s_wm = cload("s_wm", wm, [128, 5, 128])
    s_dm = cload("s_dm", dm, [128, 17, 128]); s_id = cload("s_id", ident, [128, 128]); s_I4 = cload("s_I4", I4d, [128, 512])
    s_w1 = [cload("s_wk1", w_ck1, [128, 32, 128]), cload("s_wv1", w_cv1, [128, 32, 128])]
    s_w2 = [cload("s_wk2", w_ck2, [128, 128]), cload("s_wv2", w_cv2, [128, 128])]
    s_pemb = cload("s_pemb", pembT, [128, 32]); s_cpos = cload("s_cpos", cposT, [128, NCC], I32)
    s_inv = cload("s_inv", inv128, [128, 64], F32)
    tiny = P.sb("tiny", [128, 1], F32)
    P.op("dve", lambda e: e.memset(tiny[:], 0.0), writes=[t_c])

    ps_s = [P.ps("ps_s%d" % i, [128, 512]) for i in range(2)]; t_ps_s = [PT(), PT()]
    ps_o = [P.ps("ps_o%d" % i, [128, 512]) for i in range(4)]; t_ps_o = [PT() for _ in range(4)]
    ps_c = [P.ps("ps_c%d" % i, [128, 512]) for i in range(2)]; t_ps_c = [PT(), PT()]

    selx = P.sb("selx", [128, S], BF16); t_selx = T()
    kct = P.sb("kct", [128, NCMP], BF16); t_kct = T()
    vca = P.sb("vca", [128, NCC, 129], BF16); t_vca = T()
    pt = [P.sb("pt%d" % i, [128, 512], BF16) for i in range(2)]; t_pt = [T(), T()]
    ptc = [0]
    ssc = [0]

    def score(terms, rd):
        k = ssc[0] % 2
        ssc[0] += 1
        n = len(terms)
        for idx, (of, l, r) in enumerate(terms):
            o = of(ps_s[k])
            P.op("pe", lambda e, o=o, l=l, r=r, idx=idx: e.matmul(o, l, r, start=(idx == 0), stop=(idx == n - 1),
                                                                 skip_group_check=True),
                 reads=rd, writes=[t_ps_s[k]])
        return k

    def expo(k, ncols, scale):
        j = ptc[0] % 2
        ptc[0] += 1
        P.op("act", lambda e: e.activation(pt[j][:, 0:ncols], ps_s[k][:, 0:ncols], AF.Exp, scale=scale),
             reads=[t_ps_s[k]], writes=[t_pt[j]])
        return j

    kcr = selx
    hidT = P.sb("hidT", [128, 512], BF16); t_hid = T()
    gx = [P.sb("gx%d" % i, [128, 512], F32) for i in range(3)]; t_gx = T()
    cb = P.sb("cb", [128, 2], F32); t_cb = T()
    ctab = P.sb("ctab", [128, 2, NCC, 64], F32); t_ctab = T()
    pexp = P.sb("pexp", [128, 4, 1024], F32); t_pexp = T()
    cscr = pexp[:, :, 0:NCC * 64]; t_cscr = t_pexp
    cki = P.sb("cki", [128, NCC * 64], I32)
    cposf = P.sb("cposf", [128, NCC], F32)
    P.op("dve", lambda e: e.tensor_copy(cposf[:], s_cpos[:]), reads=[t_c], writes=[t_cscr])
    angv = cscr[:, 0, :].rearrange("p (t d) -> p t d", d=64)
    P.op("dve", lambda e: e.tensor_tensor(angv, cposf[:].unsqueeze(2).broadcast_to([128, NCC, 64]),
                                          s_inv[:].unsqueeze(1).broadcast_to([128, NCC, 64]), ALU.mult),
         reads=[t_c, t_cscr], writes=[t_cscr])
    t_ang = T(); t_ang.w = t_cscr.w
    vv = lambda i: cscr[:, i, :].rearrange("p (t d) -> p t d", d=64)
    _sincos(P, angv, ctab[:, 1], ctab[:, 0], vv(1), cki[:].rearrange("p (t d) -> p t d", d=64), vv(2), vv(3), t_ang, t_ctab, t_cscr)
    kc_tm = P.sb("kc_tm", [128, 128], BF16); t_kctm = T()
    ra = P.sb("ra", [128, 1, 64], F32); rb_ = P.sb("rb_", [128, 1, 64], F32); t_rt = T()
    ps_tr = P.ps("ps_trA", [128, 128], BF16) if False else None
    P.op("pool", lambda e: e.memset(vca[:], 1.0), writes=[t_vca])
    for which, src in ((0, KcrT), (1, VcrT)):
        P.dma("sp", selx[:, 0:S], src[:, 0:S], writes=[t_selx])
        for i in range(32):
            P.op("pe", lambda e, i=i: e.matmul(ps_c[0][:, 0:1], s_w1[which][:, i, :], s_pemb[:, i:i + 1],
                                               start=(i == 0), stop=(i == 31)), reads=[t_c], writes=[t_ps_c[0]])
        P.op("act", lambda e: e.copy(cb[:, which:which + 1], ps_c[0][:, 0:1]), reads=[t_ps_c[0]], writes=[t_cb])
        for c0 in range(0, NCMP, 512):
            cw = min(512, NCMP - c0)
            for i in range(32):
                lo = 16 * c0 + i
                n_in = min(cw, (S - lo + 15) // 16)
                rhs_main = selx[:, lo:lo + 16 * (n_in - 1) + 1:16]
                P.op("pe", lambda e, i=i, rhs_main=rhs_main, n_in=n_in: e.matmul(
                    ps_c[1][:, 0:n_in], s_w1[which][:, i, :], rhs_main, start=(i == 0), stop=(i == 31),
                    skip_group_check=True), reads=[t_selx, t_c], writes=[t_ps_c[1]])
            x, x2, u = gx[0][:, 0:cw], gx[1][:, 0:cw], gx[2][:, 0:cw]
            P.op("act", lambda e, x=x, cw=cw: e.activation(x, ps_c[1][:, 0:cw], AF.Identity, bias=cb[:, which:which + 1]),
                 reads=[t_ps_c[1], t_cb], writes=[t_gx])
            P.op("dve", lambda e, x=x, x2=x2: e.tensor_tensor(x2, x, x, ALU.mult), reads=[t_gx], writes=[t_gx])
            P.op("dve", lambda e, x2=x2: e.tensor_scalar(x2, x2, 0.044715, 1.0, ALU.mult, ALU.add), reads=[t_gx], writes=[t_gx])
            P.op("dve", lambda e, x=x, x2=x2, u=u: e.tensor_tensor(u, x2, x, ALU.mult), reads=[t_gx], writes=[t_gx])
            P.op("act", lambda e, u=u: e.activation(u, u, AF.Tanh, scale=0.7978845608028654), reads=[t_gx], writes=[t_gx])
            P.op("dve", lambda e, u=u: e.tensor_scalar(u, u, 1.0, 0.5, ALU.add, ALU.mult), reads=[t_gx], writes=[t_gx])
            P.op("dve", lambda e, u=u, x=x, cw=cw: e.tensor_tensor(hidT[:, 0:cw], u, x, ALU.mult), reads=[t_gx], writes=[t_hid])
            for cc in range(cw // 128):
                gch = (c0 + cc * 128) // 128
                P.op("pe", lambda e, cc=cc: e.matmul(ps_c[0][:, 0:128], hidT[:, cc * 128:(cc + 1) * 128], s_w2[which][:],
                                                     start=True, stop=True), reads=[t_hid, t_c], writes=[t_ps_c[0]])
                if which == 0:
                    _rope(P, "dve", ps_c[0][:, 0:128].unsqueeze(1), kc_tm[:].unsqueeze(1), ctab[:, 0, gch, :], ctab[:, 1, gch, :],
                          1, 64, ra, rb_, [t_ps_c[0]], [t_kctm], t_ctab, t_rt)
                    P.op("pe", lambda e: e.transpose(ps_s[0][:, 0:128].bitcast(BF16)[:, 0:128], kc_tm[:], s_id[:]),
                         reads=[t_kctm, t_c], writes=[t_ps_s[0]])
                    P.op("act", lambda e, gch=gch: e.copy(kct[:, gch * 128:(gch + 1) * 128], ps_s[0][:, 0:128].bitcast(BF16)[:, 0:128]),
                         reads=[t_ps_s[0]], writes=[t_kct])
                else:
                    P.op("act", lambda e, gch=gch: e.copy(vca[:, gch, 0:128], ps_c[0][:, 0:128]), reads=[t_ps_c[0]], writes=[t_vca])

    qa = P.sb("qa", [128, 4, 512], BF16); qn = P.sb("qn", [128, 6, 512], BF16); qpe = P.sb("qpe", [64, 6, 512], BF16)
    dq = P.sb("dq", [128, 6, 512], BF16); gts = P.sb("gts", [128, 4, 12], F32); t_q = T()
    obuf = P.sb("obuf", [128, 4, 2048], BF16); t_ob = T()
    kg = [P.sb("kg%d" % i, [128, 2048], BF16) for i in range(2)]
    kpg = [P.sb("kpg%d" % i, [64, 2048], BF16) for i in range(2)]
    vg = [P.sb("vg%d" % i, [128, 16, 129], BF16) for i in range(2)]
    t_kv = [T(), T()]
    kvc = [0]
    kwl = P.sb("kwl", [128, 1024], BF16); vwl = P.sb("vwl", [128, 8, 129], BF16); t_wl = T()
    dkl = [P.sb("dkl%d" % i_, [128, 2560], BF16) for i_ in range(2)]
    dvl = [P.sb("dvl%d" % i_, [128, 20, 129], BF16) for i_ in range(2)]; t_dl = [T(), T()]
    psm = P.sb("psm", [128, 1024], F32); t_psm = T()
    rs = P.sb("rs", [128, 8], F32); rsum = P.sb("rsum", [128, 4], F32); t_rs = T()
    imp = P.sb("imp", [128, 256], F32); imp2 = P.sb("imp2", [128, 256], F32); t_imp = T()
    m8 = P.sb("m8", [128, 16], F32); t_m8 = T()
    selb = P.sb("selb", [128, 256], BF16); t_selb = T()
    dn = P.sb("dn", [128, 4], F32); coef = P.sb("coef", [128, 4], F32); t_dn = T()
    acc = P.sb("acc", [128, 4, 128], F32); t_acc = T()
    tinyv = 1e-30

    def full(n):
        return lambda ps: ps[:, 0:n]

    def full3(ps):
        return ps[:, 0:512].rearrange("p (h q) -> p h q", h=4)

    def cols(c0, n):
        return lambda ps: ps[:, c0:c0 + n]

    def evac_norm(r_or_h, dst):
        x = r_or_h
        P.op("dve", lambda e: e.tensor_scalar(dn[:, x:x + 1], ps_o[x][:, 128:129], tinyv, None, ALU.max),
             reads=[t_ps_o[x]], writes=[t_dn])
        P.op("dve", lambda e: e.reciprocal(dn[:, x:x + 1], dn[:, x:x + 1]), reads=[t_dn], writes=[t_dn])
        P.op("act", lambda e: e.activation(dst, ps_o[x][:, 0:128], AF.Copy, scale=dn[:, x:x + 1]),
             reads=[t_ps_o[x], t_dn], writes=[t_ob])

    for i in range(NI):
        q0 = i * 512
        P.dma("sp", qa[:], QaT[:, :, q0:q0 + 512], writes=[t_q])
        P.dma("sp", qn[:], QnT[:, :, q0:q0 + 512], writes=[t_q])
        P.dma("sp", qpe[:], QpeT[:, :, q0:q0 + 512], writes=[t_q])
        P.dma("sp", dq[:], DqT[:, :, q0:q0 + 512], writes=[t_q])
        P.dma("sp", gts[:], GTd[q0:q0 + 512, :].rearrange("(r p) g -> p r g", p=128), writes=[t_q])
        P.dma("pool", kwl[:], KwL[:, i, :], writes=[t_wl])
        P.dma("pool", vwl[:], VwL[i].rearrange("(c p) x -> p c x", p=128), writes=[t_wl])
        Ci = 128 * (i + 1)
        J = Ci // 4

        for h in range(6):
            for p in range(i + 1):
                b = kvc[0] % 2
                kvc[0] += 1
                g0 = p * 2048
                P.dma("sp", kg[b][:], KnT[:, h, g0:g0 + 2048], writes=[t_kv[b]])
                P.dma("sp", kpg[b][:], KpeT[:, g0:g0 + 2048], writes=[t_kv[b]])
                P.dma("pool", vg[b][:], Vm[g0:g0 + 2048, h, :].rearrange("(c p) x -> p c x", p=128), writes=[t_kv[b]])
                for kc in range(16):
                    ks = slice(kc * 128, (kc + 1) * 128)
                    terms = [(full(512), kg[b][:, ks], qn[:, h, :]), (full(512), kpg[b][:, ks], qpe[:, h, :])]
                    if p == i:
                        for r in range(4):
                            terms.append((cols(r * 128, 128), s_caus[:, r, ks], s_id[:]))
                    k = score(terms, [t_kv[b], t_q, t_c])
                    j = expo(k, 512, SC192)
                    first = (p == 0 and kc == 0)
                    last = (p == i and kc == 15)
                    for r in range(4):
                        P.op("pe", lambda e, r=r, j=j, kc=kc, b=b: e.matmul(ps_o[r][:, 0:129], pt[j][:, r * 128:(r + 1) * 128],
                                                                          vg[b][:, kc, :], start=first, stop=last),
                             reads=[t_pt[j], t_kv[b]], writes=[t_ps_o[r]])
            for r in range(4):
                evac_norm(r, obuf[:, r, 512 + h * 128:512 + (h + 1) * 128])

        for h in range(6):
            db = h % 2
            P.dma("sp", dkl[db][:], DkL[:, i, h, :], writes=[t_dl[db]])
            P.dma("pool", dvl[db][:], DvL[i, :, h, :].rearrange("(c p) x -> p c x", p=128), writes=[t_dl[db]])
            for r in range(4):
                rq = slice(r * 128, (r + 1) * 128)
                x = r
                us = list(range(r, r + 17))
                for b0 in range(0, 17, 4):
                    ub = us[b0:b0 + 4]
                    terms = []
                    for jj, u in enumerate(ub):
                        terms.append((cols(jj * 128, 128), dkl[db][:, u * 128:(u + 1) * 128], dq[:, h, rq]))
                        terms.append((cols(jj * 128, 128), s_dm[:, 16 - (u - r), :], s_id[:]))
                    k = ssc[0] % 2
                    ssc[0] += 1
                    for idx, (of, l, rr) in enumerate(terms):
                        P.op("pe", lambda e, of=of, l=l, rr=rr, idx=idx, k=k: e.matmul(of(ps_s[k]), l, rr, start=(idx % 2 == 0),
                                                                                   stop=(idx % 2 == 1), skip_group_check=True),
                             reads=[t_dl[db], t_q, t_c], writes=[t_ps_s[k]])
                    j = expo(k, 128 * len(ub), SC128)
                    for jj, u in enumerate(ub):
                        P.op("pe", lambda e, jj=jj, u=u, j=j, x=x, db=db: e.matmul(ps_o[x][:, 0:129], pt[j][:, jj * 128:(jj + 1) * 128],
                                                                                dvl[db][:, u, :],
                                                                                start=(u == r), stop=(u == r + 16)),
                             reads=[t_pt[j], t_dl[db]], writes=[t_ps_o[x]])
                evac_norm(x, obuf[:, r, 1280 + h * 128:1280 + (h + 1) * 128])

        for r in range(4):
            rq = slice(r * 128, (r + 1) * 128)
            qa3 = qa[:, :, rq]
            P.op("dve", lambda e: e.memset(rs[:], 0.0), writes=[t_rs])
            nh = (Ci + 511) // 512
            mh, mo = (Ci - 128) // 512, (Ci - 128) % 512
            for h in range(4):
                for hf in range(nh):
                    cw = min(512, Ci - 512 * hf)
                    P.op("pe", lambda e, hf=hf, cw=cw, h=h: e.matmul(ps_c[hf][:, 0:cw], qa[:, h, rq], kct[:, 512 * hf:512 * hf + cw],
                                                                    start=True, stop=(hf != mh), skip_group_check=True),
                         reads=[t_q, t_kct], writes=[t_ps_c[hf]])
                P.op("pe", lambda e, r=r: e.matmul(ps_c[mh][:, mo:mo + 128], s_id[:], s_cmask[:, r, :], start=False, stop=True,
                                                   skip_group_check=True), reads=[t_c], writes=[t_ps_c[mh]])
                for hf in range(nh):
                    cw = min(512, Ci - 512 * hf)
                    P.op("act", lambda e, hf=hf, cw=cw, h=h: e.activation(pexp[:, h, 512 * hf:512 * hf + cw], ps_c[hf][:, 0:cw], AF.Exp,
                                                                         scale=SC128, accum_out=rs[:, 2 * h + hf:2 * h + hf + 1]),
                         reads=[t_ps_c[hf], t_rs], writes=[t_pexp, t_rs])
            P.op("dve", lambda e: e.tensor_reduce(rsum[:], rs[:].rearrange("p (h t) -> p h t", t=2), AX.X, ALU.add),
                 reads=[t_rs], writes=[t_rs])
            P.op("dve", lambda e: e.tensor_scalar(rsum[:], rsum[:], tinyv, None, ALU.max), reads=[t_rs], writes=[t_rs])
            P.op("dve", lambda e: e.reciprocal(rsum[:], rsum[:]), reads=[t_rs], writes=[t_rs])
            P.op("dve", lambda e: e.tensor_scalar(psm[:, 0:Ci], pexp[:, 0, 0:Ci], rsum[:, 0:1], None, ALU.mult),
                 reads=[t_pexp, t_rs], writes=[t_psm])
            for h in range(1, 4):
                P.op("dve", lambda e, h=h: e.scalar_tensor_tensor(psm[:, 0:Ci], pexp[:, h, 0:Ci], rsum[:, h:h + 1], psm[:, 0:Ci],
                                                                  ALU.mult, ALU.add), reads=[t_pexp, t_rs, t_psm], writes=[t_psm])
            pv4 = psm[:, 0:Ci].rearrange("p (j k) -> p j k", k=4)
            P.op("dve", lambda e: e.tensor_reduce(imp[:, 0:J], pv4, AX.X, ALU.add), reads=[t_psm], writes=[t_imp])
            P.op("dve", lambda e: e.tensor_tensor(imp[:, 1:J], imp[:, 1:J], pv4[:, 0:J - 1, 3], ALU.add), reads=[t_psm, t_imp], writes=[t_imp])
            P.op("dve", lambda e: e.tensor_scalar(imp[:, 0:1], imp[:, 0:1], 1e9, None, ALU.add), reads=[t_imp], writes=[t_imp])
            if i >= 1:
                P.op("dve", lambda e, r=r: e.tensor_tensor(imp[:, J - 64:J], imp[:, J - 64:J], s_fmask[:, r, :], ALU.add),
                     reads=[t_imp, t_c], writes=[t_imp])
            else:
                P.op("dve", lambda e, r=r: e.tensor_tensor(imp[:, 0:32], imp[:, 0:32], s_fmask[:, r, 32:64], ALU.add),
                     reads=[t_imp, t_c], writes=[t_imp])
            P.op("dve", lambda e: e.max(m8[:, 0:8], imp[:, 0:J]), reads=[t_imp], writes=[t_m8])
            P.op("dve", lambda e: e.match_replace(imp2[:, 0:J], m8[:, 0:8], imp[:, 0:J], -3e9), reads=[t_imp, t_m8], writes=[t_imp])
            P.op("dve", lambda e: e.max(m8[:, 8:16], imp2[:, 0:J]), reads=[t_imp], writes=[t_m8])
            P.op("dve", lambda e: e.tensor_scalar(selb[:, 0:J], imp[:, 0:J], m8[:, 15:16], NEG, ALU.is_lt, ALU.mult),
                 reads=[t_imp, t_m8], writes=[t_selb])
            P.op("pool", lambda e: e.tensor_copy(selx[:, 0:64 * J].rearrange("p (j k) -> p j k", k=64),
                                                 selb[:, 0:J].unsqueeze(2).broadcast_to([128, J, 64])),
                 reads=[t_selb], writes=[t_selx])
            P.op("pool", lambda e, r=r: e.tensor_tensor(selx[:, 64 * J - 2048:64 * J], selx[:, 64 * J - 2048:64 * J], s_caus[:, r, :], ALU.add),
                 reads=[t_selx, t_c], writes=[t_selx])

            def branch_evac(br, last_branch):
                for h in range(4):
                    P.op("dve", lambda e, h=h: e.tensor_scalar(dn[:, h:h + 1], ps_o[h][:, 128:129], tinyv, None, ALU.max),
                         reads=[t_ps_o[h]], writes=[t_dn])
                P.op("dve", lambda e: e.reciprocal(dn[:], dn[:]), reads=[t_dn], writes=[t_dn])
                gv = gts[:, r, :].rearrange("p (h t) -> p h t", t=3)[:, :, br]
                P.op("dve", lambda e: e.tensor_tensor(coef[:], dn[:], gv, ALU.mult), reads=[t_dn, t_q], writes=[t_dn])
                for h in range(4):
                    dst = obuf[:, r, h * 128:(h + 1) * 128] if last_branch else acc[:, h, :]
                    wr = [t_ob] if last_branch else [t_acc]
                    if br == 0:
                        P.op("dve", lambda e, h=h, dst=dst: e.tensor_scalar(dst, ps_o[h][:, 0:128], coef[:, h:h + 1], None, ALU.mult),
                             reads=[t_ps_o[h], t_dn], writes=wr)
                    else:
                        P.op("dve", lambda e, h=h, dst=dst: e.scalar_tensor_tensor(dst, ps_o[h][:, 0:128], coef[:, h:h + 1], acc[:, h, :],
                                                                                 ALU.mult, ALU.add),
                             reads=[t_ps_o[h], t_dn, t_acc], writes=wr)

            def pv4h(j, vrhs, first, last, rd):
                for h in range(4):
                    P.op("pe", lambda e, h=h: e.matmul(ps_o[h][:, 0:129], pt[j][:, h * 128:(h + 1) * 128], vrhs, start=first, stop=last),
                         reads=[t_pt[j]] + rd, writes=[t_ps_o[h]])

            for cc in range(i + 1):
                terms = [(full3, kct[:, cc * 128:(cc + 1) * 128], qa3)]
                if cc == i:
                    terms.append((full(512), s_cmask[:, r, :], s_I4[:]))
                k = score(terms, [t_kct, t_q, t_c])
                j = expo(k, 512, SC128)
                pv4h(j, vca[:, cc, :], cc == 0, cc == i, [t_vca])
            branch_evac(0, False)
            for p in range(i + 1):
                b = kvc[0] % 2
                kvc[0] += 1
                g0 = p * 2048
                P.dma("sp", kg[b][:], KsT[:, g0:g0 + 2048], writes=[t_kv[b]])
                P.dma("pool", vg[b][:], Vs[g0:g0 + 2048, :].rearrange("(c p) x -> p c x", p=128), writes=[t_kv[b]])
                for kc in range(16):
                    ks = slice(kc * 128, (kc + 1) * 128)
                    terms = [(full3, kg[b][:, ks], qa3), (full(512), selx[:, g0 + kc * 128:g0 + (kc + 1) * 128], s_I4[:])]
                    k = score(terms, [t_kv[b], t_q, t_c, t_selx])
                    j = expo(k, 512, SC128)
                    pv4h(j, vg[b][:, kc, :], p == 0 and kc == 0, p == i and kc == 15, [t_kv[b]])
            branch_evac(1, False)
            for o in range(5):
                u = r + o
                terms = [(full3, kwl[:, u * 128:(u + 1) * 128], qa3)]
                if o in (0, 4):
                    terms.append((full(512), s_wm[:, o, :], s_I4[:]))
                k = score(terms, [t_wl, t_q, t_c])
                j = expo(k, 512, SC128)
                pv4h(j, vwl[:, u, :], o == 0, o == 4, [t_wl])
            branch_evac(2, True)

        for r in range(4):
            P.dma("pool", O[(i * 4 + r) * 128:(i * 4 + r + 1) * 128, :], obuf[:, r, :], reads=[t_ob], is_output=True)
    return P.finish()


def _rep(v):
    return np.ascontiguousarray(np.broadcast_to(np.asarray(v)[None, :], (128, np.asarray(v).size)))


def _inv_freq(dim):
    return (10000.0 ** (-np.arange(0, dim, 2, dtype=np.float32) / dim)).astype(np.float32)


def a_core_masks(c):
    q = np.arange(128)[:, None, None]
    r = np.arange(4)[None, :, None]
    kl = np.arange(2048)[None, None, :]
    caus = np.where(kl <= 512 * c + 128 * r + q, 0.0, NEG).astype(np.float32)
    cl = np.arange(128)[None, None, :]
    cmask = np.where(16 * cl + 31 <= 512 * c + 128 * r + q, 0.0, NEG).astype(np.float32)
    jl = np.arange(-32, 32)[None, None, :]
    cur = 8 * c + 2 * r + (q >= 64)
    fmask = np.where((jl == cur) | (jl == cur - 1), 1e9, np.where(jl > cur, -1e9, 0.0)).astype(np.float32)
    return caus.astype(NPBF), cmask.astype(NPBF), fmask


def a_const_masks():
    q = np.arange(128)[:, None]
    k = np.arange(128)[None, :]
    wm = np.zeros((128, 5, 128), np.float32)
    wm[:, 0, :] = np.where(k >= q, 0.0, NEG)
    wm[:, 4, :] = np.where(k <= q, 0.0, NEG)
    dm = np.zeros((128, 17, 128), np.float32)
    for dc in range(17):
        d = 128 * dc + q - k
        mult = ((d >= 0) & (d <= 128)).astype(np.int64) + ((d >= 0) & (d % 4 == 0) & (d <= 512)) + ((d >= 0) & (d % 16 == 0) & (d <= 2048))
        dm[:, dc, :] = np.where(mult > 0, np.log(np.maximum(mult, 1)) / SC128, NEG)
    ident = np.eye(128, dtype=np.float32)
    I4 = np.tile(ident, (1, 4))
    return wm.astype(NPBF), dm.astype(NPBF), ident.astype(NPBF), I4.astype(NPBF)


def a_inputs(FTb, VTb, GTb, posb, c, cw, S):
    NI = S // 2048
    idx = np.concatenate([np.arange(2048 * i + 512 * c, 2048 * i + 512 * c + 512) for i in range(NI)])
    z = lambda *sh: np.zeros(sh, NPBF)
    m = {}
    m["QaT"] = FTb[:, 0:4][:, :, idx]
    m["QnT"] = FTb[:, 8:14][:, :, idx]
    qpe = np.stack([FTb[(h % 2) * 64:(h % 2) * 64 + 64, 32 + h // 2] for h in range(6)], 1)
    m["QpeT"] = qpe[:, :, idx]
    m["DqT"] = FTb[:, 20:26][:, :, idx]
    m["GT"] = GTb[idx]
    m["KsT"] = FTb[:, 6]; m["Vs"] = VTb[:, 0]; m["KnT"] = FTb[:, 14:20]; m["KpeT"] = FTb[0:64, 35]
    m["Vm"] = VTb[:, 2:8]; m["KcrT"] = FTb[:, 4]; m["VcrT"] = FTb[:, 5]
    KwL = z(128, NI, 1024); VwL = z(NI, 1024, 129); DkL = z(128, NI, 6, 2560); DvL = z(NI, 2560, 6, 129)
    for i in range(NI):
        st = 2048 * i + 512 * c
        lo = max(0, st - 512)
        KwL[:, i, 1024 - (st + 512 - lo):] = FTb[:, 7, lo:st + 512]
        VwL[i, 1024 - (st + 512 - lo):] = VTb[lo:st + 512, 1]
        lo = max(0, st - 2048)
        DkL[:, i, :, 2560 - (st + 512 - lo):] = FTb[:, 26:32, lo:st + 512]
        DvL[i, 2560 - (st + 512 - lo):] = VTb[lo:st + 512, 8:14]
    m["KwL"], m["VwL"], m["DkL"], m["DvL"] = KwL, VwL, DkL, DvL
    m.update(cw)
    NCMP = S // 16
    cend = np.minimum(16 * np.arange(NCMP) + 31, S - 1)
    m["cposT"] = np.ascontiguousarray(posb[cend].reshape(NCMP // 128, 128).T).astype(np.int32)
    m["inv128"] = _rep(_inv_freq(128))
    m["caus"], m["cmask"], m["fmask"] = a_core_masks(c)
    m["wm"], m["dm"], m["ident"], m["I4"] = a_const_masks()
    return {k: np.ascontiguousarray(v) for k, v in m.items()}


def build_p3(NT=4096, ST=2):
    NTL = NT // 128
    NSUP = NTL // ST
    TW = ST * 128
    P = Prog(n_dma_sems=12, dma_queues=("sp", "pool"))
    di = lambda n, sh, dt=BF16: P.dram(n, sh, dt, "ExternalInput")
    h_in = di("h", [NT, D], F32); O_in = di("O", [NT, D]); mem = di("mem", [256, D], F32)
    w_out = di("w_out", [128, 16, 2048]); w_xq = di("w_xq", [128, 16, 512]); w_xkv = di("w_xkv", [128, 16, 1024])
    w_xo = di("w_xo", [128, 4, 2048]); w_up = di("w_up", [128, 16, 8192]); w_down = di("w_down", [128, 64, 2048])
    gains = di("gains", [128, 6, D], F32)
    ident = di("ident", [128, 128])
    hout = P.dram("hout", [NT, D], F32, "ExternalOutput")

    t_c = T()
    s_g = P.sb("s_g", [128, 6, D], F32)
    s_id = P.sb("s_id", [128, 128], BF16)
    P.dma("sp", s_g[:], gains[:], writes=[t_c])
    P.dma("sp", s_id[:], ident[:], writes=[t_c])
    epsb = P.sb("epsb", [128, 1], F32)
    P.op("dve", lambda e: e.memset(epsb[:], EPS), writes=[t_c])
    G_POST, G_MPRE, G_MKV, G_MPOST, G_FPRE, G_FPOST = range(6)

    hres = [P.sb("hres%d" % j, [128, D], F32) for j in range(ST)]; t_hres = [T() for _ in range(ST)]
    ybuf = [P.sb("ybuf%d" % j, [128, D], F32) for j in range(ST)]; t_y = [T() for _ in range(ST)]
    xT = P.sb("xT", [128, 16, TW], BF16); t_xT = T()
    hidT = P.sb("hidT", [128, 64, TW], BF16); t_hid = T()
    wg = [P.sb("wg%d" % i, [128, 16, 512], BF16) for i in range(2)]; t_wg = [T(), T()]
    hn = P.sb("hn", [128, D], BF16); t_hn = T()
    ss = P.sb("ss", [128, 2], F32); t_ss = T()
    rtmp = P.sb("rtmp", [128, TW], F32); t_rt = T()
    kxT = P.sb("kxT", [128, 4, 256], BF16); vx = P.sb("vx", [128, 2, 4, 129], BF16); t_kvx = T()
    qxT = P.sb("qxT", [128, 4, 128], BF16); t_qx = T()
    ptx = P.sb("ptx", [128, 1024], BF16); t_ptx = T()
    ox = P.sb("ox", [128, 512], BF16); t_ox = T()
    dn = P.sb("dn", [128, 4], F32); t_dn = T()
    ps_tr = [P.ps("ps_tr%d" % i, [128, 4, 128], BF16) for i in range(2)]; t_ps_tr = [PT(), PT()]
    ps_mm = [P.ps("ps_mm%d" % i, [128, 512]) for i in range(2)]; t_ps_mm = [PT(), PT()]
    ps_s = [P.ps("ps_sx%d" % i, [128, 512]) for i in range(2)]; t_ps_s = [PT(), PT()]
    ps_o = [P.ps("ps_ox%d" % i, [128, 512]) for i in range(2)]; t_ps_o = [PT(), PT()]
    trc, evc, wgc, mmc = [0], [0], [0], [0]

    def transposes(srcs, dst, rd, wr):
        k = trc[0] % 2
        trc[0] += 1
        for i_, sa in enumerate(srcs):
            P.op("pe", lambda e, i_=i_, sa=sa: e.transpose(ps_tr[k][:, i_, :], sa, s_id[:]), reads=rd + [t_c], writes=[t_ps_tr[k]])
        n = len(srcs)
        if evc[0] % 2 == 0:
            P.op("act", lambda e: e.copy(dst, ps_tr[k][:, 0:n, :]), reads=[t_ps_tr[k]], writes=wr)
        else:
            P.op("dve", lambda e: e.tensor_copy(dst, ps_tr[k][:, 0:n, :]), reads=[t_ps_tr[k]], writes=wr)
        evc[0] += 1

    def loadw(src_ap, kc_n, ncols):
        b = wgc[0] % 2
        wgc[0] += 1
        P.dma("sp" if b == 0 else "pool", wg[b][:, 0:kc_n, 0:ncols], src_ap, writes=[t_wg[b]])
        return b

    def norm_T(src, t_src, gi, j, ncol_tiles=16):
        P.op("act", lambda e: e.activation(hn[:], src, AF.Square, accum_out=ss[:, 0:1]), reads=[t_src], writes=[t_hn, t_ss])
        _rstd(P, ss[:, 0:1], ss[:, 1:2], D, epsb[:], [t_ss, t_c], [t_ss])
        P.op("dve", lambda e: e.scalar_tensor_tensor(hn[:], src, ss[:, 1:2], s_g[:, gi, :], ALU.mult, ALU.mult),
             reads=[t_src, t_ss, t_c], writes=[t_hn])
        for g4 in range(4):
            transposes([hn[:, (4 * g4 + i_) * 128:(4 * g4 + i_ + 1) * 128] for i_ in range(4)],
                       xT[:, 4 * g4:4 * g4 + 4, j * 128:(j + 1) * 128], [t_hn], [t_xT])

    def norm_res(j, gi):
        P.op("act", lambda e: e.activation(hn[:], ybuf[j][:], AF.Square, accum_out=ss[:, 0:1]), reads=[t_y[j]], writes=[t_hn, t_ss])
        _rstd(P, ss[:, 0:1], ss[:, 1:2], D, epsb[:], [t_ss, t_c], [t_ss])
        P.op("dve", lambda e: e.scalar_tensor_tensor(ybuf[j][:], ybuf[j][:], ss[:, 1:2], s_g[:, gi, :], ALU.mult, ALU.mult),
             reads=[t_y[j], t_ss, t_c], writes=[t_y[j]])
        P.op("pool", lambda e: e.tensor_tensor(hres[j][:], hres[j][:], ybuf[j][:], ALU.add), reads=[t_y[j], t_hres[j]], writes=[t_hres[j]])

    def linear_tm(w_src_fn, KC, rd_x, x_fn):
        for n in range(4):
            b = loadw(w_src_fn(n), KC, 512)
            for j in range(ST):
                k = mmc[0] % 2
                mmc[0] += 1
                for kc in range(KC):
                    P.op("pe", lambda e, kc=kc, j=j, k=k, b=b: e.matmul(ps_mm[k][:, :], x_fn(j, kc), wg[b][:, kc, :],
                                                                      start=(kc == 0), stop=(kc == KC - 1)),
                         reads=rd_x + [t_wg[b]], writes=[t_ps_mm[k]])
                P.op("act", lambda e, j=j, k=k, n=n: e.copy(ybuf[j][:, n * 512:(n + 1) * 512], ps_mm[k][:, :]),
                     reads=[t_ps_mm[k]], writes=[t_y[j]])

    for kt in range(2):
        P.dma("sp", hres[0][:], mem[kt * 128:(kt + 1) * 128, :], writes=[t_hres[0]])
        norm_T(hres[0][:], t_hres[0], G_MKV, kt)
    for hh in range(4):
        if hh == 0:
            b = loadw(w_xkv[:, :, 0:512], 16, 512)
        for kc in range(16):
            P.op("pe", lambda e, kc=kc, hh=hh, b=b: e.matmul(ps_mm[0][:, 0:256], wg[b][:, kc, hh * 128:(hh + 1) * 128], xT[:, kc, 0:256],
                                                             start=(kc == 0), stop=(kc == 15)), reads=[t_xT, t_wg[b]], writes=[t_ps_mm[0]])
        P.op("act", lambda e, hh=hh: e.copy(kxT[:, hh, :], ps_mm[0][:, 0:256]), reads=[t_ps_mm[0]], writes=[t_kvx])
    P.op("pool", lambda e: e.memset(vx[:], 1.0), writes=[t_kvx])
    b = loadw(w_xkv[:, :, 512:1024], 16, 512)
    for kt in range(2):
        for kc in range(16):
            P.op("pe", lambda e, kc=kc, kt=kt, b=b: e.matmul(ps_mm[1][:, :], xT[:, kc, kt * 128:(kt + 1) * 128], wg[b][:, kc, :],
                                                             start=(kc == 0), stop=(kc == 15)), reads=[t_xT, t_wg[b]], writes=[t_ps_mm[1]])
        P.op("act", lambda e, kt=kt: e.copy(vx[:, kt, :, 0:128], ps_mm[1][:, :].rearrange("p (h d) -> p h d", d=128)),
             reads=[t_ps_mm[1]], writes=[t_kvx])

    for st in range(NSUP):
        for j in range(ST):
            tl = st * ST + j
            P.dma("sp", hres[j][:], h_in[tl * 128:(tl + 1) * 128, :], writes=[t_hres[j]])
            P.dma("pool", hn[:], O_in[tl * 128:(tl + 1) * 128, :], writes=[t_hn])
            for g4 in range(4):
                transposes([hn[:, (4 * g4 + i_) * 128:(4 * g4 + i_ + 1) * 128] for i_ in range(4)],
                           xT[:, 4 * g4:4 * g4 + 4, j * 128:(j + 1) * 128], [t_hn], [t_xT])
        linear_tm(lambda n: w_out[:, :, n * 512:(n + 1) * 512], 16, [t_xT], lambda j, kc: xT[:, kc, j * 128:(j + 1) * 128])
        for j in range(ST):
            norm_res(j, G_POST)
            norm_T(hres[j][:], t_hres[j], G_MPRE, j)
        bq = loadw(w_xq[:, :, :], 16, 512)
        for j in range(ST):
            for hh in range(4):
                for kc in range(16):
                    P.op("pe", lambda e, kc=kc, hh=hh, j=j: e.matmul(ps_mm[0][:, hh * 128:(hh + 1) * 128], wg[bq][:, kc, hh * 128:(hh + 1) * 128],
                                                                    xT[:, kc, j * 128:(j + 1) * 128], start=(kc == 0), stop=(kc == 15),
                                                                    skip_group_check=True),
                         reads=[t_xT, t_wg[bq]], writes=[t_ps_mm[0]])
            P.op("act", lambda e: e.copy(qxT[:], ps_mm[0][:, :].rearrange("p (h q) -> p h q", h=4)), reads=[t_ps_mm[0]], writes=[t_qx])
            for half in range(2):
                for hi in range(2):
                    hh = 2 * half + hi
                    for kt in range(2):
                        cb_ = (hi * 2 + kt) * 128
                        P.op("pe", lambda e, hh=hh, kt=kt, cb_=cb_, half=half: e.matmul(ps_s[half][:, cb_:cb_ + 128], kxT[:, hh, kt * 128:(kt + 1) * 128],
                                                                                   qxT[:, hh, :], start=True, stop=True, skip_group_check=True),
                             reads=[t_kvx, t_qx], writes=[t_ps_s[half]])
                P.op("act", lambda e, half=half: e.activation(ptx[:, half * 512:(half + 1) * 512], ps_s[half][:, :], AF.Exp, scale=SC128),
                     reads=[t_ps_s[half]], writes=[t_ptx])
                for hi in range(2):
                    hh = 2 * half + hi
                    for kt in range(2):
                        cb_ = half * 512 + (hi * 2 + kt) * 128
                        P.op("pe", lambda e, hh=hh, kt=kt, cb_=cb_, hi=hi, half=half: e.matmul(
                            ps_o[half][:, hi * 129:(hi + 1) * 129], ptx[:, cb_:cb_ + 128], vx[:, kt, hh, :],
                            start=(kt == 0), stop=(kt == 1), skip_group_check=True), reads=[t_ptx, t_kvx], writes=[t_ps_o[half]])
                for hi in range(2):
                    hh = 2 * half + hi
                    P.op("dve", lambda e, hh=hh, hi=hi, half=half: e.reciprocal(dn[:, hh:hh + 1], ps_o[half][:, hi * 129 + 128:hi * 129 + 129]),
                         reads=[t_ps_o[half]], writes=[t_dn])
                    P.op("act", lambda e, hh=hh, hi=hi, half=half: e.activation(ox[:, hh * 128:(hh + 1) * 128], ps_o[half][:, hi * 129:hi * 129 + 128],
                                                                               AF.Copy, scale=dn[:, hh:hh + 1]),
                         reads=[t_ps_o[half], t_dn], writes=[t_ox])
            transposes([ox[:, i_ * 128:(i_ + 1) * 128] for i_ in range(4)], hidT[:, 0:4, j * 128:(j + 1) * 128], [t_ox], [t_hid])
        linear_tm(lambda n: w_xo[:, :, n * 512:(n + 1) * 512], 4, [t_hid], lambda j, kc: hidT[:, kc, j * 128:(j + 1) * 128])
        for j in range(ST):
            norm_res(j, G_MPOST)
            norm_T(hres[j][:], t_hres[j], G_FPRE, j)
        for m in range(16):
            b = loadw(w_up[:, :, m * 512:(m + 1) * 512], 16, 512)
            for hc in range(4):
                k = mmc[0] % 2
                mmc[0] += 1
                for kc in range(16):
                    P.op("pe", lambda e, kc=kc, hc=hc, k=k, b=b: e.matmul(ps_mm[k][:, 0:TW], wg[b][:, kc, hc * 128:(hc + 1) * 128], xT[:, kc, :],
                                                                        start=(kc == 0), stop=(kc == 15)),
                         reads=[t_xT, t_wg[b]], writes=[t_ps_mm[k]])
                P.op("act", lambda e, k=k: e.activation(rtmp[:], ps_mm[k][:, 0:TW], AF.Relu), reads=[t_ps_mm[k]], writes=[t_rt])
                P.op("dve", lambda e, m=m, hc=hc: e.tensor_tensor(hidT[:, m * 4 + hc, :], rtmp[:], rtmp[:], ALU.mult), reads=[t_rt], writes=[t_hid])
        for n in range(4):
            for kg in range(4):
                b = loadw(w_down[:, kg * 16:(kg + 1) * 16, n * 512:(n + 1) * 512], 16, 512)
                for j in range(ST):
                    for kc in range(16):
                        P.op("pe", lambda e, kc=kc, kg=kg, j=j, b=b: e.matmul(ps_mm[j][:, :], hidT[:, kg * 16 + kc, j * 128:(j + 1) * 128], wg[b][:, kc, :],
                                                                            start=(kg == 0 and kc == 0), stop=(kg == 3 and kc == 15),
                                                                            skip_group_check=True),
                             reads=[t_hid, t_wg[b]], writes=[t_ps_mm[j]])
            for j in range(ST):
                P.op("act", lambda e, j=j, n=n: e.copy(ybuf[j][:, n * 512:(n + 1) * 512], ps_mm[j][:, :]), reads=[t_ps_mm[j]], writes=[t_y[j]])
        for j in range(ST):
            tl = st * ST + j
            norm_res(j, G_FPOST)
            P.dma("pool", hout[tl * 128:(tl + 1) * 128, :], hres[j][:], reads=[t_hres[j]], is_output=True)
    return P.finish()


def _klay(w):
    k, n = w.shape
    return np.ascontiguousarray(w.reshape(k // 128, 128, n).transpose(1, 0, 2))


_PROGS = {}


def _prog(name, fn):
    if name not in _PROGS:
        _PROGS[name] = fn()
    return _PROGS[name]


def kernel(x, mem, positions, g_mix_pre, w_in, cmp_pos_emb, w_cmp_k1, w_cmp_k2, w_cmp_v1, w_cmp_v2,
           g_q_lora, g_kv_lora, w_uq, w_ukv, w_out, g_mix_post, g_mem_pre, g_mem_kv, w_xq, w_xkv, w_xo,
           g_mem_post, g_mlp_pre, w_up, w_down, g_mlp_post):
    f32 = lambda a: np.asarray(a, dtype=np.float32)
    x = f32(x); mem = f32(mem); positions = np.asarray(positions).astype(np.int32)
    DEPTH = int(np.asarray(w_in).shape[0])
    names, arrs = [], []
    for l in range(DEPTH):
        lay = {
            "w_in": _klay(f32(w_in[l])[:, W_IN_PERM]), "w_uq": _klay(f32(w_uq[l])), "w_ukv": _klay(f32(w_ukv[l])),
            "w_ck1": np.ascontiguousarray(f32(w_cmp_k1[l]).reshape(32, 128, 128).transpose(1, 0, 2)), "w_ck2": f32(w_cmp_k2[l]),
            "w_cv1": np.ascontiguousarray(f32(w_cmp_v1[l]).reshape(32, 128, 128).transpose(1, 0, 2)), "w_cv2": f32(w_cmp_v2[l]),
            "pembT": np.ascontiguousarray(f32(cmp_pos_emb[l]).T),
            "w_out": _klay(f32(w_out[l])), "w_xq": _klay(f32(w_xq[l])), "w_xkv": _klay(f32(w_xkv[l])), "w_xo": _klay(f32(w_xo[l])),
            "w_up": _klay(f32(w_up[l])), "w_down": _klay(f32(w_down[l])),
        }
        for k_, v_ in lay.items():
            names.append((l, k_, v_.shape))
            arrs.append(v_.reshape(-1))
    flat = np.concatenate(arrs)
    del arrs
    flat_bf = cast_on_device(flat)
    del flat
    W = [dict() for _ in range(DEPTH)]
    off = 0
    for l, k_, shp in names:
        n = int(np.prod(shp))
        W[l][k_] = flat_bf[off:off + n].reshape(shp)
        off += n

    ident = np.eye(128, dtype=np.float32).astype(NPBF)
    inv128, inv64 = _rep(_inv_freq(128)), _rep(_inv_freq(64))
    NTC = S // 4
    hcur = x.copy()
    nc_p1 = _prog("p1", lambda: build_p1(NT=NTC))
    nc_a = _prog("a", lambda: build_A(S=S))
    nc_p3 = _prog("p3", lambda: build_p3(NT=NTC))
    cores = list(range(NCORES))
    for l in range(DEPTH):
        ims = []
        for c8 in cores:
            b, c = c8 // 4, c8 % 4
            sl = slice(c * NTC, (c + 1) * NTC)
            ims.append({"h": np.ascontiguousarray(hcur[b, sl]),
                        "posT": np.ascontiguousarray(positions[b, sl].reshape(NTC // 128, 128).T),
                        "inv128": inv128, "inv64": inv64, "g_pre": _rep(f32(g_mix_pre[l])), "w_in": W[l]["w_in"],
                        "g_q": _rep(f32(g_q_lora[l])), "g_kv": _rep(f32(g_kv_lora[l])), "w_uq": W[l]["w_uq"], "w_ukv": W[l]["w_ukv"],
                        "ident": ident})
        r1 = run_bass_kernel_spmd(nc_p1, ims, core_ids=cores).results
        del ims
        ims = []
        cw = {k_: W[l][k_] for k_ in ("w_ck1", "w_ck2", "w_cv1", "w_cv2", "pembT")}
        for b in range(B):
            FTb = np.concatenate([np.asarray(r1[4 * b + c]["FT"]) for c in range(4)], axis=2)
            VTb = np.concatenate([np.asarray(r1[4 * b + c]["VT"]) for c in range(4)], axis=0)
            GTb = np.concatenate([np.asarray(r1[4 * b + c]["GT"]) for c in range(4)], axis=0)
            for c in range(4):
                ims.append(a_inputs(FTb, VTb, GTb, positions[b], c, cw, S))
        del r1
        ra = run_bass_kernel_spmd(nc_a, ims, core_ids=cores).results
        del ims
        Ob = np.zeros((B, S, D), NPBF)
        for c8 in cores:
            b, c = c8 // 4, c8 % 4
            idx = np.concatenate([np.arange(2048 * i + 512 * c, 2048 * i + 512 * c + 512) for i in range(S // 2048)])
            Ob[b, idx] = np.asarray(ra[c8]["O"])
        del ra
        gl = [g_mix_post, g_mem_pre, g_mem_kv, g_mem_post, g_mlp_pre, g_mlp_post]
        gains = np.ascontiguousarray(np.stack([_rep(f32(g_[l])) for g_ in gl], 1))
        ims = []
        for c8 in cores:
            b, c = c8 // 4, c8 % 4
            sl = slice(c * NTC, (c + 1) * NTC)
            ims.append({"h": np.ascontiguousarray(hcur[b, sl]), "O": np.ascontiguousarray(Ob[b, sl]), "mem": np.ascontiguousarray(mem[b]),
                        "w_out": W[l]["w_out"], "w_xq": W[l]["w_xq"], "w_xkv": W[l]["w_xkv"], "w_xo": W[l]["w_xo"],
                        "w_up": W[l]["w_up"], "w_down": W[l]["w_down"], "gains": gains, "ident": ident})
        r3 = run_bass_kernel_spmd(nc_p3, ims, core_ids=cores).results
        del ims
        for c8 in cores:
            b, c = c8 // 4, c8 % 4
            hcur[b, c * NTC:(c + 1) * NTC] = np.asarray(r3[c8]["hout"])
        del r3
    return hcur
```

```python
import numpy as np
import ml_dtypes
from contextlib import ExitStack
import concourse.bass as bass
import concourse.mybir as mybir
from concourse.bass_utils import run_bass_kernel_spmd

F32 = mybir.dt.float32
BF16 = mybir.dt.bfloat16
I32 = mybir.dt.int32
ALU = mybir.AluOpType
AF = mybir.ActivationFunctionType
AX = mybir.AxisListType
NPBF = ml_dtypes.bfloat16

NCORES = 8


class T:
    __slots__ = ("name", "w", "rd", "excl")

    def __init__(self, name="", excl=False):
        self.name = name
        self.w = None
        self.rd = {}
        self.excl = excl


def PT():
    return T(excl=True)


class _Rec:
    def __getattr__(self, name):
        def f(*a, **kw):
            self.call = (name, a, kw)
            return self
        return f


class _Eng:
    def __init__(self, name):
        self.name = name
        self.q = []
        self.sem = None
        self.count = 0
        self.known = {}
        self.dsems = []
        self.drr = 0


class Prog:
    def __init__(self, n_dma_sems=20, dma_queues=("sp", "pool", "act")):
        self.nc = bass.Bass("TRN2", target_bir_lowering=False)
        self.es = ExitStack()
        self.sems = []
        self.semval = []
        self.E = {n: _Eng(n) for n in ("pe", "act", "dve", "pool", "sp")}
        for n in ("pe", "act", "dve", "pool"):
            self.E[n].sem = self._newsem("p_" + n)
        for qn in dma_queues:
            for i in range(n_dma_sems):
                self.E[qn].dsems.append(self._newsem("d_%s%d" % (qn, i)))
        self.out_tokens = []
        self.n_inst = 0

    def _newsem(self, name):
        h = self.es.enter_context(self.nc.semaphore(name))
        self.sems.append(h)
        self.semval.append(0)
        return len(self.sems) - 1

    def dram(self, name, shape, dt, kind):
        return self.nc.dram_tensor(name, list(shape), dt, kind=kind).ap()

    def sb(self, name, shape, dt):
        return self.es.enter_context(self.nc.sbuf_tensor(name, list(shape), dt))

    def ps(self, name, shape, dt=F32):
        return self.es.enter_context(self.nc.psum_tensor(name, list(shape), dt))

    def _deps(self, eng, reads, writes):
        need = {}

        def add(tok):
            if tok is None:
                return
            s, v = tok
            if need.get(s, 0) < v:
                need[s] = v

        for t in reads:
            add(t.w)
        for t in writes:
            add(t.w)
            for s, v in t.rd.items():
                add((s, v))
        out = []
        for s, v in need.items():
            if s == eng.sem and eng.name == "pe":
                continue
            if eng.known.get(s, 0) >= v:
                continue
            eng.known[s] = v
            out.append((s, v))
        return out

    def _emit_waits(self, eng, waits):
        for s, v in waits:
            h = self.sems[s]
            eng.q.append(lambda e, h=h, v=v: e.wait_ge(h, v))

    def op(self, en, fn, reads=(), writes=()):
        eng = self.E[en]
        ex = [t for t in reads if t.excl]
        if ex:
            reads = [t for t in reads if not t.excl]
            writes = list(writes) + ex
        waits = self._deps(eng, reads, writes)
        self._emit_waits(eng, waits)
        eng.count += 1
        c = eng.count
        h = self.sems[eng.sem]
        rec = _Rec()
        fn(rec)
        name, a, kw = rec.call
        eng.q.append(lambda e, name=name, a=a, kw=kw, h=h: getattr(e, name)(*a, **kw).then_inc(h, 1))
        tok = (eng.sem, c)
        for t in reads:
            if t.rd.get(eng.sem, 0) < c:
                t.rd[eng.sem] = c
        for t in writes:
            t.w = tok
            t.rd = {}
        self.n_inst += 1
        return tok

    def dma(self, qn, out, in_, reads=(), writes=(), is_output=False):
        eng = self.E[qn]
        k = eng.drr
        eng.drr = (k + 1) % len(eng.dsems)
        s = eng.dsems[k]
        waits = self._deps(eng, reads, writes)
        prev = self.semval[s]
        if prev > 0 and eng.known.get(s, 0) < prev:
            eng.known[s] = prev
            waits.append((s, prev))
        self._emit_waits(eng, waits)
        self.semval[s] = prev + 16
        v = prev + 16
        h = self.sems[s]
        eng.q.append(lambda e, out=out, in_=in_, h=h: e.dma_start(out=out, in_=in_).then_inc(h, 16))
        tok = (s, v)
        for t in reads:
            if t.rd.get(s, 0) < v:
                t.rd[s] = v
        for t in writes:
            t.w = tok
            t.rd = {}
        if is_output:
            self.out_tokens.append(tok)
        self.n_inst += 1
        return tok

    def finish(self):
        sp = self.E["sp"]
        fin = {}
        for s, v in self.out_tokens:
            fin[s] = max(fin.get(s, 0), v)
        for s in range(len(self.sems)):
            if self.semval[s] > 0:
                fin[s] = max(fin.get(s, 0), self.semval[s])
        for n in ("pe", "act", "dve", "pool"):
            e = self.E[n]
            if e.count:
                fin[e.sem] = e.count
        for s, v in fin.items():
            h = self.sems[s]
            sp.q.append(lambda e, h=h, v=v: e.wait_ge(h, v))
        with self.nc.Block() as block:
            @block.sync
            def _(e):
                for f in self.E["sp"].q:
                    f(e)

            @block.tensor
            def _(e):
                for f in self.E["pe"].q:
                    f(e)

            @block.scalar
            def _(e):
                for f in self.E["act"].q:
                    f(e)

            @block.vector
            def _(e):
                for f in self.E["dve"].q:
                    f(e)

            @block.gpsimd
            def _(e):
                for f in self.E["pool"].q:
                    f(e)
        self.es.close()
        return self.nc


def build_cast(F, CH=4096):
    P = Prog()
    x = P.dram("x", [128, F], F32, "ExternalInput")
    y = P.dram("y", [128, F], BF16, "ExternalOutput")
    NB = 3
    xin = [P.sb("xin%d" % i, [128, CH], F32) for i in range(NB)]
    xo = [P.sb("xo%d" % i, [128, CH], BF16) for i in range(NB)]
    tin = [T() for _ in range(NB)]
    to = [T() for _ in range(NB)]
    engs = ["dve", "pool", "act"]
    nch = (F + CH - 1) // CH
    for c in range(nch):
        b = c % NB
        w = min(CH, F - c * CH)
        P.dma("sp", xin[b][:, :w], x[:, c * CH:c * CH + w], writes=[tin[b]])
        en = engs[c % 3]
        if en == "act":
            P.op(en, lambda e, b=b, w=w: e.copy(xo[b][:, :w], xin[b][:, :w]), reads=[tin[b]], writes=[to[b]])
        else:
            P.op(en, lambda e, b=b, w=w: e.tensor_copy(xo[b][:, :w], xin[b][:, :w]), reads=[tin[b]], writes=[to[b]])
        P.dma("pool", y[:, c * CH:c * CH + w], xo[b][:, :w], reads=[to[b]], is_output=True)
    return P.finish()


def cast_on_device(flat):
    n = flat.size
    per = -(-n // (NCORES * 128))
    per = -(-per // 8) * 8
    pad = np.zeros(NCORES * 128 * per, np.float32)
    pad[:n] = flat
    shards = pad.reshape(NCORES, 128, per)
    nc = build_cast(per)
    res = run_bass_kernel_spmd(nc, [{"x": shards[i]} for i in range(NCORES)], core_ids=list(range(NCORES)))
    out = np.concatenate([np.asarray(r["y"]).reshape(-1) for r in res.results])
    return out[:n]


D = 2048
S = 16384
B = 2
DIN = 4684
EPS = 1e-6
TWO_PI = 6.283185307179586
CW1 = 6.28125
CW2 = TWO_PI - CW1
W_IN_PERM = np.concatenate([np.arange(0, 1280), np.arange(1292, 2316), np.arange(2380, 4684),
                            np.arange(2316, 2380), np.arange(1280, 1292)])
C_NQ, C_KC, C_VC, C_KS, C_VS, C_KW, C_VW = 0, 512, 640, 768, 896, 1024, 1152
C_CQ, C_CKV, C_DQ, C_DK, C_DV, C_KR, C_G = 1280, 1792, 2304, 3072, 3840, 4608, 4672
NFB = 36
NVB = 14


def _sincos(P, ang, sin_out, cos_out, y, ki, kf, r1, t_ang, t_out, t_scr):
    import math
    for which, dst in ((0, sin_out), (1, cos_out)):
        if which == 1:
            P.op("dve", lambda e: e.tensor_scalar(r1, ang, math.pi / 2, None, ALU.add), reads=[t_ang], writes=[t_scr])
            src = r1
        else:
            src = ang
        P.op("dve", lambda e, src=src: e.tensor_scalar(y, src, 1.0 / TWO_PI, None, ALU.mult), reads=[t_ang, t_scr], writes=[t_scr])
        P.op("dve", lambda e: e.tensor_copy(ki, y), reads=[t_scr], writes=[t_scr])
        P.op("dve", lambda e: e.tensor_copy(kf, ki), reads=[t_scr], writes=[t_scr])
        P.op("dve", lambda e, src=src: e.scalar_tensor_tensor(y, kf, -CW1, src, ALU.mult, ALU.add), reads=[t_scr, t_ang], writes=[t_scr])
        P.op("dve", lambda e: e.scalar_tensor_tensor(y, kf, -CW2, y, ALU.mult, ALU.add), reads=[t_scr], writes=[t_scr])
        P.op("dve", lambda e: e.tensor_scalar(y, y, -math.pi, math.pi, ALU.max, ALU.min), reads=[t_scr], writes=[t_scr])
        P.op("act", lambda e, dst=dst: e.activation(dst, y, AF.Sin), reads=[t_scr], writes=[t_out])


def _rope(P, en, src, dst, cos, sin, H, hd, tmp_a, tmp_b, rd, wr, t_tab, t_tmp):
    c = cos.unsqueeze(1).broadcast_to([128, H, hd])
    s = sin.unsqueeze(1).broadcast_to([128, H, hd])
    x1, x2 = src[:, :, 0:hd], src[:, :, hd:2 * hd]
    ta, tb = tmp_a[:, 0:H, 0:hd], tmp_b[:, 0:H, 0:hd]
    P.op(en, lambda e: e.tensor_tensor(ta, x1, c, ALU.mult), reads=rd + [t_tab], writes=[t_tmp])
    P.op(en, lambda e: e.tensor_tensor(tb, x2, s, ALU.mult), reads=rd + [t_tab], writes=[t_tmp])
    P.op(en, lambda e: e.tensor_tensor(dst[:, :, 0:hd], ta, tb, ALU.subtract), reads=[t_tmp], writes=wr)
    P.op(en, lambda e: e.tensor_tensor(ta, x2, c, ALU.mult), reads=rd + [t_tab], writes=[t_tmp])
    P.op(en, lambda e: e.tensor_tensor(tb, x1, s, ALU.mult), reads=rd + [t_tab], writes=[t_tmp])
    P.op(en, lambda e: e.tensor_tensor(dst[:, :, hd:2 * hd], ta, tb, ALU.add), reads=[t_tmp], writes=wr)


def _rstd(P, ss, rstd, n, epsb, rd, wr):
    P.op("act", lambda e: e.activation(rstd, ss, AF.Sqrt, bias=epsb, scale=1.0 / n), reads=rd, writes=wr)
    P.op("dve", lambda e: e.reciprocal(rstd, rstd), reads=wr, writes=wr)


def build_p1(NT=4096, ST=2):
    NTL = NT // 128
    NSUP = NTL // ST
    P = Prog()
    h = P.dram("h", [NT, D], F32, "ExternalInput")
    posT = P.dram("posT", [128, NTL], I32, "ExternalInput")
    inv128 = P.dram("inv128", [128, 64], F32, "ExternalInput")
    inv64 = P.dram("inv64", [128, 32], F32, "ExternalInput")
    g_pre = P.dram("g_pre", [128, D], F32, "ExternalInput")
    w_in = P.dram("w_in", [128, 16, DIN], BF16, "ExternalInput")
    g_q = P.dram("g_q", [128, 512], F32, "ExternalInput")
    g_kv = P.dram("g_kv", [128, 512], F32, "ExternalInput")
    w_uq = P.dram("w_uq", [128, 4, 1152], BF16, "ExternalInput")
    w_ukv = P.dram("w_ukv", [128, 4, 1536], BF16, "ExternalInput")
    ident = P.dram("ident", [128, 128], BF16, "ExternalInput")
    FT = P.dram("FT", [128, NFB, NT], BF16, "ExternalOutput")
    VT = P.dram("VT", [NT, NVB, 129], BF16, "ExternalOutput")
    GT = P.dram("GT", [NT, 12], F32, "ExternalOutput")

    s_g = P.sb("s_g", [128, D], F32); t_g = T()
    s_gq = P.sb("s_gq", [128, 512], F32)
    s_gkv = P.sb("s_gkv", [128, 512], F32)
    s_wuq = P.sb("s_wuq", [128, 4, 1152], BF16)
    s_wukv = P.sb("s_wukv", [128, 4, 1536], BF16)
    s_id = P.sb("s_id", [128, 128], BF16)
    s_pos = P.sb("s_pos", [128, NTL], I32)
    s_posf = P.sb("s_posf", [128, NTL], F32)
    s_inv128 = P.sb("s_inv128", [128, 64], F32)
    s_inv64 = P.sb("s_inv64", [128, 32], F32)
    epsb = P.sb("epsb", [128, 1], F32)
    t_c = T()
    for dst, src in ((s_g, g_pre), (s_gq, g_q), (s_gkv, g_kv), (s_wuq, w_uq), (s_wukv, w_ukv), (s_id, ident),
                     (s_pos, posT), (s_inv128, inv128), (s_inv64, inv64)):
        P.dma("sp", dst[:], src[:], writes=[t_c])
    P.op("dve", lambda e: e.memset(epsb[:], EPS), writes=[t_c])
    P.op("dve", lambda e: e.tensor_copy(s_posf[:], s_pos[:]), reads=[t_c], writes=[t_c])

    cos128 = P.sb("cos128", [128, NTL, 64], F32)
    sin128 = P.sb("sin128", [128, NTL, 64], F32)
    cos64 = P.sb("cos64", [128, NTL, 32], F32)
    sin64 = P.sb("sin64", [128, NTL, 32], F32)
    t_tab = T()
    pj = [P.sb("pj%d" % j, [128, DIN], F32) for j in range(ST)]
    t_pj = [T() for _ in range(ST)]
    s_ki = P.sb("s_ki", [128, NTL * 64], I32)
    t_ang, t_scr = T(), T()
    for hd, inv, co, si in ((64, s_inv128, cos128, sin128), (32, s_inv64, cos64, sin64)):
        n = NTL * hd
        ang = pj[0][:, 0:n].rearrange("p (t d) -> p t d", d=hd)
        yv = pj[0][:, 2048:2048 + n].rearrange("p (t d) -> p t d", d=hd)
        kfv = pj[1][:, 0:n].rearrange("p (t d) -> p t d", d=hd)
        r1v = pj[1][:, 2048:2048 + n].rearrange("p (t d) -> p t d", d=hd)
        kiv = s_ki[:, 0:n].rearrange("p (t d) -> p t d", d=hd)
        pb = s_posf[:].unsqueeze(2).broadcast_to([128, NTL, hd])
        ib = inv[:].unsqueeze(1).broadcast_to([128, NTL, hd])
        P.op("dve", lambda e, ang=ang, pb=pb, ib=ib: e.tensor_tensor(ang, pb, ib, ALU.mult), reads=[t_c, t_scr], writes=[t_ang])
        _sincos(P, ang, si[:], co[:], yv, kiv, kfv, r1v, t_ang, t_tab, t_scr)
    for j in range(ST):
        t_pj[j] = T()
        t_pj[j].rd = dict(t_tab.rd)
    t_pj[0].w = t_scr.w; t_pj[0].rd = dict(t_scr.rd); t_pj[0].rd.update(t_ang.rd)
    if ST > 1:
        t_pj[1].w = t_scr.w; t_pj[1].rd = dict(t_scr.rd)
    if t_ang.w is not None:
        s_, v_ = t_ang.w
        t_pj[0].rd[s_] = max(t_pj[0].rd.get(s_, 0), v_)

    hin = [P.sb("hin%d" % i, [128, D], F32) for i in range(2)]
    t_hin = [T(), T()]
    hn = P.sb("hn", [128, D], BF16); t_hn = T()
    ss = P.sb("ss", [128, 4], F32); t_ss = T()
    hnT = [P.sb("hnT%d" % j, [128, 16, 128], BF16) for j in range(ST)]
    t_hnT = [T() for _ in range(ST)]
    wg = [P.sb("wg%d" % i, [128, 16, 512], BF16) for i in range(2)]
    t_wg = [T(), T()]
    rb = P.sb("rb", [128, NFB, 128], BF16)
    t_rb = [T() for _ in range(9)]
    tsb = P.sb("tsb", [128, NFB, ST * 128], BF16); t_tsb = T()
    vt = P.sb("vt", [128, NVB, 129], BF16); t_vt = T()
    gsb = P.sb("gsb", [128, 12], F32); t_gsb = T()
    cqn = P.sb("cqn", [128, 2, 512], BF16); t_cqn = T()
    cT = P.sb("cT", [128, 2, 4, 128], BF16); t_cT = T()
    tmp_a = P.sb("tmp_a", [128, 12, 64], F32)
    tmp_b = P.sb("tmp_b", [128, 12, 64], F32)
    t_tmp = T()
    tmp_c = P.sb("tmp_c", [128, 1, 64], F32)
    tmp_d = P.sb("tmp_d", [128, 1, 64], F32)
    t_tmp2 = T()
    ps_tr = [P.ps("ps_tr%d" % i, [128, 4, 128], BF16) for i in range(2)]
    t_ps_tr = [PT(), PT()]
    ps_mm = [P.ps("ps_mm%d" % i, [128, 512], F32) for i in range(2)]
    t_ps_mm = [PT(), PT()]
    ps_ml = [P.ps("ps_ml%d" % i, [128, 512], F32) for i in range(2)]
    t_ps_ml = [PT(), PT()]

    P.op("pool", lambda e: e.memset(vt[:], 1.0), writes=[t_vt])
    P.op("pool", lambda e: e.memset(rb[:], 0.0), writes=t_rb)

    ngrp = (DIN + 511) // 512
    trc = [0]
    mmc = [0]
    mlc = [0]
    evc = [0]

    def transposes(src_blocks, dst_fn, rd, wr):
        k = trc[0] % 2
        trc[0] += 1
        for i, sa in enumerate(src_blocks):
            w = sa.shape[1]
            P.op("pe", lambda e, i=i, sa=sa, w=w, k=k: e.transpose(ps_tr[k][0:w, i, :], sa, s_id[:]),
                 reads=rd + [t_c], writes=[t_ps_tr[k]])
        en = "act" if evc[0] % 2 == 0 else "dve"
        evc[0] += 1
        dst, src = dst_fn(ps_tr[k])
        if en == "act":
            P.op("act", lambda e: e.copy(dst, src), reads=[t_ps_tr[k]], writes=wr)
        else:
            P.op("dve", lambda e: e.tensor_copy(dst, src), reads=[t_ps_tr[k]], writes=wr)

    import os
    LEVEL = float(os.environ.get("P1_LEVEL", "9"))
    PEN = "dve" if os.environ.get("P1_NOPOOL") else "pool"
    for st in range(NSUP if LEVEL >= 2 else 0):
        for j in range(ST):
            tl = st * ST + j
            b = tl % 2
            P.dma("sp", hin[b][:], h[tl * 128:(tl + 1) * 128, :], writes=[t_hin[b]])
            P.op("act", lambda e, b=b: e.activation(hn[:], hin[b][:], AF.Square, accum_out=ss[:, 0:1]),
                 reads=[t_hin[b]], writes=[t_hn, t_ss])
            _rstd(P, ss[:, 0:1], ss[:, 1:2], D, epsb[:], [t_ss, t_c], [t_ss])
            P.op("dve", lambda e, b=b: e.scalar_tensor_tensor(hn[:], hin[b][:], ss[:, 1:2], s_g[:], ALU.mult, ALU.mult),
                 reads=[t_hin[b], t_ss, t_c], writes=[t_hn])
            for g4 in range(4):
                transposes([hn[:, (4 * g4 + i) * 128:(4 * g4 + i + 1) * 128] for i in range(4)],
                           lambda pt, j=j, g4=g4: (hnT[j][:, 4 * g4:4 * g4 + 4, :], pt[:]),
                           [t_hn], [t_hnT[j]])
        for g in range(ngrp if LEVEL >= 3 else 0):
            n0 = g * 512
            nw = min(512, DIN - n0)
            wb = (st * ngrp + g) % 2
            P.dma("pool", wg[wb][:, :, 0:nw], w_in[:, :, n0:n0 + nw], writes=[t_wg[wb]])
            for j in range(ST):
                k = mmc[0] % 2
                mmc[0] += 1
                for kc in range(16):
                    P.op("pe", lambda e, k=k, j=j, kc=kc, wb=wb, nw=nw: e.matmul(
                        ps_mm[k][:, 0:nw], hnT[j][:, kc, :], wg[wb][:, kc, 0:nw], start=(kc == 0), stop=(kc == 15)),
                        reads=[t_hnT[j], t_wg[wb]], writes=[t_ps_mm[k]])
                P.op("act", lambda e, k=k, j=j, n0=n0, nw=nw: e.copy(pj[j][:, n0:n0 + nw], ps_mm[k][:, 0:nw]),
                     reads=[t_ps_mm[k]], writes=[t_pj[j]])
        for j in range(ST if LEVEL >= 4 else 0):
            tl = st * ST + j
            pjt = pj[j]
            c128, s128 = cos128[:, tl, :], sin128[:, tl, :]
            c64, s64 = cos64[:, tl, :], sin64[:, tl, :]
            rd = [t_pj[j]]

            def blk(b0, nb):
                return rb[:, b0:b0 + nb, :]

            def colv(c0, nb):
                return pjt[:, c0:c0 + nb * 128].rearrange("p (h d) -> p h d", d=128)
            _rope(P, "dve", colv(C_NQ, 4), blk(0, 4), c128, s128, 4, 64, tmp_a, tmp_b, rd, [t_rb[0]], t_tab, t_tmp)
            _rope(P, PEN, colv(C_KS, 1), blk(6, 1), c128, s128, 1, 64, tmp_c, tmp_d, rd, [t_rb[1]], t_tab, t_tmp2)
            _rope(P, PEN, colv(C_KW, 1), blk(7, 1), c128, s128, 1, 64, tmp_c, tmp_d, rd, [t_rb[1]], t_tab, t_tmp2)
            _rope(P, "dve", colv(C_DQ, 12), blk(20, 12), c128, s128, 12, 64, tmp_a, tmp_b, rd, [t_rb[5], t_rb[6], t_rb[7]], t_tab, t_tmp)
            if LEVEL < 4.2:
                continue
            P.op(PEN, lambda e: e.tensor_copy(blk(4, 2), colv(C_KC, 2)), reads=rd, writes=[t_rb[1]])
            _rope(P, PEN, pjt[:, C_KR:C_KR + 64].unsqueeze(1), rb[:, 35, 0:64].unsqueeze(1), c64, s64, 1, 32,
                  tmp_c, tmp_d, rd, [t_rb[8]], t_tab, t_tmp2)
            P.op(PEN, lambda e: e.tensor_copy(vt[:, 0, 0:128], pjt[:, C_VS:C_VS + 128]), reads=rd, writes=[t_vt])
            P.op(PEN, lambda e: e.tensor_copy(vt[:, 1, 0:128], pjt[:, C_VW:C_VW + 128]), reads=rd, writes=[t_vt])
            P.op(PEN, lambda e: e.tensor_copy(vt[:, 8:14, 0:128], colv(C_DV, 6)), reads=rd, writes=[t_vt])
            P.op("act", lambda e: e.activation(gsb[:], pjt[:, C_G:C_G + 12], AF.Sigmoid), reads=rd, writes=[t_gsb])
            P.dma("pool", GT[tl * 128:(tl + 1) * 128, :], gsb[:], reads=[t_gsb], is_output=True)
            if LEVEL < 4.3:
                continue
            for which, c0, gsbuf in ((0, C_CQ, s_gq), (1, C_CKV, s_gkv)):
                P.op("act", lambda e, which=which, c0=c0: e.activation(cqn[:, which, :], pjt[:, c0:c0 + 512], AF.Square,
                                                                     accum_out=ss[:, 2:3]),
                     reads=rd, writes=[t_cqn, t_ss])
                _rstd(P, ss[:, 2:3], ss[:, 3:4], 512, epsb[:], [t_ss, t_c], [t_ss])
                P.op("dve", lambda e, which=which, c0=c0, gsbuf=gsbuf: e.scalar_tensor_tensor(
                    cqn[:, which, :], pjt[:, c0:c0 + 512], ss[:, 3:4], gsbuf[:], ALU.mult, ALU.mult),
                    reads=rd + [t_ss, t_c], writes=[t_cqn])
                transposes([cqn[:, which, i * 128:(i + 1) * 128] for i in range(4)],
                           lambda pt, which=which: (cT[:, which, :, :], pt[:]), [t_cqn], [t_cT])
            if LEVEL < 4.4:
                continue
            for g in range(3):
                k = mlc[0] % 2
                mlc[0] += 1
                for kc in range(4):
                    P.op("pe", lambda e, k=k, kc=kc, g=g: e.matmul(ps_ml[k][:, 0:384], cT[:, 0, kc, :],
                                                                  s_wuq[:, kc, g * 384:(g + 1) * 384],
                                                                  start=(kc == 0), stop=(kc == 3)),
                         reads=[t_cT, t_c], writes=[t_ps_ml[k]])
                pv = ps_ml[k][:, 0:384].rearrange("p (h d) -> p h d", d=192)
                P.op("act", lambda e, pv=pv, g=g: e.copy(rb[:, 8 + 2 * g:10 + 2 * g, :], pv[:, :, 0:128]),
                     reads=[t_ps_ml[k]], writes=[t_rb[2], t_rb[3]])
                if LEVEL < 4.42:
                    continue
                qpe_dst = rb[:, 32:35, :].rearrange("p b (h d) -> p (b h) d", d=64)[:, 2 * g:2 * g + 2, :]
                _rope(P, "dve", pv[:, :, 128:192], qpe_dst, c64, s64, 2, 32, tmp_a, tmp_b, [t_ps_ml[k]], [t_rb[8]], t_tab, t_tmp)
            if LEVEL < 4.5:
                continue
            for g in range(3):
                k = mlc[0] % 2
                mlc[0] += 1
                for kc in range(4):
                    P.op("pe", lambda e, k=k, kc=kc, g=g: e.matmul(ps_ml[k][:, 0:512], cT[:, 1, kc, :],
                                                                  s_wukv[:, kc, g * 512:(g + 1) * 512],
                                                                  start=(kc == 0), stop=(kc == 3)),
                         reads=[t_cT, t_c], writes=[t_ps_ml[k]])
                pv = ps_ml[k][:, 0:512].rearrange("p (h d) -> p h d", d=256)
                P.op("act", lambda e, pv=pv, g=g: e.copy(rb[:, 14 + 2 * g:16 + 2 * g, :], pv[:, :, 0:128]),
                     reads=[t_ps_ml[k]], writes=[t_rb[3], t_rb[4]])
                P.op("dve", lambda e, pv=pv, g=g: e.tensor_copy(vt[:, 2 + 2 * g:4 + 2 * g, 0:128], pv[:, :, 128:256]),
                     reads=[t_ps_ml[k]], writes=[t_vt])
            P.dma("pool", VT[tl * 128:(tl + 1) * 128, :, :], vt[:], reads=[t_vt], is_output=True)
            for g4 in range(9 if LEVEL >= 5 else 0):
                srcs = [rb[:, 4 * g4 + i, :] for i in range(4)]
                if g4 == 8:
                    srcs[3] = rb[:, 35, 0:64]
                transposes(srcs, lambda pt, j=j, g4=g4: (tsb[:, 4 * g4:4 * g4 + 4, j * 128:(j + 1) * 128], pt[:]),
                           t_rb, [t_tsb])
        if LEVEL >= 6:
            P.dma("pool", FT[:, :, st * ST * 128:(st + 1) * ST * 128], tsb[:], reads=[t_tsb], is_output=True)
    return P.finish()


NEG = -30000.0
SC128 = 128 ** -0.5
SC192 = 192 ** -0.5


def build_A(S=16384):
    import math
    NI = S // 2048
    NTQ = NI * 512
    NCC = S // 2048
    NCMP = S // 16
    P = Prog(n_dma_sems=12, dma_queues=("sp", "pool"))
    di = lambda n, sh, dt=BF16: P.dram(n, sh, dt, "ExternalInput")
    QaT = di("QaT", [128, 4, NTQ]); QnT = di("QnT", [128, 6, NTQ]); QpeT = di("QpeT", [64, 6, NTQ])
    DqT = di("DqT", [128, 6, NTQ]); GTd = di("GT", [NTQ, 12], F32)
    KsT = di("KsT", [128, S]); Vs = di("Vs", [S, 129]); KnT = di("KnT", [128, 6, S]); KpeT = di("KpeT", [64, S])
    Vm = di("Vm", [S, 6, 129]); KcrT = di("KcrT", [128, S]); VcrT = di("VcrT", [128, S])
    KwL = di("KwL", [128, NI, 1024]); VwL = di("VwL", [NI, 1024, 129])
    DkL = di("DkL", [128, NI, 6, 2560]); DvL = di("DvL", [NI, 2560, 6, 129])
    w_ck1 = di("w_ck1", [128, 32, 128]); w_ck2 = di("w_ck2", [128, 128])
    w_cv1 = di("w_cv1", [128, 32, 128]); w_cv2 = di("w_cv2", [128, 128])
    pembT = di("pembT", [128, 32]); cposT = di("cposT", [128, NCC], I32); inv128 = di("inv128", [128, 64], F32)
    caus = di("caus", [128, 4, 2048]); cmask = di("cmask", [128, 4, 128]); fmask = di("fmask", [128, 4, 64], F32)
    wm = di("wm", [128, 5, 128]); dm = di("dm", [128, 17, 128]); ident = di("ident", [128, 128]); I4d = di("I4", [128, 512])
    O = P.dram("O", [NTQ, 2048], BF16, "ExternalOutput")

    t_c = T()
    def cload(name, src, shape, dt=BF16):
        t = P.sb(name, shape, dt)
        P.dma("sp", t[:], src[:], writes=[t_c])
        return t
    s_caus = cload("s_caus", caus, [128, 4, 2048]); s_cmask = cload("s_cmask", cmask, [128, 4, 128])
    s_fmask = cload("s_fmask", fmask, [128, 4, 64], F32); s_wm = cload("s_wm", wm, [128, 5, 128])
    s_dm = cload("s_dm", dm, [128, 17, 128]); s_id = cload("s_id", ident, [128, 128]); s_I4 = cload("s_I4", I4d, [128, 512])
    s_w1 = [cload("s_wk1", w_ck1, [128, 32, 128]), cload("s_wv1", w_cv1, [128, 32, 128])]
    s_w2 = [cload("s_wk2", w_ck2, [128, 128]), cload("s_wv2", w_cv2, [128, 128])]
    s_pemb = cload("s_pemb", pembT, [128, 32]); s_cpos = cload("s_cpos", cposT, [128, NCC], I32)
    s_inv = cload("s_inv", inv128, [128, 64], F32)
    tiny = P.sb("tiny", [128, 1], F32)
    P.op("dve", lambda e: e.memset(tiny[:], 0.0), writes=[t_c])

    ps_s = [P.ps("ps_s%d" % i, [128, 512]) for i in range(2)]; t_ps_s = [PT(), PT()]
    ps_o = [P.ps("ps_o%d" % i, [128, 512]) for i in range(4)]; t_ps_o = [PT() for _ in range(4)]
    ps_c = [P.ps("ps_c%d" % i, [128, 512]) for i in range(2)]; t_ps_c = [PT(), PT()]

    selx = P.sb("selx", [128, S], BF16); t_selx = T()
    kct = P.sb("kct", [128, NCMP], BF16); t_kct = T()
    vca = P.sb("vca", [128, NCC, 129], BF16); t_vca = T()
    pt = [P.sb("pt%d" % i, [128, 512], BF16) for i in range(2)]; t_pt = [T(), T()]
    ptc = [0]
    ssc = [0]

    def score(terms, rd, pairs=False):
        k = ssc[0] % 2
        ssc[0] += 1
        n = len(terms)
        for idx, (of, l, r) in enumerate(terms):
            o = of(ps_s[k])
            st_ = (idx % 2 == 0) if pairs else (idx == 0)
            sp_ = (idx % 2 == 1) if pairs else (idx == n - 1)
            P.op("pe", lambda e, o=o, l=l, r=r, st_=st_, sp_=sp_: e.matmul(o, l, r, start=st_, stop=sp_, skip_group_check=True),
                 reads=rd, writes=[t_ps_s[k]])
        return k

    def pipeline(seq):
        n = len(seq)

        def do_score(t):
            it = seq[t]
            if it.get("pre"):
                it["pre"]()
            return score(it["terms"](), it["rd"](), it.get("pairs", False))
        k_next = do_score(0)
        for t in range(n):
            k_cur = k_next
            if t + 1 < n:
                k_next = do_score(t + 1)
            j = expo(k_cur, seq[t]["ncols"], seq[t]["scale"])
            seq[t]["pv"](j)

    def expo(k, ncols, scale):
        j = ptc[0] % 2
        ptc[0] += 1
        P.op("act", lambda e: e.activation(pt[j][:, 0:ncols], ps_s[k][:, 0:ncols], AF.Exp, scale=scale),
             reads=[t_ps_s[k]], writes=[t_pt[j]])
        return j

    kcr = selx
    hidT = P.sb("hidT", [128, 512], BF16); t_hid = T()
    gx = [P.sb("gx%d" % i, [128, 512], F32) for i in range(3)]; t_gx = T()
    cb = P.sb("cb", [128, 2], F32); t_cb = T()
    ctab = P.sb("ctab", [128, 2, NCC, 64], F32); t_ctab = T()
    pexp = P.sb("pexp", [128, 4, 1024], F32); t_pexp = T()
    cscr = pexp[:, :, 0:NCC * 64]; t_cscr = t_pexp
    cki = P.sb("cki", [128, NCC * 64], I32)
    cposf = P.sb("cposf", [128, NCC], F32)
    P.op("dve", lambda e: e.tensor_copy(cposf[:], s_cpos[:]), reads=[t_c], writes=[t_cscr])
    angv = cscr[:, 0, :].rearrange("p (t d) -> p t d", d=64)
    P.op("dve", lambda e: e.tensor_tensor(angv, cposf[:].unsqueeze(2).broadcast_to([128, NCC, 64]),
                                          s_inv[:].unsqueeze(1).broadcast_to([128, NCC, 64]), ALU.mult),
         reads=[t_c, t_cscr], writes=[t_cscr])
    t_ang = T(); t_ang.w = t_cscr.w
    vv = lambda i: cscr[:, i, :].rearrange("p (t d) -> p t d", d=64)
    _sincos(P, angv, ctab[:, 1], ctab[:, 0], vv(1), cki[:].rearrange("p (t d) -> p t d", d=64), vv(2), vv(3), t_ang, t_ctab, t_cscr)
    kc_tm = P.sb("kc_tm", [128, 128], BF16); t_kctm = T()
    ra = P.sb("ra", [128, 1, 64], F32); rb_ = P.sb("rb_", [128, 1, 64], F32); t_rt = T()
    ps_tr = P.ps("ps_trA", [128, 128], BF16) if False else None
    P.op("pool", lambda e: e.memset(vca[:], 1.0), writes=[t_vca])
    for which, src in ((0, KcrT), (1, VcrT)):
        P.dma("sp", selx[:, 0:S], src[:, 0:S], writes=[t_selx])
        for i in range(32):
            P.op("pe", lambda e, i=i: e.matmul(ps_c[0][:, 0:1], s_w1[which][:, i, :], s_pemb[:, i:i + 1],
                                               start=(i == 0), stop=(i == 31)), reads=[t_c], writes=[t_ps_c[0]])
        P.op("act", lambda e: e.copy(cb[:, which:which + 1], ps_c[0][:, 0:1]), reads=[t_ps_c[0]], writes=[t_cb])
        for c0 in range(0, NCMP, 512):
            cw = min(512, NCMP - c0)
            for i in range(32):
                lo = 16 * c0 + i
                n_in = min(cw, (S - lo + 15) // 16)
                rhs_main = selx[:, lo:lo + 16 * (n_in - 1) + 1:16]
                P.op("pe", lambda e, i=i, rhs_main=rhs_main, n_in=n_in: e.matmul(
                    ps_c[1][:, 0:n_in], s_w1[which][:, i, :], rhs_main, start=(i == 0), stop=(i == 31),
                    skip_group_check=True), reads=[t_selx, t_c], writes=[t_ps_c[1]])
            x, x2, u = gx[0][:, 0:cw], gx[1][:, 0:cw], gx[2][:, 0:cw]
            P.op("act", lambda e, x=x, cw=cw: e.activation(x, ps_c[1][:, 0:cw], AF.Identity, bias=cb[:, which:which + 1]),
                 reads=[t_ps_c[1], t_cb], writes=[t_gx])
            P.op("dve", lambda e, x=x, x2=x2: e.tensor_tensor(x2, x, x, ALU.mult), reads=[t_gx], writes=[t_gx])
            P.op("dve", lambda e, x2=x2: e.tensor_scalar(x2, x2, 0.044715, 1.0, ALU.mult, ALU.add), reads=[t_gx], writes=[t_gx])
            P.op("dve", lambda e, x=x, x2=x2, u=u: e.tensor_tensor(u, x2, x, ALU.mult), reads=[t_gx], writes=[t_gx])
            P.op("act", lambda e, u=u: e.activation(u, u, AF.Tanh, scale=0.7978845608028654), reads=[t_gx], writes=[t_gx])
            P.op("dve", lambda e, u=u: e.tensor_scalar(u, u, 1.0, 0.5, ALU.add, ALU.mult), reads=[t_gx], writes=[t_gx])
            P.op("dve", lambda e, u=u, x=x, cw=cw: e.tensor_tensor(hidT[:, 0:cw], u, x, ALU.mult), reads=[t_gx], writes=[t_hid])
            for cc in range(cw // 128):
                gch = (c0 + cc * 128) // 128
                P.op("pe", lambda e, cc=cc: e.matmul(ps_c[0][:, 0:128], hidT[:, cc * 128:(cc + 1) * 128], s_w2[which][:],
                                                     start=True, stop=True), reads=[t_hid, t_c], writes=[t_ps_c[0]])
                if which == 0:
                    _rope(P, "dve", ps_c[0][:, 0:128].unsqueeze(1), kc_tm[:].unsqueeze(1), ctab[:, 0, gch, :], ctab[:, 1, gch, :],
                          1, 64, ra, rb_, [t_ps_c[0]], [t_kctm], t_ctab, t_rt)
                    P.op("pe", lambda e: e.transpose(ps_s[0][:, 0:128].bitcast(BF16)[:, 0:128], kc_tm[:], s_id[:]),
                         reads=[t_kctm, t_c], writes=[t_ps_s[0]])
                    P.op("act", lambda e, gch=gch: e.copy(kct[:, gch * 128:(gch + 1) * 128], ps_s[0][:, 0:128].bitcast(BF16)[:, 0:128]),
                         reads=[t_ps_s[0]], writes=[t_kct])
                else:
                    P.op("act", lambda e, gch=gch: e.copy(vca[:, gch, 0:128], ps_c[0][:, 0:128]), reads=[t_ps_c[0]], writes=[t_vca])

    qa = P.sb("qa", [128, 4, 512], BF16); qn = P.sb("qn", [128, 6, 512], BF16); qpe = P.sb("qpe", [64, 6, 512], BF16)
    dq = P.sb("dq", [128, 6, 512], BF16); gts = P.sb("gts", [128, 4, 12], F32); t_q = T()
    obuf = P.sb("obuf", [128, 4, 2048], BF16); t_ob = T()
    kg = [P.sb("kg%d" % i, [128, 2048], BF16) for i in range(2)]
    kpg = [P.sb("kpg%d" % i, [64, 2048], BF16) for i in range(2)]
    vg = [P.sb("vg%d" % i, [128, 16, 129], BF16) for i in range(2)]
    t_kv = [T(), T()]
    kvc = [0]
    kwl = P.sb("kwl", [128, 1024], BF16); vwl = P.sb("vwl", [128, 8, 129], BF16); t_wl = T()
    dkl = [P.sb("dkl%d" % i_, [128, 2560], BF16) for i_ in range(2)]
    dvl = [P.sb("dvl%d" % i_, [128, 20, 129], BF16) for i_ in range(2)]; t_dl = [T(), T()]
    psm = P.sb("psm", [128, 1024], F32); t_psm = T()
    rs = P.sb("rs", [128, 8], F32); rsum = P.sb("rsum", [128, 4], F32); t_rs = T()
    imp = P.sb("imp", [128, 256], F32); imp2 = P.sb("imp2", [128, 256], F32); t_imp = T()
    m8 = P.sb("m8", [128, 16], F32); t_m8 = T()
    selb = P.sb("selb", [128, 256], BF16); t_selb = T()
    dn = P.sb("dn", [128, 4], F32); coef = P.sb("coef", [128, 4], F32); t_dn = T()
    acc = P.sb("acc", [128, 4, 128], F32); t_acc = T()
    tinyv = 1e-30

    def full(n):
        return lambda ps: ps[:, 0:n]

    def full3(ps):
        return ps[:, 0:512].rearrange("p (h q) -> p h q", h=4)

    def cols(c0, n):
        return lambda ps: ps[:, c0:c0 + n]

    def evac_norm(r_or_h, dst):
        x = r_or_h
        P.op("dve", lambda e: e.tensor_scalar(dn[:, x:x + 1], ps_o[x][:, 128:129], tinyv, None, ALU.max),
             reads=[t_ps_o[x]], writes=[t_dn])
        P.op("dve", lambda e: e.reciprocal(dn[:, x:x + 1], dn[:, x:x + 1]), reads=[t_dn], writes=[t_dn])
        P.op("act", lambda e: e.activation(dst, ps_o[x][:, 0:128], AF.Copy, scale=dn[:, x:x + 1]),
             reads=[t_ps_o[x], t_dn], writes=[t_ob])

    for i in range(NI):
        q0 = i * 512
        P.dma("sp", qa[:], QaT[:, :, q0:q0 + 512], writes=[t_q])
        P.dma("sp", qn[:], QnT[:, :, q0:q0 + 512], writes=[t_q])
        P.dma("sp", qpe[:], QpeT[:, :, q0:q0 + 512], writes=[t_q])
        P.dma("sp", dq[:], DqT[:, :, q0:q0 + 512], writes=[t_q])
        P.dma("sp", gts[:], GTd[q0:q0 + 512, :].rearrange("(r p) g -> p r g", p=128), writes=[t_q])
        P.dma("pool", kwl[:], KwL[:, i, :], writes=[t_wl])
        P.dma("pool", vwl[:], VwL[i].rearrange("(c p) x -> p c x", p=128), writes=[t_wl])
        Ci = 128 * (i + 1)
        J = Ci // 4

        for h in range(6):
            seq = []
            for p in range(i + 1):
                st = {}

                def pre(st=st, p=p, h=h):
                    b = kvc[0] % 2
                    kvc[0] += 1
                    st["b"] = b
                    g0 = p * 2048
                    P.dma("sp", kg[b][:], KnT[:, h, g0:g0 + 2048], writes=[t_kv[b]])
                    P.dma("sp", kpg[b][:], KpeT[:, g0:g0 + 2048], writes=[t_kv[b]])
                    P.dma("pool", vg[b][:], Vm[g0:g0 + 2048, h, :].rearrange("(c p) x -> p c x", p=128), writes=[t_kv[b]])
                for kc in range(16):
                    def terms(st=st, kc=kc, p=p, h=h):
                        b = st["b"]
                        ks = slice(kc * 128, (kc + 1) * 128)
                        tt = [(full(512), kg[b][:, ks], qn[:, h, :]), (full(512), kpg[b][:, ks], qpe[:, h, :])]
                        if p == i:
                            for r in range(4):
                                tt.append((cols(r * 128, 128), s_caus[:, r, ks], s_id[:]))
                        return tt

                    def pv(j, st=st, kc=kc, p=p):
                        b = st["b"]
                        first = (p == 0 and kc == 0)
                        last = (p == i and kc == 15)
                        for r in range(4):
                            P.op("pe", lambda e, r=r: e.matmul(ps_o[r][:, 0:129], pt[j][:, r * 128:(r + 1) * 128],
                                                               vg[b][:, kc, :], start=first, stop=last),
                                 reads=[t_pt[j], t_kv[b]], writes=[t_ps_o[r]])
                    seq.append(dict(pre=pre if kc == 0 else None, terms=terms, rd=lambda st=st: [t_kv[st["b"]], t_q, t_c],
                                    ncols=512, scale=SC192, pv=pv))
            pipeline(seq)
            for r in range(4):
                evac_norm(r, obuf[:, r, 512 + h * 128:512 + (h + 1) * 128])

        for h in range(6):
            db = h % 2
            P.dma("sp", dkl[db][:], DkL[:, i, h, :], writes=[t_dl[db]])
            P.dma("pool", dvl[db][:], DvL[i, :, h, :].rearrange("(c p) x -> p c x", p=128), writes=[t_dl[db]])
            for r in range(4):
                rq = slice(r * 128, (r + 1) * 128)
                x = r
                us = list(range(r, r + 17))
                seq = []
                for b0 in range(0, 17, 4):
                    ub = us[b0:b0 + 4]

                    def terms(ub=ub, r=r, rq=rq, h=h, db=db):
                        tt = []
                        for jj, u in enumerate(ub):
                            tt.append((cols(jj * 128, 128), dkl[db][:, u * 128:(u + 1) * 128], dq[:, h, rq]))
                            tt.append((cols(jj * 128, 128), s_dm[:, 16 - (u - r), :], s_id[:]))
                        return tt

                    def pv(j, ub=ub, r=r, x=x, db=db):
                        for jj, u in enumerate(ub):
                            P.op("pe", lambda e, jj=jj, u=u: e.matmul(ps_o[x][:, 0:129], pt[j][:, jj * 128:(jj + 1) * 128], dvl[db][:, u, :],
                                                                     start=(u == r), stop=(u == r + 16)),
                                 reads=[t_pt[j], t_dl[db]], writes=[t_ps_o[x]])
                    seq.append(dict(pre=None, terms=terms, rd=lambda db=db: [t_dl[db], t_q, t_c], ncols=128 * len(ub), scale=SC128,
                                    pv=pv, pairs=True))
                pipeline(seq)
                evac_norm(x, obuf[:, r, 1280 + h * 128:1280 + (h + 1) * 128])

        for r in range(4):
            rq = slice(r * 128, (r + 1) * 128)
            qa3 = qa[:, :, rq]
            P.op("dve", lambda e: e.memset(rs[:], 0.0), writes=[t_rs])
            nh = (Ci + 511) // 512
            mh, mo = (Ci - 128) // 512, (Ci - 128) % 512
            for h in range(4):
                for hf in range(nh):
                    cw = min(512, Ci - 512 * hf)
                    P.op("pe", lambda e, hf=hf, cw=cw, h=h: e.matmul(ps_c[hf][:, 0:cw], qa[:, h, rq], kct[:, 512 * hf:512 * hf + cw],
                                                                    start=True, stop=(hf != mh), skip_group_check=True),
                         reads=[t_q, t_kct], writes=[t_ps_c[hf]])
                P.op("pe", lambda e, r=r: e.matmul(ps_c[mh][:, mo:mo + 128], s_id[:], s_cmask[:, r, :], start=False, stop=True,
                                                   skip_group_check=True), reads=[t_c], writes=[t_ps_c[mh]])
                for hf in range(nh):
                    cw = min(512, Ci - 512 * hf)
                    P.op("act", lambda e, hf=hf, cw=cw, h=h: e.activation(pexp[:, h, 512 * hf:512 * hf + cw], ps_c[hf][:, 0:cw], AF.Exp,
                                                                         scale=SC128, accum_out=rs[:, 2 * h + hf:2 * h + hf + 1]),
                         reads=[t_ps_c[hf], t_rs], writes=[t_pexp, t_rs])
            P.op("dve", lambda e: e.tensor_reduce(rsum[:], rs[:].rearrange("p (h t) -> p h t", t=2), AX.X, ALU.add),
                 reads=[t_rs], writes=[t_rs])
            P.op("dve", lambda e: e.tensor_scalar(rsum[:], rsum[:], tinyv, None, ALU.max), reads=[t_rs], writes=[t_rs])
            P.op("dve", lambda e: e.reciprocal(rsum[:], rsum[:]), reads=[t_rs], writes=[t_rs])
            P.op("dve", lambda e: e.tensor_scalar(psm[:, 0:Ci], pexp[:, 0, 0:Ci], rsum[:, 0:1], None, ALU.mult),
                 reads=[t_pexp, t_rs], writes=[t_psm])
            for h in range(1, 4):
                P.op("dve", lambda e, h=h: e.scalar_tensor_tensor(psm[:, 0:Ci], pexp[:, h, 0:Ci], rsum[:, h:h + 1], psm[:, 0:Ci],
                                                                  ALU.mult, ALU.add), reads=[t_pexp, t_rs, t_psm], writes=[t_psm])
            pv4 = psm[:, 0:Ci].rearrange("p (j k) -> p j k", k=4)
            P.op("dve", lambda e: e.tensor_reduce(imp[:, 0:J], pv4, AX.X, ALU.add), reads=[t_psm], writes=[t_imp])
            P.op("dve", lambda e: e.tensor_tensor(imp[:, 1:J], imp[:, 1:J], pv4[:, 0:J - 1, 3], ALU.add), reads=[t_psm, t_imp], writes=[t_imp])
            P.op("dve", lambda e: e.tensor_scalar(imp[:, 0:1], imp[:, 0:1], 1e9, None, ALU.add), reads=[t_imp], writes=[t_imp])
            if i >= 1:
                P.op("dve", lambda e, r=r: e.tensor_tensor(imp[:, J - 64:J], imp[:, J - 64:J], s_fmask[:, r, :], ALU.add),
                     reads=[t_imp, t_c], writes=[t_imp])
            else:
                P.op("dve", lambda e, r=r: e.tensor_tensor(imp[:, 0:32], imp[:, 0:32], s_fmask[:, r, 32:64], ALU.add),
                     reads=[t_imp, t_c], writes=[t_imp])
            P.op("dve", lambda e: e.max(m8[:, 0:8], imp[:, 0:J]), reads=[t_imp], writes=[t_m8])
            P.op("dve", lambda e: e.match_replace(imp2[:, 0:J], m8[:, 0:8], imp[:, 0:J], -3e9), reads=[t_imp, t_m8], writes=[t_imp])
            P.op("dve", lambda e: e.max(m8[:, 8:16], imp2[:, 0:J]), reads=[t_imp], writes=[t_m8])
            P.op("dve", lambda e: e.tensor_scalar(selb[:, 0:J], imp[:, 0:J], m8[:, 15:16], NEG, ALU.is_lt, ALU.mult),
                 reads=[t_imp, t_m8], writes=[t_selb])
            P.op("pool", lambda e: e.tensor_copy(selx[:, 0:64 * J].rearrange("p (j k) -> p j k", k=64),
                                                 selb[:, 0:J].unsqueeze(2).broadcast_to([128, J, 64])),
                 reads=[t_selb], writes=[t_selx])
            P.op("pool", lambda e, r=r: e.tensor_tensor(selx[:, 64 * J - 2048:64 * J], selx[:, 64 * J - 2048:64 * J], s_caus[:, r, :], ALU.add),
                 reads=[t_selx, t_c], writes=[t_selx])

            def branch_evac(br, last_branch):
                for h in range(4):
                    P.op("dve", lambda e, h=h: e.tensor_scalar(dn[:, h:h + 1], ps_o[h][:, 128:129], tinyv, None, ALU.max),
                         reads=[t_ps_o[h]], writes=[t_dn])
                P.op("dve", lambda e: e.reciprocal(dn[:], dn[:]), reads=[t_dn], writes=[t_dn])
                gv = gts[:, r, :].rearrange("p (h t) -> p h t", t=3)[:, :, br]
                P.op("dve", lambda e: e.tensor_tensor(coef[:], dn[:], gv, ALU.mult), reads=[t_dn, t_q], writes=[t_dn])
                for h in range(4):
                    dst = obuf[:, r, h * 128:(h + 1) * 128] if last_branch else acc[:, h, :]
                    wr = [t_ob] if last_branch else [t_acc]
                    if br == 0:
                        P.op("dve", lambda e, h=h, dst=dst: e.tensor_scalar(dst, ps_o[h][:, 0:128], coef[:, h:h + 1], None, ALU.mult),
                             reads=[t_ps_o[h], t_dn], writes=wr)
                    else:
                        P.op("dve", lambda e, h=h, dst=dst: e.scalar_tensor_tensor(dst, ps_o[h][:, 0:128], coef[:, h:h + 1], acc[:, h, :],
                                                                                 ALU.mult, ALU.add),
                             reads=[t_ps_o[h], t_dn, t_acc], writes=wr)

            def pv4h(j, vrhs, first, last, rd):
                for h in range(4):
                    P.op("pe", lambda e, h=h: e.matmul(ps_o[h][:, 0:129], pt[j][:, h * 128:(h + 1) * 128], vrhs, start=first, stop=last),
                         reads=[t_pt[j]] + rd, writes=[t_ps_o[h]])

            seq = []
            for cc in range(i + 1):
                def terms(cc=cc, r=r):
                    tt = [(full3, kct[:, cc * 128:(cc + 1) * 128], qa3)]
                    if cc == i:
                        tt.append((full(512), s_cmask[:, r, :], s_I4[:]))
                    return tt
                seq.append(dict(pre=None, terms=terms, rd=lambda: [t_kct, t_q, t_c], ncols=512, scale=SC128,
                                pv=lambda j, cc=cc: pv4h(j, vca[:, cc, :], cc == 0, cc == i, [t_vca])))
            pipeline(seq)
            branch_evac(0, False)
            seq = []
            for o in range(5):
                def terms(o=o, r=r):
                    u = r + o
                    tt = [(full3, kwl[:, u * 128:(u + 1) * 128], qa3)]
                    if o in (0, 4):
                        tt.append((full(512), s_wm[:, o, :], s_I4[:]))
                    return tt
                seq.append(dict(pre=None, terms=terms, rd=lambda: [t_wl, t_q, t_c], ncols=512, scale=SC128,
                                pv=lambda j, o=o, r=r: pv4h(j, vwl[:, r + o, :], o == 0, o == 4, [t_wl])))
            pipeline(seq)
            branch_evac(2, False)

            seq = []
            for p in range(i + 1):
                st = {}

                def pre(st=st, p=p):
                    b = kvc[0] % 2
                    kvc[0] += 1
                    st["b"] = b
                    g0 = p * 2048
                    P.dma("sp", kg[b][:], KsT[:, g0:g0 + 2048], writes=[t_kv[b]])
                    P.dma("pool", vg[b][:], Vs[g0:g0 + 2048, :].rearrange("(c p) x -> p c x", p=128), writes=[t_kv[b]])
                for kc in range(16):
                    def terms(st=st, kc=kc, p=p):
                        b = st["b"]
                        g0 = p * 2048
                        ks = slice(kc * 128, (kc + 1) * 128)
                        return [(full3, kg[b][:, ks], qa3), (full(512), selx[:, g0 + kc * 128:g0 + (kc + 1) * 128], s_I4[:])]
                    seq.append(dict(pre=pre if kc == 0 else None, terms=terms, rd=lambda st=st: [t_kv[st["b"]], t_q, t_c, t_selx],
                                    ncols=512, scale=SC128,
                                    pv=lambda j, st=st, kc=kc, p=p: pv4h(j, vg[st["b"]][:, kc, :], p == 0 and kc == 0, p == i and kc == 15,
                                                                       [t_kv[st["b"]]])))
            pipeline(seq)
            branch_evac(1, True)
        for r in range(4):
            P.dma("pool", O[(i * 4 + r) * 128:(i * 4 + r + 1) * 128, :], obuf[:, r, :], reads=[t_ob], is_output=True)
    return P.finish()


def _rep(v):
    return np.ascontiguousarray(np.broadcast_to(np.asarray(v)[None, :], (128, np.asarray(v).size)))


def _inv_freq(dim):
    return (10000.0 ** (-np.arange(0, dim, 2, dtype=np.float32) / dim)).astype(np.float32)


def a_core_masks(c):
    q = np.arange(128)[:, None, None]
    r = np.arange(4)[None, :, None]
    kl = np.arange(2048)[None, None, :]
    caus = np.where(kl <= 512 * c + 128 * r + q, 0.0, NEG).astype(np.float32)
    cl = np.arange(128)[None, None, :]
    cmask = np.where(16 * cl + 31 <= 512 * c + 128 * r + q, 0.0, NEG).astype(np.float32)
    jl = np.arange(-32, 32)[None, None, :]
    cur = 8 * c + 2 * r + (q >= 64)
    fmask = np.where((jl == cur) | (jl == cur - 1), 1e9, np.where(jl > cur, -1e9, 0.0)).astype(np.float32)
    return caus.astype(NPBF), cmask.astype(NPBF), fmask


def a_const_masks():
    q = np.arange(128)[:, None]
    k = np.arange(128)[None, :]
    wm = np.zeros((128, 5, 128), np.float32)
    wm[:, 0, :] = np.where(k >= q, 0.0, NEG)
    wm[:, 4, :] = np.where(k <= q, 0.0, NEG)
    dm = np.zeros((128, 17, 128), np.float32)
    for dc in range(17):
        d = 128 * dc + q - k
        mult = ((d >= 0) & (d <= 128)).astype(np.int64) + ((d >= 0) & (d % 4 == 0) & (d <= 512)) + ((d >= 0) & (d % 16 == 0) & (d <= 2048))
        dm[:, dc, :] = np.where(mult > 0, np.log(np.maximum(mult, 1)) / SC128, NEG)
    ident = np.eye(128, dtype=np.float32)
    I4 = np.tile(ident, (1, 4))
    return wm.astype(NPBF), dm.astype(NPBF), ident.astype(NPBF), I4.astype(NPBF)


def a_inputs(FTb, VTb, GTb, posb, c, cw, S):
    NI = S // 2048
    idx = np.concatenate([np.arange(2048 * i + 512 * c, 2048 * i + 512 * c + 512) for i in range(NI)])
    z = lambda *sh: np.zeros(sh, NPBF)
    m = {}
    m["QaT"] = FTb[:, 0:4][:, :, idx]
    m["QnT"] = FTb[:, 8:14][:, :, idx]
    qpe = np.stack([FTb[(h % 2) * 64:(h % 2) * 64 + 64, 32 + h // 2] for h in range(6)], 1)
    m["QpeT"] = qpe[:, :, idx]
    m["DqT"] = FTb[:, 20:26][:, :, idx]
    m["GT"] = GTb[idx]
    m["KsT"] = FTb[:, 6]; m["Vs"] = VTb[:, 0]; m["KnT"] = FTb[:, 14:20]; m["KpeT"] = FTb[0:64, 35]
    m["Vm"] = VTb[:, 2:8]; m["KcrT"] = FTb[:, 4]; m["VcrT"] = FTb[:, 5]
    KwL = z(128, NI, 1024); VwL = z(NI, 1024, 129); DkL = z(128, NI, 6, 2560); DvL = z(NI, 2560, 6, 129)
    for i in range(NI):
        st = 2048 * i + 512 * c
        lo = max(0, st - 512)
        KwL[:, i, 1024 - (st + 512 - lo):] = FTb[:, 7, lo:st + 512]
        VwL[i, 1024 - (st + 512 - lo):] = VTb[lo:st + 512, 1]
        lo = max(0, st - 2048)
        DkL[:, i, :, 2560 - (st + 512 - lo):] = FTb[:, 26:32, lo:st + 512]
        DvL[i, 2560 - (st + 512 - lo):] = VTb[lo:st + 512, 8:14]
    m["KwL"], m["VwL"], m["DkL"], m["DvL"] = KwL, VwL, DkL, DvL
    m.update(cw)
    NCMP = S // 16
    cend = np.minimum(16 * np.arange(NCMP) + 31, S - 1)
    m["cposT"] = np.ascontiguousarray(posb[cend].reshape(NCMP // 128, 128).T).astype(np.int32)
    m["inv128"] = _rep(_inv_freq(128))
    m["caus"], m["cmask"], m["fmask"] = a_core_masks(c)
    m["wm"], m["dm"], m["ident"], m["I4"] = a_const_masks()
    return {k: np.ascontiguousarray(v) for k, v in m.items()}


def build_p3(NT=4096, ST=4):
    NTL = NT // 128
    NSUP = NTL // ST
    TW = ST * 128
    P = Prog(n_dma_sems=12, dma_queues=("sp", "pool"))
    di = lambda n, sh, dt=BF16: P.dram(n, sh, dt, "ExternalInput")
    h_in = di("h", [NT, D], F32); O_in = di("O", [NT, D]); mem = di("mem", [256, D], F32)
    w_out = di("w_out", [128, 16, 2048]); w_xq = di("w_xq", [128, 16, 512]); w_xkv = di("w_xkv", [128, 16, 1024])
    w_xo = di("w_xo", [128, 4, 2048]); w_up = di("w_up", [128, 16, 8192]); w_down = di("w_down", [128, 64, 2048])
    gains = di("gains", [128, 6, D], F32)
    ident = di("ident", [128, 128])
    hout = P.dram("hout", [NT, D], F32, "ExternalOutput")

    t_c = T()
    gbuf = [P.sb("gbuf%d" % i, [128, D], F32) for i in range(2)]; t_gb = [T(), T()]
    gbc = [0]

    def gload(gi):
        k = gbc[0] % 2
        gbc[0] += 1
        P.dma("pool", gbuf[k][:], gains[:, gi, :], writes=[t_gb[k]])
        return k
    s_id = P.sb("s_id", [128, 128], BF16)
    P.dma("sp", s_id[:], ident[:], writes=[t_c])
    epsb = P.sb("epsb", [128, 1], F32)
    P.op("dve", lambda e: e.memset(epsb[:], EPS), writes=[t_c])
    G_POST, G_MPRE, G_MKV, G_MPOST, G_FPRE, G_FPOST = range(6)

    hres = [P.sb("hres%d" % j, [128, D], F32) for j in range(ST)]; t_hres = [T() for _ in range(ST)]
    ybuf = [P.sb("ybuf%d" % j, [128, D], F32) for j in range(ST)]; t_y = [T() for _ in range(ST)]
    xT = P.sb("xT", [128, 16, TW], BF16); t_xT = T()
    hidT = P.sb("hidT", [128, 64, TW], BF16); t_hid = T()
    wg = [P.sb("wg%d" % i, [128, 16, 512], BF16) for i in range(2)]; t_wg = [T(), T()]
    hn = P.sb("hn", [128, D], BF16); t_hn = T()
    ss = P.sb("ss", [128, 2], F32); t_ss = T()
    rtmp = P.sb("rtmp", [128, TW], F32); t_rt = T()
    kxT = P.sb("kxT", [128, 4, 256], BF16); vx = P.sb("vx", [128, 2, 4, 129], BF16); t_kvx = T()
    qxT = P.sb("qxT", [128, 4, 128], BF16); t_qx = T()
    ptx = P.sb("ptx", [128, 1024], BF16); t_ptx = T()
    ox = P.sb("ox", [128, 512], BF16); t_ox = T()
    dn = P.sb("dn", [128, 4], F32); t_dn = T()
    ps_tr = [P.ps("ps_tr%d" % i, [128, 4, 128], BF16) for i in range(2)]; t_ps_tr = [PT(), PT()]
    NMM = max(ST, 2)
    ps_mm = [P.ps("ps_mm%d" % i, [128, 512]) for i in range(NMM)]; t_ps_mm = [PT() for _ in range(NMM)]
    ps_s1 = P.ps("ps_sx", [128, 512]); ps_o1 = P.ps("ps_ox", [128, 512])
    ps_s = [ps_s1, ps_s1]; t_ps_s = [PT()]; t_ps_s.append(t_ps_s[0])
    ps_o = [ps_o1, ps_o1]; t_ps_o = [PT()]; t_ps_o.append(t_ps_o[0])
    trc, evc, wgc, mmc = [0], [0], [0], [0]

    def transposes(srcs, dst, rd, wr):
        k = trc[0] % 2
        trc[0] += 1
        for i_, sa in enumerate(srcs):
            P.op("pe", lambda e, i_=i_, sa=sa: e.transpose(ps_tr[k][:, i_, :], sa, s_id[:]), reads=rd + [t_c], writes=[t_ps_tr[k]])
        n = len(srcs)
        if evc[0] % 2 == 0:
            P.op("act", lambda e: e.copy(dst, ps_tr[k][:, 0:n, :]), reads=[t_ps_tr[k]], writes=wr)
        else:
            P.op("dve", lambda e: e.tensor_copy(dst, ps_tr[k][:, 0:n, :]), reads=[t_ps_tr[k]], writes=wr)
        evc[0] += 1

    def loadw(src_ap, kc_n, ncols):
        b = wgc[0] % 2
        wgc[0] += 1
        P.dma("sp" if b == 0 else "pool", wg[b][:, 0:kc_n, 0:ncols], src_ap, writes=[t_wg[b]])
        return b

    def norm_T(src, t_src, gk, j, ncol_tiles=16):
        P.op("act", lambda e: e.activation(hn[:], src, AF.Square, accum_out=ss[:, 0:1]), reads=[t_src], writes=[t_hn, t_ss])
        _rstd(P, ss[:, 0:1], ss[:, 1:2], D, epsb[:], [t_ss, t_c], [t_ss])
        P.op("dve", lambda e: e.scalar_tensor_tensor(hn[:], src, ss[:, 1:2], gbuf[gk][:], ALU.mult, ALU.mult),
             reads=[t_src, t_ss, t_c, t_gb[gk]], writes=[t_hn])
        for g4 in range(4):
            transposes([hn[:, (4 * g4 + i_) * 128:(4 * g4 + i_ + 1) * 128] for i_ in range(4)],
                       xT[:, 4 * g4:4 * g4 + 4, j * 128:(j + 1) * 128], [t_hn], [t_xT])

    def norm_res(j, gk):
        P.op("act", lambda e: e.activation(hn[:], ybuf[j][:], AF.Square, accum_out=ss[:, 0:1]), reads=[t_y[j]], writes=[t_hn, t_ss])
        _rstd(P, ss[:, 0:1], ss[:, 1:2], D, epsb[:], [t_ss, t_c], [t_ss])
        P.op("dve", lambda e: e.scalar_tensor_tensor(ybuf[j][:], ybuf[j][:], ss[:, 1:2], gbuf[gk][:], ALU.mult, ALU.mult),
             reads=[t_y[j], t_ss, t_c, t_gb[gk]], writes=[t_y[j]])
        P.op("pool", lambda e: e.tensor_tensor(hres[j][:], hres[j][:], ybuf[j][:], ALU.add), reads=[t_y[j], t_hres[j]], writes=[t_hres[j]])

    def linear_tm(w_src_fn, KC, rd_x, x_fn):
        for n in range(4):
            b = loadw(w_src_fn(n), KC, 512)
            for j in range(ST):
                k = mmc[0] % NMM
                mmc[0] += 1
                for kc in range(KC):
                    P.op("pe", lambda e, kc=kc, j=j, k=k, b=b: e.matmul(ps_mm[k][:, :], x_fn(j, kc), wg[b][:, kc, :],
                                                                      start=(kc == 0), stop=(kc == KC - 1)),
                         reads=rd_x + [t_wg[b]], writes=[t_ps_mm[k]])
                P.op("act", lambda e, j=j, k=k, n=n: e.copy(ybuf[j][:, n * 512:(n + 1) * 512], ps_mm[k][:, :]),
                     reads=[t_ps_mm[k]], writes=[t_y[j]])

    gk0 = gload(G_MKV)
    for kt in range(2):
        P.dma("sp", hres[0][:], mem[kt * 128:(kt + 1) * 128, :], writes=[t_hres[0]])
        norm_T(hres[0][:], t_hres[0], gk0, kt)
    for hh in range(4):
        if hh == 0:
            b = loadw(w_xkv[:, :, 0:512], 16, 512)
        for kc in range(16):
            P.op("pe", lambda e, kc=kc, hh=hh, b=b: e.matmul(ps_mm[0][:, 0:256], wg[b][:, kc, hh * 128:(hh + 1) * 128], xT[:, kc, 0:256],
                                                             start=(kc == 0), stop=(kc == 15)), reads=[t_xT, t_wg[b]], writes=[t_ps_mm[0]])
        P.op("act", lambda e, hh=hh: e.copy(kxT[:, hh, :], ps_mm[0][:, 0:256]), reads=[t_ps_mm[0]], writes=[t_kvx])
    P.op("pool", lambda e: e.memset(vx[:], 1.0), writes=[t_kvx])
    b = loadw(w_xkv[:, :, 512:1024], 16, 512)
    for kt in range(2):
        for kc in range(16):
            P.op("pe", lambda e, kc=kc, kt=kt, b=b: e.matmul(ps_mm[1][:, :], xT[:, kc, kt * 128:(kt + 1) * 128], wg[b][:, kc, :],
                                                             start=(kc == 0), stop=(kc == 15)), reads=[t_xT, t_wg[b]], writes=[t_ps_mm[1]])
        P.op("act", lambda e, kt=kt: e.copy(vx[:, kt, :, 0:128], ps_mm[1][:, :].rearrange("p (h d) -> p h d", d=128)),
             reads=[t_ps_mm[1]], writes=[t_kvx])

    for st in range(NSUP):
        for j in range(ST):
            tl = st * ST + j
            P.dma("sp", hres[j][:], h_in[tl * 128:(tl + 1) * 128, :], writes=[t_hres[j]])
            P.dma("pool", hn[:], O_in[tl * 128:(tl + 1) * 128, :], writes=[t_hn])
            for g4 in range(4):
                transposes([hn[:, (4 * g4 + i_) * 128:(4 * g4 + i_ + 1) * 128] for i_ in range(4)],
                           xT[:, 4 * g4:4 * g4 + 4, j * 128:(j + 1) * 128], [t_hn], [t_xT])
        linear_tm(lambda n: w_out[:, :, n * 512:(n + 1) * 512], 16, [t_xT], lambda j, kc: xT[:, kc, j * 128:(j + 1) * 128])
        ga, gb_ = gload(G_POST), gload(G_MPRE)
        for j in range(ST):
            norm_res(j, ga)
            norm_T(hres[j][:], t_hres[j], gb_, j)
        bq = loadw(w_xq[:, :, :], 16, 512)
        for j in range(ST):
            for hh in range(4):
                for kc in range(16):
                    P.op("pe", lambda e, kc=kc, hh=hh, j=j: e.matmul(ps_mm[0][:, hh * 128:(hh + 1) * 128], wg[bq][:, kc, hh * 128:(hh + 1) * 128],
                                                                    xT[:, kc, j * 128:(j + 1) * 128], start=(kc == 0), stop=(kc == 15),
                                                                    skip_group_check=True),
                         reads=[t_xT, t_wg[bq]], writes=[t_ps_mm[0]])
            P.op("act", lambda e: e.copy(qxT[:], ps_mm[0][:, :].rearrange("p (h q) -> p h q", h=4)), reads=[t_ps_mm[0]], writes=[t_qx])
            for half in range(2):
                for hi in range(2):
                    hh = 2 * half + hi
                    for kt in range(2):
                        cb_ = (hi * 2 + kt) * 128
                        P.op("pe", lambda e, hh=hh, kt=kt, cb_=cb_, half=half: e.matmul(ps_s[half][:, cb_:cb_ + 128], kxT[:, hh, kt * 128:(kt + 1) * 128],
                                                                                   qxT[:, hh, :], start=True, stop=True, skip_group_check=True),
                             reads=[t_kvx, t_qx], writes=[t_ps_s[half]])
                P.op("act", lambda e, half=half: e.activation(ptx[:, half * 512:(half + 1) * 512], ps_s[half][:, :], AF.Exp, scale=SC128),
                     reads=[t_ps_s[half]], writes=[t_ptx])
                for hi in range(2):
                    hh = 2 * half + hi
                    for kt in range(2):
                        cb_ = half * 512 + (hi * 2 + kt) * 128
                        P.op("pe", lambda e, hh=hh, kt=kt, cb_=cb_, hi=hi, half=half: e.matmul(
                            ps_o[half][:, hi * 129:(hi + 1) * 129], ptx[:, cb_:cb_ + 128], vx[:, kt, hh, :],
                            start=(kt == 0), stop=(kt == 1), skip_group_check=True), reads=[t_ptx, t_kvx], writes=[t_ps_o[half]])
                for hi in range(2):
                    hh = 2 * half + hi
                    P.op("dve", lambda e, hh=hh, hi=hi, half=half: e.reciprocal(dn[:, hh:hh + 1], ps_o[half][:, hi * 129 + 128:hi * 129 + 129]),
                         reads=[t_ps_o[half]], writes=[t_dn])
                    P.op("act", lambda e, hh=hh, hi=hi, half=half: e.activation(ox[:, hh * 128:(hh + 1) * 128], ps_o[half][:, hi * 129:hi * 129 + 128],
                                                                               AF.Copy, scale=dn[:, hh:hh + 1]),
                         reads=[t_ps_o[half], t_dn], writes=[t_ox])
            transposes([ox[:, i_ * 128:(i_ + 1) * 128] for i_ in range(4)], hidT[:, 0:4, j * 128:(j + 1) * 128], [t_ox], [t_hid])
        linear_tm(lambda n: w_xo[:, :, n * 512:(n + 1) * 512], 4, [t_hid], lambda j, kc: hidT[:, kc, j * 128:(j + 1) * 128])
        ga, gb_ = gload(G_MPOST), gload(G_FPRE)
        for j in range(ST):
            norm_res(j, ga)
            norm_T(hres[j][:], t_hres[j], gb_, j)
        for m in range(16):
            b = loadw(w_up[:, :, m * 512:(m + 1) * 512], 16, 512)
            for hc in range(4):
                k = mmc[0] % NMM
                mmc[0] += 1
                for kc in range(16):
                    P.op("pe", lambda e, kc=kc, hc=hc, k=k, b=b: e.matmul(ps_mm[k][:, 0:TW], wg[b][:, kc, hc * 128:(hc + 1) * 128], xT[:, kc, :],
                                                                        start=(kc == 0), stop=(kc == 15)),
                         reads=[t_xT, t_wg[b]], writes=[t_ps_mm[k]])
                P.op("act", lambda e, k=k: e.activation(rtmp[:], ps_mm[k][:, 0:TW], AF.Relu), reads=[t_ps_mm[k]], writes=[t_rt])
                P.op("dve", lambda e, m=m, hc=hc: e.tensor_tensor(hidT[:, m * 4 + hc, :], rtmp[:], rtmp[:], ALU.mult), reads=[t_rt], writes=[t_hid])
        for n in range(4):
            for kg in range(4):
                b = loadw(w_down[:, kg * 16:(kg + 1) * 16, n * 512:(n + 1) * 512], 16, 512)
                for j in range(ST):
                    for kc in range(16):
                        P.op("pe", lambda e, kc=kc, kg=kg, j=j, b=b: e.matmul(ps_mm[j][:, :], hidT[:, kg * 16 + kc, j * 128:(j + 1) * 128], wg[b][:, kc, :],
                                                                            start=(kg == 0 and kc == 0), stop=(kg == 3 and kc == 15),
                                                                            skip_group_check=True),
                             reads=[t_hid, t_wg[b]], writes=[t_ps_mm[j]])
            for j in range(ST):
                P.op("act", lambda e, j=j, n=n: e.copy(ybuf[j][:, n * 512:(n + 1) * 512], ps_mm[j][:, :]), reads=[t_ps_mm[j]], writes=[t_y[j]])
        ga = gload(G_FPOST)
        for j in range(ST):
            tl = st * ST + j
            norm_res(j, ga)
            P.dma("pool", hout[tl * 128:(tl + 1) * 128, :], hres[j][:], reads=[t_hres[j]], is_output=True)
    return P.finish()


def _klay(w):
    k, n = w.shape
    return np.ascontiguousarray(w.reshape(k // 128, 128, n).transpose(1, 0, 2))


_PROGS = {}


def _prog(name, fn):
    if name not in _PROGS:
        _PROGS[name] = fn()
    return _PROGS[name]


def kernel(x, mem, positions, g_mix_pre, w_in, cmp_pos_emb, w_cmp_k1, w_cmp_k2, w_cmp_v1, w_cmp_v2,
           g_q_lora, g_kv_lora, w_uq, w_ukv, w_out, g_mix_post, g_mem_pre, g_mem_kv, w_xq, w_xkv, w_xo,
           g_mem_post, g_mlp_pre, w_up, w_down, g_mlp_post):
    f32 = lambda a: np.asarray(a, dtype=np.float32)
    x = f32(x); mem = f32(mem); positions = np.asarray(positions).astype(np.int32)
    DEPTH = int(np.asarray(w_in).shape[0])
    names, arrs = [], []
    for l in range(DEPTH):
        lay = {
            "w_in": _klay(f32(w_in[l])[:, W_IN_PERM]), "w_uq": _klay(f32(w_uq[l])), "w_ukv": _klay(f32(w_ukv[l])),
            "w_ck1": np.ascontiguousarray(f32(w_cmp_k1[l]).reshape(32, 128, 128).transpose(1, 0, 2)), "w_ck2": f32(w_cmp_k2[l]),
            "w_cv1": np.ascontiguousarray(f32(w_cmp_v1[l]).reshape(32, 128, 128).transpose(1, 0, 2)), "w_cv2": f32(w_cmp_v2[l]),
            "pembT": np.ascontiguousarray(f32(cmp_pos_emb[l]).T),
            "w_out": _klay(f32(w_out[l])), "w_xq": _klay(f32(w_xq[l])), "w_xkv": _klay(f32(w_xkv[l])), "w_xo": _klay(f32(w_xo[l])),
            "w_up": _klay(f32(w_up[l])), "w_down": _klay(f32(w_down[l])),
        }
        for k_, v_ in lay.items():
            names.append((l, k_, v_.shape))
            arrs.append(v_.reshape(-1))
    flat = np.concatenate(arrs)
    del arrs
    flat_bf = cast_on_device(flat)
    del flat
    W = [dict() for _ in range(DEPTH)]
    off = 0
    for l, k_, shp in names:
        n = int(np.prod(shp))
        W[l][k_] = flat_bf[off:off + n].reshape(shp)
        off += n

    ident = np.eye(128, dtype=np.float32).astype(NPBF)
    inv128, inv64 = _rep(_inv_freq(128)), _rep(_inv_freq(64))
    NTC = S // 4
    hcur = x.copy()
    nc_p1 = _prog("p1", lambda: build_p1(NT=NTC))
    nc_a = _prog("a", lambda: build_A(S=S))
    nc_p3 = _prog("p3", lambda: build_p3(NT=NTC))
    cores = list(range(NCORES))
    for l in range(DEPTH):
        ims = []
        for c8 in cores:
            b, c = c8 // 4, c8 % 4
            sl = slice(c * NTC, (c + 1) * NTC)
            ims.append({"h": np.ascontiguousarray(hcur[b, sl]),
                        "posT": np.ascontiguousarray(positions[b, sl].reshape(NTC // 128, 128).T),
                        "inv128": inv128, "inv64": inv64, "g_pre": _rep(f32(g_mix_pre[l])), "w_in": W[l]["w_in"],
                        "g_q": _rep(f32(g_q_lora[l])), "g_kv": _rep(f32(g_kv_lora[l])), "w_uq": W[l]["w_uq"], "w_ukv": W[l]["w_ukv"],
                        "ident": ident})
        r1 = run_bass_kernel_spmd(nc_p1, ims, core_ids=cores).results
        del ims
        ims = []
        cw = {k_: W[l][k_] for k_ in ("w_ck1", "w_ck2", "w_cv1", "w_cv2", "pembT")}
        for b in range(B):
            FTb = np.concatenate([np.asarray(r1[4 * b + c]["FT"]) for c in range(4)], axis=2)
            VTb = np.concatenate([np.asarray(r1[4 * b + c]["VT"]) for c in range(4)], axis=0)
            GTb = np.concatenate([np.asarray(r1[4 * b + c]["GT"]) for c in range(4)], axis=0)
            for c in range(4):
                ims.append(a_inputs(FTb, VTb, GTb, positions[b], c, cw, S))
        del r1
        ra = run_bass_kernel_spmd(nc_a, ims, core_ids=cores).results
        del ims
        Ob = np.zeros((B, S, D), NPBF)
        for c8 in cores:
            b, c = c8 // 4, c8 % 4
            idx = np.concatenate([np.arange(2048 * i + 512 * c, 2048 * i + 512 * c + 512) for i in range(S // 2048)])
            Ob[b, idx] = np.asarray(ra[c8]["O"])
        del ra
        gl = [g_mix_post, g_mem_pre, g_mem_kv, g_mem_post, g_mlp_pre, g_mlp_post]
        gains = np.ascontiguousarray(np.stack([_rep(f32(g_[l])) for g_ in gl], 1))
        ims = []
        for c8 in cores:
            b, c = c8 // 4, c8 % 4
            sl = slice(c * NTC, (c + 1) * NTC)
            ims.append({"h": np.ascontiguousarray(hcur[b, sl]), "O": np.ascontiguousarray(Ob[b, sl]), "mem": np.ascontiguousarray(mem[b]),
                        "w_out": W[l]["w_out"], "w_xq": W[l]["w_xq"], "w_xkv": W[l]["w_xkv"], "w_xo": W[l]["w_xo"],
                        "w_up": W[l]["w_up"], "w_down": W[l]["w_down"], "gains": gains, "ident": ident})
        r3 = run_bass_kernel_spmd(nc_p3, ims, core_ids=cores).results
        del ims
        for c8 in cores:
            b, c = c8 // 4, c8 % 4
            hcur[b, c * NTC:(c + 1) * NTC] = np.asarray(r3[c8]["hout"])
        del r3
    return hcur
```

```python
import numpy as np
import ml_dtypes
from contextlib import ExitStack
import concourse.bass as bass
import concourse.mybir as mybir
from concourse.bass_utils import run_bass_kernel_spmd

F32 = mybir.dt.float32
BF16 = mybir.dt.bfloat16
I32 = mybir.dt.int32
ALU = mybir.AluOpType
AF = mybir.ActivationFunctionType
AX = mybir.AxisListType
NPBF = ml_dtypes.bfloat16

NCORES = 8


class T:
    __slots__ = ("name", "w", "rd", "excl")

    def __init__(self, name="", excl=False):
        self.name = name
        self.w = None
        self.rd = {}
        self.excl = excl


def PT():
    return T(excl=True)


class _Rec:
    def __getattr__(self, name):
        def f(*a, **kw):
            self.call = (name, a, kw)
            return self
        return f


class _Eng:
    def __init__(self, name):
        self.name = name
        self.q = []
        self.sem = None
        self.count = 0
        self.known = {}
        self.dsems = []
        self.drr = 0


class Prog:
    def __init__(self, n_dma_sems=20, dma_queues=("sp", "pool", "act")):
        self.nc = bass.Bass("TRN2", target_bir_lowering=False)
        self.es = ExitStack()
        self.sems = []
        self.semval = []
        self.E = {n: _Eng(n) for n in ("pe", "act", "dve", "pool", "sp")}
        for n in ("pe", "act", "dve", "pool"):
            self.E[n].sem = self._newsem("p_" + n)
        for qn in dma_queues:
            for i in range(n_dma_sems):
                self.E[qn].dsems.append(self._newsem("d_%s%d" % (qn, i)))
        self.out_tokens = []
        self.n_inst = 0

    def _newsem(self, name):
        h = self.es.enter_context(self.nc.semaphore(name))
        self.sems.append(h)
        self.semval.append(0)
        return len(self.sems) - 1

    def dram(self, name, shape, dt, kind):
        return self.nc.dram_tensor(name, list(shape), dt, kind=kind).ap()

    def sb(self, name, shape, dt):
        return self.es.enter_context(self.nc.sbuf_tensor(name, list(shape), dt))

    def ps(self, name, shape, dt=F32):
        return self.es.enter_context(self.nc.psum_tensor(name, list(shape), dt))

    def _deps(self, eng, reads, writes):
        need = {}

        def add(tok):
            if tok is None:
                return
            s, v = tok
            if need.get(s, 0) < v:
                need[s] = v

        for t in reads:
            add(t.w)
        for t in writes:
            add(t.w)
            for s, v in t.rd.items():
                add((s, v))
        out = []
        for s, v in need.items():
            if s == eng.sem and eng.name == "pe":
                continue
            if eng.known.get(s, 0) >= v:
                continue
            eng.known[s] = v
            out.append((s, v))
        return out

    def _emit_waits(self, eng, waits):
        for s, v in waits:
            h = self.sems[s]
            eng.q.append(lambda e, h=h, v=v: e.wait_ge(h, v))

    def op(self, en, fn, reads=(), writes=()):
        eng = self.E[en]
        ex = [t for t in reads if t.excl]
        if ex:
            reads = [t for t in reads if not t.excl]
            writes = list(writes) + ex
        waits = self._deps(eng, reads, writes)
        self._emit_waits(eng, waits)
        eng.count += 1
        c = eng.count
        h = self.sems[eng.sem]
        rec = _Rec()
        fn(rec)
        name, a, kw = rec.call
        eng.q.append(lambda e, name=name, a=a, kw=kw, h=h: getattr(e, name)(*a, **kw).then_inc(h, 1))
        tok = (eng.sem, c)
        for t in reads:
            if t.rd.get(eng.sem, 0) < c:
                t.rd[eng.sem] = c
        for t in writes:
            t.w = tok
            t.rd = {}
        self.n_inst += 1
        return tok

    def dma(self, qn, out, in_, reads=(), writes=(), is_output=False):
        eng = self.E[qn]
        k = eng.drr
        eng.drr = (k + 1) % len(eng.dsems)
        s = eng.dsems[k]
        waits = self._deps(eng, reads, writes)
        prev = self.semval[s]
        if prev > 0 and eng.known.get(s, 0) < prev:
            eng.known[s] = prev
            waits.append((s, prev))
        self._emit_waits(eng, waits)
        self.semval[s] = prev + 16
        v = prev + 16
        h = self.sems[s]
        eng.q.append(lambda e, out=out, in_=in_, h=h: e.dma_start(out=out, in_=in_).then_inc(h, 16))
        tok = (s, v)
        for t in reads:
            if t.rd.get(s, 0) < v:
                t.rd[s] = v
        for t in writes:
            t.w = tok
            t.rd = {}
        if is_output:
            self.out_tokens.append(tok)
        self.n_inst += 1
        return tok

    def finish(self):
        sp = self.E["sp"]
        fin = {}
        for s, v in self.out_tokens:
            fin[s] = max(fin.get(s, 0), v)
        for s in range(len(self.sems)):
            if self.semval[s] > 0:
                fin[s] = max(fin.get(s, 0), self.semval[s])
        for n in ("pe", "act", "dve", "pool"):
            e = self.E[n]
            if e.count:
                fin[e.sem] = e.count
        for s, v in fin.items():
            h = self.sems[s]
            sp.q.append(lambda e, h=h, v=v: e.wait_ge(h, v))
        with self.nc.Block() as block:
            @block.sync
            def _(e):
                for f in self.E["sp"].q:
                    f(e)

            @block.tensor
            def _(e):
                for f in self.E["pe"].q:
                    f(e)

            @block.scalar
            def _(e):
                for f in self.E["act"].q:
                    f(e)

            @block.vector
            def _(e):
                for f in self.E["dve"].q:
                    f(e)

            @block.gpsimd
            def _(e):
                for f in self.E["pool"].q:
                    f(e)
        self.es.close()
        return self.nc


def build_cast(F, CH=4096):
    P = Prog()
    x = P.dram("x", [128, F], F32, "ExternalInput")
    y = P.dram("y", [128, F], BF16, "ExternalOutput")
    NB = 3
    xin = [P.sb("xin%d" % i, [128, CH], F32) for i in range(NB)]
    xo = [P.sb("xo%d" % i, [128, CH], BF16) for i in range(NB)]
    tin = [T() for _ in range(NB)]
    to = [T() for _ in range(NB)]
    engs = ["dve", "pool", "act"]
    nch = (F + CH - 1) // CH
    for c in range(nch):
        b = c % NB
        w = min(CH, F - c * CH)
        P.dma("sp", xin[b][:, :w], x[:, c * CH:c * CH + w], writes=[tin[b]])
        en = engs[c % 3]
        if en == "act":
            P.op(en, lambda e, b=b, w=w: e.copy(xo[b][:, :w], xin[b][:, :w]), reads=[tin[b]], writes=[to[b]])
        else:
            P.op(en, lambda e, b=b, w=w: e.tensor_copy(xo[b][:, :w], xin[b][:, :w]), reads=[tin[b]], writes=[to[b]])
        P.dma("pool", y[:, c * CH:c * CH + w], xo[b][:, :w], reads=[to[b]], is_output=True)
    return P.finish()


def cast_on_device(flat):
    n = flat.size
    per = -(-n // (NCORES * 128))
    per = -(-per // 8) * 8
    pad = np.zeros(NCORES * 128 * per, np.float32)
    pad[:n] = flat
    shards = pad.reshape(NCORES, 128, per)
    nc = build_cast(per)
    res = run_bass_kernel_spmd(nc, [{"x": shards[i]} for i in range(NCORES)], core_ids=list(range(NCORES)))
    out = np.concatenate([np.asarray(r["y"]).reshape(-1) for r in res.results])
    return out[:n]


D = 2048
S = 16384
B = 2
DIN = 4684
EPS = 1e-6
TWO_PI = 6.283185307179586
CW1 = 6.28125
CW2 = TWO_PI - CW1
W_IN_PERM = np.concatenate([np.arange(0, 1280), np.arange(1292, 2316), np.arange(2380, 4684),
                            np.arange(2316, 2380), np.arange(1280, 1292)])
C_NQ, C_KC, C_VC, C_KS, C_VS, C_KW, C_VW = 0, 512, 640, 768, 896, 1024, 1152
C_CQ, C_CKV, C_DQ, C_DK, C_DV, C_KR, C_G = 1280, 1792, 2304, 3072, 3840, 4608, 4672
NFB = 36
NVB = 14


def _sincos(P, ang, sin_out, cos_out, y, ki, kf, r1, t_ang, t_out, t_scr):
    import math
    for which, dst in ((0, sin_out), (1, cos_out)):
        if which == 1:
            P.op("dve", lambda e: e.tensor_scalar(r1, ang, math.pi / 2, None, ALU.add), reads=[t_ang], writes=[t_scr])
            src = r1
        else:
            src = ang
        P.op("dve", lambda e, src=src: e.tensor_scalar(y, src, 1.0 / TWO_PI, None, ALU.mult), reads=[t_ang, t_scr], writes=[t_scr])
        P.op("dve", lambda e: e.tensor_copy(ki, y), reads=[t_scr], writes=[t_scr])
        P.op("dve", lambda e: e.tensor_copy(kf, ki), reads=[t_scr], writes=[t_scr])
        P.op("dve", lambda e, src=src: e.scalar_tensor_tensor(y, kf, -CW1, src, ALU.mult, ALU.add), reads=[t_scr, t_ang], writes=[t_scr])
        P.op("dve", lambda e: e.scalar_tensor_tensor(y, kf, -CW2, y, ALU.mult, ALU.add), reads=[t_scr], writes=[t_scr])
        P.op("dve", lambda e: e.tensor_scalar(y, y, -math.pi, math.pi, ALU.max, ALU.min), reads=[t_scr], writes=[t_scr])
        P.op("act", lambda e, dst=dst: e.activation(dst, y, AF.Sin), reads=[t_scr], writes=[t_out])


def _rope(P, en, src, dst, cos, sin, H, hd, tmp_a, tmp_b, rd, wr, t_tab, t_tmp):
    c = cos.unsqueeze(1).broadcast_to([128, H, hd])
    s = sin.unsqueeze(1).broadcast_to([128, H, hd])
    x1, x2 = src[:, :, 0:hd], src[:, :, hd:2 * hd]
    ta, tb = tmp_a[:, 0:H, 0:hd], tmp_b[:, 0:H, 0:hd]
    P.op(en, lambda e: e.tensor_tensor(ta, x1, c, ALU.mult), reads=rd + [t_tab], writes=[t_tmp])
    P.op(en, lambda e: e.tensor_tensor(tb, x2, s, ALU.mult), reads=rd + [t_tab], writes=[t_tmp])
    P.op(en, lambda e: e.tensor_tensor(dst[:, :, 0:hd], ta, tb, ALU.subtract), reads=[t_tmp], writes=wr)
    P.op(en, lambda e: e.tensor_tensor(ta, x2, c, ALU.mult), reads=rd + [t_tab], writes=[t_tmp])
    P.op(en, lambda e: e.tensor_tensor(tb, x1, s, ALU.mult), reads=rd + [t_tab], writes=[t_tmp])
    P.op(en, lambda e: e.tensor_tensor(dst[:, :, hd:2 * hd], ta, tb, ALU.add), reads=[t_tmp], writes=wr)


def _rstd(P, ss, rstd, n, epsb, rd, wr):
    P.op("act", lambda e: e.activation(rstd, ss, AF.Sqrt, bias=epsb, scale=1.0 / n), reads=rd, writes=wr)
    P.op("dve", lambda e: e.reciprocal(rstd, rstd), reads=wr, writes=wr)


def build_p1(NT=4096, ST=2):
    NTL = NT // 128
    NSUP = NTL // ST
    P = Prog()
    h = P.dram("h", [NT, D], F32, "ExternalInput")
    posT = P.dram("posT", [128, NTL], I32, "ExternalInput")
    inv128 = P.dram("inv128", [128, 64], F32, "ExternalInput")
    inv64 = P.dram("inv64", [128, 32], F32, "ExternalInput")
    g_pre = P.dram("g_pre", [128, D], F32, "ExternalInput")
    w_in = P.dram("w_in", [128, 16, DIN], BF16, "ExternalInput")
    g_q = P.dram("g_q", [128, 512], F32, "ExternalInput")
    g_kv = P.dram("g_kv", [128, 512], F32, "ExternalInput")
    w_uq = P.dram("w_uq", [128, 4, 1152], BF16, "ExternalInput")
    w_ukv = P.dram("w_ukv", [128, 4, 1536], BF16, "ExternalInput")
    ident = P.dram("ident", [128, 128], BF16, "ExternalInput")
    FT = P.dram("FT", [128, NFB, NT], BF16, "ExternalOutput")
    VT = P.dram("VT", [NT, NVB, 129], BF16, "ExternalOutput")
    GT = P.dram("GT", [NT, 12], F32, "ExternalOutput")

    s_g = P.sb("s_g", [128, D], F32); t_g = T()
    s_gq = P.sb("s_gq", [128, 512], F32)
    s_gkv = P.sb("s_gkv", [128, 512], F32)
    s_wuq = P.sb("s_wuq", [128, 4, 1152], BF16)
    s_wukv = P.sb("s_wukv", [128, 4, 1536], BF16)
    s_id = P.sb("s_id", [128, 128], BF16)
    s_pos = P.sb("s_pos", [128, NTL], I32)
    s_posf = P.sb("s_posf", [128, NTL], F32)
    s_inv128 = P.sb("s_inv128", [128, 64], F32)
    s_inv64 = P.sb("s_inv64", [128, 32], F32)
    epsb = P.sb("epsb", [128, 1], F32)
    t_c = T()
    for dst, src in ((s_g, g_pre), (s_gq, g_q), (s_gkv, g_kv), (s_wuq, w_uq), (s_wukv, w_ukv), (s_id, ident),
                     (s_pos, posT), (s_inv128, inv128), (s_inv64, inv64)):
        P.dma("sp", dst[:], src[:], writes=[t_c])
    P.op("dve", lambda e: e.memset(epsb[:], EPS), writes=[t_c])
    P.op("dve", lambda e: e.tensor_copy(s_posf[:], s_pos[:]), reads=[t_c], writes=[t_c])

    cos128 = P.sb("cos128", [128, NTL, 64], F32)
    sin128 = P.sb("sin128", [128, NTL, 64], F32)
    cos64 = P.sb("cos64", [128, NTL, 32], F32)
    sin64 = P.sb("sin64", [128, NTL, 32], F32)
    t_tab = T()
    pj = [P.sb("pj%d" % j, [128, DIN], F32) for j in range(ST)]
    t_pj = [T() for _ in range(ST)]
    s_ki = P.sb("s_ki", [128, NTL * 64], I32)
    t_ang, t_scr = T(), T()
    for hd, inv, co, si in ((64, s_inv128, cos128, sin128), (32, s_inv64, cos64, sin64)):
        n = NTL * hd
        ang = pj[0][:, 0:n].rearrange("p (t d) -> p t d", d=hd)
        yv = pj[0][:, 2048:2048 + n].rearrange("p (t d) -> p t d", d=hd)
        kfv = pj[1][:, 0:n].rearrange("p (t d) -> p t d", d=hd)
        r1v = pj[1][:, 2048:2048 + n].rearrange("p (t d) -> p t d", d=hd)
        kiv = s_ki[:, 0:n].rearrange("p (t d) -> p t d", d=hd)
        pb = s_posf[:].unsqueeze(2).broadcast_to([128, NTL, hd])
        ib = inv[:].unsqueeze(1).broadcast_to([128, NTL, hd])
        P.op("dve", lambda e, ang=ang, pb=pb, ib=ib: e.tensor_tensor(ang, pb, ib, ALU.mult), reads=[t_c, t_scr], writes=[t_ang])
        _sincos(P, ang, si[:], co[:], yv, kiv, kfv, r1v, t_ang, t_tab, t_scr)
    for j in range(ST):
        t_pj[j] = T()
        t_pj[j].rd = dict(t_tab.rd)
    t_pj[0].w = t_scr.w; t_pj[0].rd = dict(t_scr.rd); t_pj[0].rd.update(t_ang.rd)
    if ST > 1:
        t_pj[1].w = t_scr.w; t_pj[1].rd = dict(t_scr.rd)
    if t_ang.w is not None:
        s_, v_ = t_ang.w
        t_pj[0].rd[s_] = max(t_pj[0].rd.get(s_, 0), v_)

    hin = [P.sb("hin%d" % i, [128, D], F32) for i in range(2)]
    t_hin = [T(), T()]
    hn = P.sb("hn", [128, D], BF16); t_hn = T()
    ss = P.sb("ss", [128, 4], F32); t_ss = T()
    hnT = [P.sb("hnT%d" % j, [128, 16, 128], BF16) for j in range(ST)]
    t_hnT = [T() for _ in range(ST)]
    wg = [P.sb("wg%d" % i, [128, 16, 512], BF16) for i in range(2)]
    t_wg = [T(), T()]
    rb = P.sb("rb", [128, NFB, 128], BF16)
    t_rb = [T() for _ in range(9)]
    tsb = P.sb("tsb", [128, NFB, ST * 128], BF16); t_tsb = T()
    vt = P.sb("vt", [128, NVB, 129], BF16); t_vt = T()
    gsb = P.sb("gsb", [128, 12], F32); t_gsb = T()
    cqn = P.sb("cqn", [128, 2, 512], BF16); t_cqn = T()
    cT = P.sb("cT", [128, 2, 4, 128], BF16); t_cT = T()
    tmp_a = P.sb("tmp_a", [128, 12, 64], F32)
    tmp_b = P.sb("tmp_b", [128, 12, 64], F32)
    t_tmp = T()
    tmp_c = P.sb("tmp_c", [128, 1, 64], F32)
    tmp_d = P.sb("tmp_d", [128, 1, 64], F32)
    t_tmp2 = T()
    ps_tr = [P.ps("ps_tr%d" % i, [128, 4, 128], BF16) for i in range(2)]
    t_ps_tr = [PT(), PT()]
    ps_mm = [P.ps("ps_mm%d" % i, [128, 512], F32) for i in range(2)]
    t_ps_mm = [PT(), PT()]
    ps_ml = [P.ps("ps_ml%d" % i, [128, 512], F32) for i in range(2)]
    t_ps_ml = [PT(), PT()]

    P.op("pool", lambda e: e.memset(vt[:], 1.0), writes=[t_vt])
    P.op("pool", lambda e: e.memset(rb[:], 0.0), writes=t_rb)

    ngrp = (DIN + 511) // 512
    trc = [0]
    mmc = [0]
    mlc = [0]
    evc = [0]

    def transposes(src_blocks, dst_fn, rd, wr):
        k = trc[0] % 2
        trc[0] += 1
        for i, sa in enumerate(src_blocks):
            w = sa.shape[1]
            P.op("pe", lambda e, i=i, sa=sa, w=w, k=k: e.transpose(ps_tr[k][0:w, i, :], sa, s_id[:]),
                 reads=rd + [t_c], writes=[t_ps_tr[k]])
        en = "act" if evc[0] % 2 == 0 else "dve"
        evc[0] += 1
        dst, src = dst_fn(ps_tr[k])
        if en == "act":
            P.op("act", lambda e: e.copy(dst, src), reads=[t_ps_tr[k]], writes=wr)
        else:
            P.op("dve", lambda e: e.tensor_copy(dst, src), reads=[t_ps_tr[k]], writes=wr)

    import os
    LEVEL = float(os.environ.get("P1_LEVEL", "9"))
    PEN = "dve" if os.environ.get("P1_NOPOOL") else "pool"
    for st in range(NSUP if LEVEL >= 2 else 0):
        for j in range(ST):
            tl = st * ST + j
            b = tl % 2
            P.dma("sp", hin[b][:], h[tl * 128:(tl + 1) * 128, :], writes=[t_hin[b]])
            P.op("act", lambda e, b=b: e.activation(hn[:], hin[b][:], AF.Square, accum_out=ss[:, 0:1]),
                 reads=[t_hin[b]], writes=[t_hn, t_ss])
            _rstd(P, ss[:, 0:1], ss[:, 1:2], D, epsb[:], [t_ss, t_c], [t_ss])
            P.op("dve", lambda e, b=b: e.scalar_tensor_tensor(hn[:], hin[b][:], ss[:, 1:2], s_g[:], ALU.mult, ALU.mult),
                 reads=[t_hin[b], t_ss, t_c], writes=[t_hn])
            for g4 in range(4):
                transposes([hn[:, (4 * g4 + i) * 128:(4 * g4 + i + 1) * 128] for i in range(4)],
                           lambda pt, j=j, g4=g4: (hnT[j][:, 4 * g4:4 * g4 + 4, :], pt[:]),
                           [t_hn], [t_hnT[j]])
        for g in range(ngrp if LEVEL >= 3 else 0):
            n0 = g * 512
            nw = min(512, DIN - n0)
            wb = (st * ngrp + g) % 2
            P.dma("sp", wg[wb][:, :, 0:nw], w_in[:, :, n0:n0 + nw], writes=[t_wg[wb]])
            for j in range(ST):
                k = mmc[0] % 2
                mmc[0] += 1
                for kc in range(16):
                    P.op("pe", lambda e, k=k, j=j, kc=kc, wb=wb, nw=nw: e.matmul(
                        ps_mm[k][:, 0:nw], hnT[j][:, kc, :], wg[wb][:, kc, 0:nw], start=(kc == 0), stop=(kc == 15)),
                        reads=[t_hnT[j], t_wg[wb]], writes=[t_ps_mm[k]])
                P.op("act", lambda e, k=k, j=j, n0=n0, nw=nw: e.copy(pj[j][:, n0:n0 + nw], ps_mm[k][:, 0:nw]),
                     reads=[t_ps_mm[k]], writes=[t_pj[j]])
        for j in range(ST if LEVEL >= 4 else 0):
            tl = st * ST + j
            pjt = pj[j]
            c128, s128 = cos128[:, tl, :], sin128[:, tl, :]
            c64, s64 = cos64[:, tl, :], sin64[:, tl, :]
            rd = [t_pj[j]]

            def blk(b0, nb):
                return rb[:, b0:b0 + nb, :]

            def colv(c0, nb):
                return pjt[:, c0:c0 + nb * 128].rearrange("p (h d) -> p h d", d=128)
            _rope(P, "dve", colv(C_NQ, 4), blk(0, 4), c128, s128, 4, 64, tmp_a, tmp_b, rd, [t_rb[0]], t_tab, t_tmp)
            _rope(P, PEN, colv(C_KS, 1), blk(6, 1), c128, s128, 1, 64, tmp_c, tmp_d, rd, [t_rb[1]], t_tab, t_tmp2)
            _rope(P, PEN, colv(C_KW, 1), blk(7, 1), c128, s128, 1, 64, tmp_c, tmp_d, rd, [t_rb[1]], t_tab, t_tmp2)
            _rope(P, "dve", colv(C_DQ, 12), blk(20, 12), c128, s128, 12, 64, tmp_a, tmp_b, rd, [t_rb[5], t_rb[6], t_rb[7]], t_tab, t_tmp)
            if LEVEL < 4.2:
                continue
            P.op(PEN, lambda e: e.tensor_copy(blk(4, 2), colv(C_KC, 2)), reads=rd, writes=[t_rb[1]])
            _rope(P, PEN, pjt[:, C_KR:C_KR + 64].unsqueeze(1), rb[:, 35, 0:64].unsqueeze(1), c64, s64, 1, 32,
                  tmp_c, tmp_d, rd, [t_rb[8]], t_tab, t_tmp2)
            P.op(PEN, lambda e: e.tensor_copy(vt[:, 0, 0:128], pjt[:, C_VS:C_VS + 128]), reads=rd, writes=[t_vt])
            P.op(PEN, lambda e: e.tensor_copy(vt[:, 1, 0:128], pjt[:, C_VW:C_VW + 128]), reads=rd, writes=[t_vt])
            P.op(PEN, lambda e: e.tensor_copy(vt[:, 8:14, 0:128], colv(C_DV, 6)), reads=rd, writes=[t_vt])
            P.op("act", lambda e: e.activation(gsb[:], pjt[:, C_G:C_G + 12], AF.Sigmoid), reads=rd, writes=[t_gsb])
            P.dma("pool", GT[tl * 128:(tl + 1) * 128, :], gsb[:], reads=[t_gsb], is_output=True)
            if LEVEL < 4.3:
                continue
            for which, c0, gsbuf in ((0, C_CQ, s_gq), (1, C_CKV, s_gkv)):
                P.op("act", lambda e, which=which, c0=c0: e.activation(cqn[:, which, :], pjt[:, c0:c0 + 512], AF.Square,
                                                                     accum_out=ss[:, 2:3]),
                     reads=rd, writes=[t_cqn, t_ss])
                _rstd(P, ss[:, 2:3], ss[:, 3:4], 512, epsb[:], [t_ss, t_c], [t_ss])
                P.op("dve", lambda e, which=which, c0=c0, gsbuf=gsbuf: e.scalar_tensor_tensor(
                    cqn[:, which, :], pjt[:, c0:c0 + 512], ss[:, 3:4], gsbuf[:], ALU.mult, ALU.mult),
                    reads=rd + [t_ss, t_c], writes=[t_cqn])
                transposes([cqn[:, which, i * 128:(i + 1) * 128] for i in range(4)],
                           lambda pt, which=which: (cT[:, which, :, :], pt[:]), [t_cqn], [t_cT])
            if LEVEL < 4.4:
                continue
            for g in range(3):
                k = mlc[0] % 2
                mlc[0] += 1
                for kc in range(4):
                    P.op("pe", lambda e, k=k, kc=kc, g=g: e.matmul(ps_ml[k][:, 0:384], cT[:, 0, kc, :],
                                                                  s_wuq[:, kc, g * 384:(g + 1) * 384],
                                                                  start=(kc == 0), stop=(kc == 3)),
                         reads=[t_cT, t_c], writes=[t_ps_ml[k]])
                pv = ps_ml[k][:, 0:384].rearrange("p (h d) -> p h d", d=192)
                P.op("act", lambda e, pv=pv, g=g: e.copy(rb[:, 8 + 2 * g:10 + 2 * g, :], pv[:, :, 0:128]),
                     reads=[t_ps_ml[k]], writes=[t_rb[2], t_rb[3]])
                if LEVEL < 4.42:
                    continue
                qpe_dst = rb[:, 32:35, :].rearrange("p b (h d) -> p (b h) d", d=64)[:, 2 * g:2 * g + 2, :]
                _rope(P, "dve", pv[:, :, 128:192], qpe_dst, c64, s64, 2, 32, tmp_a, tmp_b, [t_ps_ml[k]], [t_rb[8]], t_tab, t_tmp)
            if LEVEL < 4.5:
                continue
            for g in range(3):
                k = mlc[0] % 2
                mlc[0] += 1
                for kc in range(4):
                    P.op("pe", lambda e, k=k, kc=kc, g=g: e.matmul(ps_ml[k][:, 0:512], cT[:, 1, kc, :],
                                                                  s_wukv[:, kc, g * 512:(g + 1) * 512],
                                                                  start=(kc == 0), stop=(kc == 3)),
                         reads=[t_cT, t_c], writes=[t_ps_ml[k]])
                pv = ps_ml[k][:, 0:512].rearrange("p (h d) -> p h d", d=256)
                P.op("act", lambda e, pv=pv, g=g: e.copy(rb[:, 14 + 2 * g:16 + 2 * g, :], pv[:, :, 0:128]),
                     reads=[t_ps_ml[k]], writes=[t_rb[3], t_rb[4]])
                P.op("dve", lambda e, pv=pv, g=g: e.tensor_copy(vt[:, 2 + 2 * g:4 + 2 * g, 0:128], pv[:, :, 128:256]),
                     reads=[t_ps_ml[k]], writes=[t_vt])
            P.dma("pool", VT[tl * 128:(tl + 1) * 128, :, :], vt[:], reads=[t_vt], is_output=True)
            for g4 in range(9 if LEVEL >= 5 else 0):
                srcs = [rb[:, 4 * g4 + i, :] for i in range(4)]
                if g4 == 8:
                    srcs[3] = rb[:, 35, 0:64]
                transposes(srcs, lambda pt, j=j, g4=g4: (tsb[:, 4 * g4:4 * g4 + 4, j * 128:(j + 1) * 128], pt[:]),
                           t_rb, [t_tsb])
        if LEVEL >= 6:
            P.dma("pool", FT[:, :, st * ST * 128:(st + 1) * ST * 128], tsb[:], reads=[t_tsb], is_output=True)
    return P.finish()


NEG = -30000.0
SC128 = 128 ** -0.5
SC192 = 192 ** -0.5


def build_A(S=16384):
    import math
    NI = S // 2048
    NTQ = NI * 512
    NCC = S // 2048
    NCMP = S // 16
    P = Prog(n_dma_sems=12, dma_queues=("sp", "pool"))
    di = lambda n, sh, dt=BF16: P.dram(n, sh, dt, "ExternalInput")
    QaT = di("QaT", [128, 4, NTQ]); QnT = di("QnT", [128, 6, NTQ]); QpeT = di("QpeT", [64, 6, NTQ])
    DqT = di("DqT", [128, 6, NTQ]); GTd = di("GT", [NTQ, 12], F32)
    KsT = di("KsT", [128, S]); Vs = di("Vs", [S // 2048, 128, 16, 129]); KnT = di("KnT", [128, 6, S]); KpeT = di("KpeT", [64, S])
    Vm = di("Vm", [6, S // 2048, 128, 16, 129]); KcrT = di("KcrT", [128, S]); VcrT = di("VcrT", [128, S])
    KwL = di("KwL", [128, NI, 1024]); VwL = di("VwL", [NI, 1024, 129])
    DkL = di("DkL", [128, NI, 6, 2560]); DvL = di("DvL", [NI, 2560, 6, 129])
    w_ck1 = di("w_ck1", [128, 32, 128]); w_ck2 = di("w_ck2", [128, 128])
    w_cv1 = di("w_cv1", [128, 32, 128]); w_cv2 = di("w_cv2", [128, 128])
    pembT = di("pembT", [128, 32]); cposT = di("cposT", [128, NCC], I32); inv128 = di("inv128", [128, 64], F32)
    caus = di("caus", [128, 4, 2048]); cmask = di("cmask", [128, 4, 128]); fmask = di("fmask", [128, 4, 64], F32)
    wm = di("wm", [128, 5, 128]); dm = di("dm", [128, 17, 128]); ident = di("ident", [128, 128]); I4d = di("I4", [128, 512])
    O = P.dram("O", [NTQ, 2048], BF16, "ExternalOutput")

    t_c = T()
    def cload(name, src, shape, dt=BF16):
        t = P.sb(name, shape, dt)
        P.dma("sp", t[:], src[:], writes=[t_c])
        return t
    s_caus = cload("s_caus", caus, [128, 4, 2048]); s_cmask = cload("s_cmask", cmask, [128, 4, 128])
    s_fmask = cload("s_fmask", fmask, [128, 4, 64], F32); s_wm = cload("s_wm", wm, [128, 5, 128])
    s_dm = cload("s_dm", dm, [128, 17, 128]); s_id = cload("s_id", ident, [128, 128]); s_I4 = cload("s_I4", I4d, [128, 512])
    s_w1 = [cload("s_wk1", w_ck1, [128, 32, 128]), cload("s_wv1", w_cv1, [128, 32, 128])]
    s_w2 = [cload("s_wk2", w_ck2, [128, 128]), cload("s_wv2", w_cv2, [128, 128])]
    s_pemb = cload("s_pemb", pembT, [128, 32]); s_cpos = cload("s_cpos", cposT, [128, NCC], I32)
    s_inv = cload("s_inv", inv128, [128, 64], F32)
    tiny = P.sb("tiny", [128, 1], F32)
    P.op("dve", lambda e: e.memset(tiny[:], 0.0), writes=[t_c])

    ps_s = [P.ps("ps_s%d" % i, [128, 512]) for i in range(2)]; t_ps_s = [PT(), PT()]
    ps_o = [P.ps("ps_o%d" % i, [128, 512]) for i in range(4)]; t_ps_o = [PT() for _ in range(4)]
    ps_c = [P.ps("ps_c%d" % i, [128, 512]) for i in range(2)]; t_ps_c = [PT(), PT()]

    selx = P.sb("selx", [128, S], BF16); t_selx = T()
    kct = P.sb("kct", [128, NCMP], BF16); t_kct = T()
    vca = P.sb("vca", [128, NCC, 129], BF16); t_vca = T()
    pt = [P.sb("pt%d" % i, [128, 512], BF16) for i in range(2)]; t_pt = [T(), T()]
    ptc = [0]
    ssc = [0]

    def score(terms, rd, pairs=False):
        k = ssc[0] % 2
        ssc[0] += 1
        n = len(terms)
        for idx, (of, l, r) in enumerate(terms):
            o = of(ps_s[k])
            st_ = (idx % 2 == 0) if pairs else (idx == 0)
            sp_ = (idx % 2 == 1) if pairs else (idx == n - 1)
            P.op("pe", lambda e, o=o, l=l, r=r, st_=st_, sp_=sp_: e.matmul(o, l, r, start=st_, stop=sp_, skip_group_check=True),
                 reads=rd, writes=[t_ps_s[k]])
        return k

    def pipeline(seq):
        n = len(seq)

        def do_score(t):
            it = seq[t]
            if it.get("pre"):
                it["pre"]()
            return score(it["terms"](), it["rd"](), it.get("pairs", False))
        k_next = do_score(0)
        for t in range(n):
            k_cur = k_next
            if t + 1 < n:
                k_next = do_score(t + 1)
            j = expo(k_cur, seq[t]["ncols"], seq[t]["scale"])
            seq[t]["pv"](j)

    def expo(k, ncols, scale):
        j = ptc[0] % 2
        ptc[0] += 1
        P.op("act", lambda e: e.activation(pt[j][:, 0:ncols], ps_s[k][:, 0:ncols], AF.Exp, scale=scale),
             reads=[t_ps_s[k]], writes=[t_pt[j]])
        return j

    kcr = selx
    hidT = P.sb("hidT", [128, 512], BF16); t_hid = T()
    gx = [P.sb("gx%d" % i, [128, 512], F32) for i in range(3)]; t_gx = T()
    cb = P.sb("cb", [128, 2], F32); t_cb = T()
    ctab = P.sb("ctab", [128, 2, NCC, 64], F32); t_ctab = T()
    pexp = P.sb("pexp", [128, 4, 1024], F32); t_pexp = T()
    cscr = pexp[:, :, 0:NCC * 64]; t_cscr = t_pexp
    cki = P.sb("cki", [128, NCC * 64], I32)
    cposf = P.sb("cposf", [128, NCC], F32)
    P.op("dve", lambda e: e.tensor_copy(cposf[:], s_cpos[:]), reads=[t_c], writes=[t_cscr])
    angv = cscr[:, 0, :].rearrange("p (t d) -> p t d", d=64)
    P.op("dve", lambda e: e.tensor_tensor(angv, cposf[:].unsqueeze(2).broadcast_to([128, NCC, 64]),
                                          s_inv[:].unsqueeze(1).broadcast_to([128, NCC, 64]), ALU.mult),
         reads=[t_c, t_cscr], writes=[t_cscr])
    t_ang = T(); t_ang.w = t_cscr.w
    vv = lambda i: cscr[:, i, :].rearrange("p (t d) -> p t d", d=64)
    _sincos(P, angv, ctab[:, 1], ctab[:, 0], vv(1), cki[:].rearrange("p (t d) -> p t d", d=64), vv(2), vv(3), t_ang, t_ctab, t_cscr)
    kc_tm = P.sb("kc_tm", [128, 128], BF16); t_kctm = T()
    ra = P.sb("ra", [128, 1, 64], F32); rb_ = P.sb("rb_", [128, 1, 64], F32); t_rt = T()
    ps_tr = P.ps("ps_trA", [128, 128], BF16) if False else None
    P.op("pool", lambda e: e.memset(vca[:], 1.0), writes=[t_vca])
    for which, src in ((0, KcrT), (1, VcrT)):
        P.dma("sp", selx[:, 0:S], src[:, 0:S], writes=[t_selx])
        for i in range(32):
            P.op("pe", lambda e, i=i: e.matmul(ps_c[0][:, 0:1], s_w1[which][:, i, :], s_pemb[:, i:i + 1],
                                               start=(i == 0), stop=(i == 31)), reads=[t_c], writes=[t_ps_c[0]])
        P.op("act", lambda e: e.copy(cb[:, which:which + 1], ps_c[0][:, 0:1]), reads=[t_ps_c[0]], writes=[t_cb])
        for c0 in range(0, NCMP, 512):
            cw = min(512, NCMP - c0)
            for i in range(32):
                lo = 16 * c0 + i
                n_in = min(cw, (S - lo + 15) // 16)
                rhs_main = selx[:, lo:lo + 16 * (n_in - 1) + 1:16]
                P.op("pe", lambda e, i=i, rhs_main=rhs_main, n_in=n_in: e.matmul(
                    ps_c[1][:, 0:n_in], s_w1[which][:, i, :], rhs_main, start=(i == 0), stop=(i == 31),
                    skip_group_check=True), reads=[t_selx, t_c], writes=[t_ps_c[1]])
            x, x2, u = gx[0][:, 0:cw], gx[1][:, 0:cw], gx[2][:, 0:cw]
            P.op("act", lambda e, x=x, cw=cw: e.activation(x, ps_c[1][:, 0:cw], AF.Identity, bias=cb[:, which:which + 1]),
                 reads=[t_ps_c[1], t_cb], writes=[t_gx])
            P.op("dve", lambda e, x=x, x2=x2: e.tensor_tensor(x2, x, x, ALU.mult), reads=[t_gx], writes=[t_gx])
            P.op("dve", lambda e, x2=x2: e.tensor_scalar(x2, x2, 0.044715, 1.0, ALU.mult, ALU.add), reads=[t_gx], writes=[t_gx])
            P.op("dve", lambda e, x=x, x2=x2, u=u: e.tensor_tensor(u, x2, x, ALU.mult), reads=[t_gx], writes=[t_gx])
            P.op("act", lambda e, u=u: e.activation(u, u, AF.Tanh, scale=0.7978845608028654), reads=[t_gx], writes=[t_gx])
            P.op("dve", lambda e, u=u: e.tensor_scalar(u, u, 1.0, 0.5, ALU.add, ALU.mult), reads=[t_gx], writes=[t_gx])
            P.op("dve", lambda e, u=u, x=x, cw=cw: e.tensor_tensor(hidT[:, 0:cw], u, x, ALU.mult), reads=[t_gx], writes=[t_hid])
            for cc in range(cw // 128):
                gch = (c0 + cc * 128) // 128
                P.op("pe", lambda e, cc=cc: e.matmul(ps_c[0][:, 0:128], hidT[:, cc * 128:(cc + 1) * 128], s_w2[which][:],
                                                     start=True, stop=True), reads=[t_hid, t_c], writes=[t_ps_c[0]])
                if which == 0:
                    _rope(P, "dve", ps_c[0][:, 0:128].unsqueeze(1), kc_tm[:].unsqueeze(1), ctab[:, 0, gch, :], ctab[:, 1, gch, :],
                          1, 64, ra, rb_, [t_ps_c[0]], [t_kctm], t_ctab, t_rt)
                    P.op("pe", lambda e: e.transpose(ps_s[0][:, 0:128].bitcast(BF16)[:, 0:128], kc_tm[:], s_id[:]),
                         reads=[t_kctm, t_c], writes=[t_ps_s[0]])
                    P.op("act", lambda e, gch=gch: e.copy(kct[:, gch * 128:(gch + 1) * 128], ps_s[0][:, 0:128].bitcast(BF16)[:, 0:128]),
                         reads=[t_ps_s[0]], writes=[t_kct])
                else:
                    P.op("act", lambda e, gch=gch: e.copy(vca[:, gch, 0:128], ps_c[0][:, 0:128]), reads=[t_ps_c[0]], writes=[t_vca])

    qa = P.sb("qa", [128, 4, 512], BF16); qn = P.sb("qn", [128, 6, 512], BF16); qpe = P.sb("qpe", [64, 6, 512], BF16)
    dq = P.sb("dq", [128, 6, 512], BF16); gts = P.sb("gts", [128, 4, 12], F32); t_q = T()
    obuf = P.sb("obuf", [128, 4, 2048], BF16); t_ob = T()
    kg = [P.sb("kg%d" % i, [128, 2048], BF16) for i in range(2)]
    kpg = [P.sb("kpg%d" % i, [64, 2048], BF16) for i in range(2)]
    vg = [P.sb("vg%d" % i, [128, 16, 129], BF16) for i in range(2)]
    t_kv = [T(), T()]
    kvc = [0]
    kwl = P.sb("kwl", [128, 1024], BF16); vwl = P.sb("vwl", [128, 8, 129], BF16); t_wl = T()
    dkl = [P.sb("dkl%d" % i_, [128, 2560], BF16) for i_ in range(2)]
    dvl = [P.sb("dvl%d" % i_, [128, 20, 129], BF16) for i_ in range(2)]; t_dl = [T(), T()]
    psm = P.sb("psm", [128, 1024], F32); t_psm = T()
    rs = P.sb("rs", [128, 8], F32); rsum = P.sb("rsum", [128, 4], F32); t_rs = T()
    imp = P.sb("imp", [128, 256], F32); imp2 = P.sb("imp2", [128, 256], F32); t_imp = T()
    m8 = P.sb("m8", [128, 16], F32); t_m8 = T()
    selb = P.sb("selb", [128, 256], BF16); t_selb = T()
    dn = P.sb("dn", [128, 4], F32); coef = P.sb("coef", [128, 4], F32); t_dn = T()
    acc = P.sb("acc", [128, 4, 128], F32); t_acc = T()
    tinyv = 1e-30

    def full(n):
        return lambda ps: ps[:, 0:n]

    def full3(ps):
        return ps[:, 0:512].rearrange("p (h q) -> p h q", h=4)

    def cols(c0, n):
        return lambda ps: ps[:, c0:c0 + n]

    def evac_norm(r_or_h, dst):
        x = r_or_h
        P.op("dve", lambda e: e.tensor_scalar(dn[:, x:x + 1], ps_o[x][:, 128:129], tinyv, None, ALU.max),
             reads=[t_ps_o[x]], writes=[t_dn])
        P.op("dve", lambda e: e.reciprocal(dn[:, x:x + 1], dn[:, x:x + 1]), reads=[t_dn], writes=[t_dn])
        P.op("act", lambda e: e.activation(dst, ps_o[x][:, 0:128], AF.Copy, scale=dn[:, x:x + 1]),
             reads=[t_ps_o[x], t_dn], writes=[t_ob])

    for i in range(NI):
        q0 = i * 512
        P.dma("sp", qa[:], QaT[:, :, q0:q0 + 512], writes=[t_q])
        P.dma("sp", qn[:], QnT[:, :, q0:q0 + 512], writes=[t_q])
        P.dma("sp", qpe[:], QpeT[:, :, q0:q0 + 512], writes=[t_q])
        P.dma("sp", dq[:], DqT[:, :, q0:q0 + 512], writes=[t_q])
        P.dma("sp", gts[:], GTd[q0:q0 + 512, :].rearrange("(r p) g -> p r g", p=128), writes=[t_q])
        P.dma("pool", kwl[:], KwL[:, i, :], writes=[t_wl])
        P.dma("pool", vwl[:], VwL[i].rearrange("(c p) x -> p c x", p=128), writes=[t_wl])
        Ci = 128 * (i + 1)
        J = Ci // 4

        for h in range(6):
            seq = []
            for p in range(i + 1):
                st = {}

                def pre(st=st, p=p, h=h):
                    b = kvc[0] % 2
                    kvc[0] += 1
                    st["b"] = b
                    g0 = p * 2048
                    P.dma("sp", kg[b][:], KnT[:, h, g0:g0 + 2048], writes=[t_kv[b]])
                    P.dma("sp", kpg[b][:], KpeT[:, g0:g0 + 2048], writes=[t_kv[b]])
                    P.dma("pool", vg[b][:], Vm[h, p], writes=[t_kv[b]])
                for kc in range(16):
                    def terms(st=st, kc=kc, p=p, h=h):
                        b = st["b"]
                        ks = slice(kc * 128, (kc + 1) * 128)
                        tt = [(full(512), kg[b][:, ks], qn[:, h, :]), (full(512), kpg[b][:, ks], qpe[:, h, :])]
                        if p == i:
                            for r in range(4):
                                tt.append((cols(r * 128, 128), s_caus[:, r, ks], s_id[:]))
                        return tt

                    def pv(j, st=st, kc=kc, p=p):
                        b = st["b"]
                        first = (p == 0 and kc == 0)
                        last = (p == i and kc == 15)
                        for r in range(4):
                            P.op("pe", lambda e, r=r: e.matmul(ps_o[r][:, 0:129], pt[j][:, r * 128:(r + 1) * 128],
                                                               vg[b][:, kc, :], start=first, stop=last),
                                 reads=[t_pt[j], t_kv[b]], writes=[t_ps_o[r]])
                    seq.append(dict(pre=pre if kc == 0 else None, terms=terms, rd=lambda st=st: [t_kv[st["b"]], t_q, t_c],
                                    ncols=512, scale=SC192, pv=pv))
            pipeline(seq)
            for r in range(4):
                evac_norm(r, obuf[:, r, 512 + h * 128:512 + (h + 1) * 128])

        for h in range(6):
            db = h % 2
            P.dma("sp", dkl[db][:], DkL[:, i, h, :], writes=[t_dl[db]])
            P.dma("pool", dvl[db][:], DvL[i, :, h, :].rearrange("(c p) x -> p c x", p=128), writes=[t_dl[db]])
            for r in range(4):
                rq = slice(r * 128, (r + 1) * 128)
                x = r
                us = list(range(r, r + 17))
                seq = []
                for b0 in range(0, 17, 4):
                    ub = us[b0:b0 + 4]

                    def terms(ub=ub, r=r, rq=rq, h=h, db=db):
                        tt = []
                        for jj, u in enumerate(ub):
                            tt.append((cols(jj * 128, 128), dkl[db][:, u * 128:(u + 1) * 128], dq[:, h, rq]))
                            tt.append((cols(jj * 128, 128), s_dm[:, 16 - (u - r), :], s_id[:]))
                        return tt

                    def pv(j, ub=ub, r=r, x=x, db=db):
                        for jj, u in enumerate(ub):
                            P.op("pe", lambda e, jj=jj, u=u: e.matmul(ps_o[x][:, 0:129], pt[j][:, jj * 128:(jj + 1) * 128], dvl[db][:, u, :],
                                                                     start=(u == r), stop=(u == r + 16)),
                                 reads=[t_pt[j], t_dl[db]], writes=[t_ps_o[x]])
                    seq.append(dict(pre=None, terms=terms, rd=lambda db=db: [t_dl[db], t_q, t_c], ncols=128 * len(ub), scale=SC128,
                                    pv=pv, pairs=True))
                pipeline(seq)
                evac_norm(x, obuf[:, r, 1280 + h * 128:1280 + (h + 1) * 128])

        for r in range(4):
            rq = slice(r * 128, (r + 1) * 128)
            qa3 = qa[:, :, rq]
            P.op("dve", lambda e: e.memset(rs[:], 0.0), writes=[t_rs])
            nh = (Ci + 511) // 512
            mh, mo = (Ci - 128) // 512, (Ci - 128) % 512
            for h in range(4):
                for hf in range(nh):
                    cw = min(512, Ci - 512 * hf)
                    P.op("pe", lambda e, hf=hf, cw=cw, h=h: e.matmul(ps_c[hf][:, 0:cw], qa[:, h, rq], kct[:, 512 * hf:512 * hf + cw],
                                                                    start=True, stop=(hf != mh), skip_group_check=True),
                         reads=[t_q, t_kct], writes=[t_ps_c[hf]])
                P.op("pe", lambda e, r=r: e.matmul(ps_c[mh][:, mo:mo + 128], s_id[:], s_cmask[:, r, :], start=False, stop=True,
                                                   skip_group_check=True), reads=[t_c], writes=[t_ps_c[mh]])
                for hf in range(nh):
                    cw = min(512, Ci - 512 * hf)
                    P.op("act", lambda e, hf=hf, cw=cw, h=h: e.activation(pexp[:, h, 512 * hf:512 * hf + cw], ps_c[hf][:, 0:cw], AF.Exp,
                                                                         scale=SC128, accum_out=rs[:, 2 * h + hf:2 * h + hf + 1]),
                         reads=[t_ps_c[hf], t_rs], writes=[t_pexp, t_rs])
            P.op("dve", lambda e: e.tensor_reduce(rsum[:], rs[:].rearrange("p (h t) -> p h t", t=2), AX.X, ALU.add),
                 reads=[t_rs], writes=[t_rs])
            P.op("dve", lambda e: e.tensor_scalar(rsum[:], rsum[:], tinyv, None, ALU.max), reads=[t_rs], writes=[t_rs])
            P.op("dve", lambda e: e.reciprocal(rsum[:], rsum[:]), reads=[t_rs], writes=[t_rs])
            P.op("dve", lambda e: e.tensor_scalar(psm[:, 0:Ci], pexp[:, 0, 0:Ci], rsum[:, 0:1], None, ALU.mult),
                 reads=[t_pexp, t_rs], writes=[t_psm])
            for h in range(1, 4):
                P.op("dve", lambda e, h=h: e.scalar_tensor_tensor(psm[:, 0:Ci], pexp[:, h, 0:Ci], rsum[:, h:h + 1], psm[:, 0:Ci],
                                                                  ALU.mult, ALU.add), reads=[t_pexp, t_rs, t_psm], writes=[t_psm])
            pv4 = psm[:, 0:Ci].rearrange("p (j k) -> p j k", k=4)
            P.op("dve", lambda e: e.tensor_reduce(imp[:, 0:J], pv4, AX.X, ALU.add), reads=[t_psm], writes=[t_imp])
            P.op("dve", lambda e: e.tensor_tensor(imp[:, 1:J], imp[:, 1:J], pv4[:, 0:J - 1, 3], ALU.add), reads=[t_psm, t_imp], writes=[t_imp])
            P.op("dve", lambda e: e.tensor_scalar(imp[:, 0:1], imp[:, 0:1], 1e9, None, ALU.add), reads=[t_imp], writes=[t_imp])
            if i >= 1:
                P.op("dve", lambda e, r=r: e.tensor_tensor(imp[:, J - 64:J], imp[:, J - 64:J], s_fmask[:, r, :], ALU.add),
                     reads=[t_imp, t_c], writes=[t_imp])
            else:
                P.op("dve", lambda e, r=r: e.tensor_tensor(imp[:, 0:32], imp[:, 0:32], s_fmask[:, r, 32:64], ALU.add),
                     reads=[t_imp, t_c], writes=[t_imp])
            P.op("dve", lambda e: e.max(m8[:, 0:8], imp[:, 0:J]), reads=[t_imp], writes=[t_m8])
            P.op("dve", lambda e: e.match_replace(imp2[:, 0:J], m8[:, 0:8], imp[:, 0:J], -3e9), reads=[t_imp, t_m8], writes=[t_imp])
            P.op("dve", lambda e: e.max(m8[:, 8:16], imp2[:, 0:J]), reads=[t_imp], writes=[t_m8])
            P.op("dve", lambda e: e.tensor_scalar(selb[:, 0:J], imp[:, 0:J], m8[:, 15:16], NEG, ALU.is_lt, ALU.mult),
                 reads=[t_imp, t_m8], writes=[t_selb])
            P.op("pool", lambda e: e.tensor_copy(selx[:, 0:64 * J].rearrange("p (j k) -> p j k", k=64),
                                                 selb[:, 0:J].unsqueeze(2).broadcast_to([128, J, 64])),
                 reads=[t_selb], writes=[t_selx])
            P.op("pool", lambda e, r=r: e.tensor_tensor(selx[:, 64 * J - 2048:64 * J], selx[:, 64 * J - 2048:64 * J], s_caus[:, r, :], ALU.add),
                 reads=[t_selx, t_c], writes=[t_selx])

            def branch_evac(br, last_branch):
                for h in range(4):
                    P.op("dve", lambda e, h=h: e.tensor_scalar(dn[:, h:h + 1], ps_o[h][:, 128:129], tinyv, None, ALU.max),
                         reads=[t_ps_o[h]], writes=[t_dn])
                P.op("dve", lambda e: e.reciprocal(dn[:], dn[:]), reads=[t_dn], writes=[t_dn])
                gv = gts[:, r, :].rearrange("p (h t) -> p h t", t=3)[:, :, br]
                P.op("dve", lambda e: e.tensor_tensor(coef[:], dn[:], gv, ALU.mult), reads=[t_dn, t_q], writes=[t_dn])
                for h in range(4):
                    dst = obuf[:, r, h * 128:(h + 1) * 128] if last_branch else acc[:, h, :]
                    wr = [t_ob] if last_branch else [t_acc]
                    if br == 0:
                        P.op("dve", lambda e, h=h, dst=dst: e.tensor_scalar(dst, ps_o[h][:, 0:128], coef[:, h:h + 1], None, ALU.mult),
                             reads=[t_ps_o[h], t_dn], writes=wr)
                    else:
                        P.op("dve", lambda e, h=h, dst=dst: e.scalar_tensor_tensor(dst, ps_o[h][:, 0:128], coef[:, h:h + 1], acc[:, h, :],
                                                                                 ALU.mult, ALU.add),
                             reads=[t_ps_o[h], t_dn, t_acc], writes=wr)

            def pv4h(j, vrhs, first, last, rd):
                for h in range(4):
                    P.op("pe", lambda e, h=h: e.matmul(ps_o[h][:, 0:129], pt[j][:, h * 128:(h + 1) * 128], vrhs, start=first, stop=last),
                         reads=[t_pt[j]] + rd, writes=[t_ps_o[h]])

            seq = []
            for cc in range(i + 1):
                def terms(cc=cc, r=r):
                    tt = [(full3, kct[:, cc * 128:(cc + 1) * 128], qa3)]
                    if cc == i:
                        tt.append((full(512), s_cmask[:, r, :], s_I4[:]))
                    return tt
                seq.append(dict(pre=None, terms=terms, rd=lambda: [t_kct, t_q, t_c], ncols=512, scale=SC128,
                                pv=lambda j, cc=cc: pv4h(j, vca[:, cc, :], cc == 0, cc == i, [t_vca])))
            pipeline(seq)
            branch_evac(0, False)
            seq = []
            for o in range(5):
                def terms(o=o, r=r):
                    u = r + o
                    tt = [(full3, kwl[:, u * 128:(u + 1) * 128], qa3)]
                    if o in (0, 4):
                        tt.append((full(512), s_wm[:, o, :], s_I4[:]))
                    return tt
                seq.append(dict(pre=None, terms=terms, rd=lambda: [t_wl, t_q, t_c], ncols=512, scale=SC128,
                                pv=lambda j, o=o, r=r: pv4h(j, vwl[:, r + o, :], o == 0, o == 4, [t_wl])))
            pipeline(seq)
            branch_evac(2, False)

            seq = []
            for p in range(i + 1):
                st = {}

                def pre(st=st, p=p):
                    b = kvc[0] % 2
                    kvc[0] += 1
                    st["b"] = b
                    g0 = p * 2048
                    P.dma("sp", kg[b][:], KsT[:, g0:g0 + 2048], writes=[t_kv[b]])
                    P.dma("pool", vg[b][:], Vs[p], writes=[t_kv[b]])
                for kc in range(16):
                    def terms(st=st, kc=kc, p=p):
                        b = st["b"]
                        g0 = p * 2048
                        ks = slice(kc * 128, (kc + 1) * 128)
                        return [(full3, kg[b][:, ks], qa3), (full(512), selx[:, g0 + kc * 128:g0 + (kc + 1) * 128], s_I4[:])]
                    seq.append(dict(pre=pre if kc == 0 else None, terms=terms, rd=lambda st=st: [t_kv[st["b"]], t_q, t_c, t_selx],
                                    ncols=512, scale=SC128,
                                    pv=lambda j, st=st, kc=kc, p=p: pv4h(j, vg[st["b"]][:, kc, :], p == 0 and kc == 0, p == i and kc == 15,
                                                                       [t_kv[st["b"]]])))
            pipeline(seq)
            branch_evac(1, True)
        for r in range(4):
            P.dma("pool", O[(i * 4 + r) * 128:(i * 4 + r + 1) * 128, :], obuf[:, r, :], reads=[t_ob], is_output=True)
    return P.finish()


def _rep(v):
    return np.ascontiguousarray(np.broadcast_to(np.asarray(v)[None, :], (128, np.asarray(v).size)))


def _inv_freq(dim):
    return (10000.0 ** (-np.arange(0, dim, 2, dtype=np.float32) / dim)).astype(np.float32)


def a_core_masks(c):
    q = np.arange(128)[:, None, None]
    r = np.arange(4)[None, :, None]
    kl = np.arange(2048)[None, None, :]
    caus = np.where(kl <= 512 * c + 128 * r + q, 0.0, NEG).astype(np.float32)
    cl = np.arange(128)[None, None, :]
    cmask = np.where(16 * cl + 31 <= 512 * c + 128 * r + q, 0.0, NEG).astype(np.float32)
    jl = np.arange(-32, 32)[None, None, :]
    cur = 8 * c + 2 * r + (q >= 64)
    fmask = np.where((jl == cur) | (jl == cur - 1), 1e9, np.where(jl > cur, -1e9, 0.0)).astype(np.float32)
    return caus.astype(NPBF), cmask.astype(NPBF), fmask


def a_const_masks():
    q = np.arange(128)[:, None]
    k = np.arange(128)[None, :]
    wm = np.zeros((128, 5, 128), np.float32)
    wm[:, 0, :] = np.where(k >= q, 0.0, NEG)
    wm[:, 4, :] = np.where(k <= q, 0.0, NEG)
    dm = np.zeros((128, 17, 128), np.float32)
    for dc in range(17):
        d = 128 * dc + q - k
        mult = ((d >= 0) & (d <= 128)).astype(np.int64) + ((d >= 0) & (d % 4 == 0) & (d <= 512)) + ((d >= 0) & (d % 16 == 0) & (d <= 2048))
        dm[:, dc, :] = np.where(mult > 0, np.log(np.maximum(mult, 1)) / SC128, NEG)
    ident = np.eye(128, dtype=np.float32)
    I4 = np.tile(ident, (1, 4))
    return wm.astype(NPBF), dm.astype(NPBF), ident.astype(NPBF), I4.astype(NPBF)


def a_inputs(FTb, VTb, GTb, posb, c, cw, S):
    NI = S // 2048
    idx = np.concatenate([np.arange(2048 * i + 512 * c, 2048 * i + 512 * c + 512) for i in range(NI)])
    z = lambda *sh: np.zeros(sh, NPBF)
    m = {}
    m["QaT"] = FTb[:, 0:4][:, :, idx]
    m["QnT"] = FTb[:, 8:14][:, :, idx]
    qpe = np.stack([FTb[(h % 2) * 64:(h % 2) * 64 + 64, 32 + h // 2] for h in range(6)], 1)
    m["QpeT"] = qpe[:, :, idx]
    m["DqT"] = FTb[:, 20:26][:, :, idx]
    m["GT"] = GTb[idx]
    m["KsT"] = FTb[:, 6]; m["KnT"] = FTb[:, 14:20]; m["KpeT"] = FTb[0:64, 35]
    m["Vs"] = VTb[:, 0].reshape(S // 2048, 16, 128, 129).transpose(0, 2, 1, 3)
    m["Vm"] = VTb[:, 2:8].reshape(S // 2048, 16, 128, 6, 129).transpose(3, 0, 2, 1, 4)
    m["KcrT"] = FTb[:, 4]; m["VcrT"] = FTb[:, 5]
    KwL = z(128, NI, 1024); VwL = z(NI, 1024, 129); DkL = z(128, NI, 6, 2560); DvL = z(NI, 2560, 6, 129)
    for i in range(NI):
        st = 2048 * i + 512 * c
        lo = max(0, st - 512)
        KwL[:, i, 1024 - (st + 512 - lo):] = FTb[:, 7, lo:st + 512]
        VwL[i, 1024 - (st + 512 - lo):] = VTb[lo:st + 512, 1]
        lo = max(0, st - 2048)
        DkL[:, i, :, 2560 - (st + 512 - lo):] = FTb[:, 26:32, lo:st + 512]
        DvL[i, 2560 - (st + 512 - lo):] = VTb[lo:st + 512, 8:14]
    m["KwL"], m["VwL"], m["DkL"], m["DvL"] = KwL, VwL, DkL, DvL
    m.update(cw)
    NCMP = S // 16
    cend = np.minimum(16 * np.arange(NCMP) + 31, S - 1)
    m["cposT"] = np.ascontiguousarray(posb[cend].reshape(NCMP // 128, 128).T).astype(np.int32)
    m["inv128"] = _rep(_inv_freq(128))
    m["caus"], m["cmask"], m["fmask"] = a_core_masks(c)
    m["wm"], m["dm"], m["ident"], m["I4"] = a_const_masks()
    return {k: np.ascontiguousarray(v) for k, v in m.items()}


def build_p3(NT=4096, ST=4):
    NTL = NT // 128
    NSUP = NTL // ST
    TW = ST * 128
    P = Prog(n_dma_sems=12, dma_queues=("sp", "pool"))
    di = lambda n, sh, dt=BF16: P.dram(n, sh, dt, "ExternalInput")
    h_in = di("h", [NT, D], F32); O_in = di("O", [NT, D]); mem = di("mem", [256, D], F32)
    w_out = di("w_out", [128, 16, 2048]); w_xq = di("w_xq", [128, 16, 512]); w_xkv = di("w_xkv", [128, 16, 1024])
    w_xo = di("w_xo", [128, 4, 2048]); w_up = di("w_up", [128, 16, 8192]); w_down = di("w_down", [128, 64, 2048])
    gains = di("gains", [128, 6, D], F32)
    ident = di("ident", [128, 128])
    hout = P.dram("hout", [NT, D], F32, "ExternalOutput")

    t_c = T()
    gbuf = [P.sb("gbuf%d" % i, [128, D], F32) for i in range(2)]; t_gb = [T(), T()]
    gbc = [0]

    def gload(gi):
        k = gbc[0] % 2
        gbc[0] += 1
        P.dma("pool", gbuf[k][:], gains[:, gi, :], writes=[t_gb[k]])
        return k
    s_id = P.sb("s_id", [128, 128], BF16)
    P.dma("sp", s_id[:], ident[:], writes=[t_c])
    epsb = P.sb("epsb", [128, 1], F32)
    P.op("dve", lambda e: e.memset(epsb[:], EPS), writes=[t_c])
    G_POST, G_MPRE, G_MKV, G_MPOST, G_FPRE, G_FPOST = range(6)

    hres = [P.sb("hres%d" % j, [128, D], F32) for j in range(ST)]; t_hres = [T() for _ in range(ST)]
    ybuf = [P.sb("ybuf%d" % j, [128, D], F32) for j in range(ST)]; t_y = [T() for _ in range(ST)]
    xT = P.sb("xT", [128, 16, TW], BF16); t_xT = T()
    hidT = P.sb("hidT", [128, 64, TW], BF16); t_hid = T()
    wg = [P.sb("wg%d" % i, [128, 16, 512], BF16) for i in range(2)]; t_wg = [T(), T()]
    hn = P.sb("hn", [128, D], BF16); t_hn = T()
    ss = P.sb("ss", [128, 2], F32); t_ss = T()
    rtmp = P.sb("rtmp", [128, TW], F32); t_rt = T()
    kxT = P.sb("kxT", [128, 4, 256], BF16); vx = P.sb("vx", [128, 2, 4, 129], BF16); t_kvx = T()
    qxT = P.sb("qxT", [128, 4, 128], BF16); t_qx = T()
    ptx = P.sb("ptx", [128, 1024], BF16); t_ptx = T()
    ox = P.sb("ox", [128, 512], BF16); t_ox = T()
    dn = P.sb("dn", [128, 4], F32); t_dn = T()
    ps_tr = [P.ps("ps_tr%d" % i, [128, 4, 128], BF16) for i in range(2)]; t_ps_tr = [PT(), PT()]
    NMM = max(ST, 2)
    ps_mm = [P.ps("ps_mm%d" % i, [128, 512]) for i in range(NMM)]; t_ps_mm = [PT() for _ in range(NMM)]
    ps_s1 = P.ps("ps_sx", [128, 512]); ps_o1 = P.ps("ps_ox", [128, 512])
    ps_s = [ps_s1, ps_s1]; t_ps_s = [PT()]; t_ps_s.append(t_ps_s[0])
    ps_o = [ps_o1, ps_o1]; t_ps_o = [PT()]; t_ps_o.append(t_ps_o[0])
    trc, evc, wgc, mmc = [0], [0], [0], [0]

    def transposes(srcs, dst, rd, wr):
        k = trc[0] % 2
        trc[0] += 1
        for i_, sa in enumerate(srcs):
            P.op("pe", lambda e, i_=i_, sa=sa: e.transpose(ps_tr[k][:, i_, :], sa, s_id[:]), reads=rd + [t_c], writes=[t_ps_tr[k]])
        n = len(srcs)
        if evc[0] % 2 == 0:
            P.op("act", lambda e: e.copy(dst, ps_tr[k][:, 0:n, :]), reads=[t_ps_tr[k]], writes=wr)
        else:
            P.op("dve", lambda e: e.tensor_copy(dst, ps_tr[k][:, 0:n, :]), reads=[t_ps_tr[k]], writes=wr)
        evc[0] += 1

    def loadw(src_ap, kc_n, ncols):
        b = wgc[0] % 2
        wgc[0] += 1
        P.dma("sp" if b == 0 else "pool", wg[b][:, 0:kc_n, 0:ncols], src_ap, writes=[t_wg[b]])
        return b

    def norm_T(src, t_src, gk, j, ncol_tiles=16):
        P.op("act", lambda e: e.activation(hn[:], src, AF.Square, accum_out=ss[:, 0:1]), reads=[t_src], writes=[t_hn, t_ss])
        _rstd(P, ss[:, 0:1], ss[:, 1:2], D, epsb[:], [t_ss, t_c], [t_ss])
        P.op("dve", lambda e: e.scalar_tensor_tensor(hn[:], src, ss[:, 1:2], gbuf[gk][:], ALU.mult, ALU.mult),
             reads=[t_src, t_ss, t_c, t_gb[gk]], writes=[t_hn])
        for g4 in range(4):
            transposes([hn[:, (4 * g4 + i_) * 128:(4 * g4 + i_ + 1) * 128] for i_ in range(4)],
                       xT[:, 4 * g4:4 * g4 + 4, j * 128:(j + 1) * 128], [t_hn], [t_xT])

    def norm_res(j, gk):
        P.op("act", lambda e: e.activation(hn[:], ybuf[j][:], AF.Square, accum_out=ss[:, 0:1]), reads=[t_y[j]], writes=[t_hn, t_ss])
        _rstd(P, ss[:, 0:1], ss[:, 1:2], D, epsb[:], [t_ss, t_c], [t_ss])
        P.op("dve", lambda e: e.scalar_tensor_tensor(ybuf[j][:], ybuf[j][:], ss[:, 1:2], gbuf[gk][:], ALU.mult, ALU.mult),
             reads=[t_y[j], t_ss, t_c, t_gb[gk]], writes=[t_y[j]])
        P.op("pool", lambda e: e.tensor_tensor(hres[j][:], hres[j][:], ybuf[j][:], ALU.add), reads=[t_y[j], t_hres[j]], writes=[t_hres[j]])

    def linear_tm(w_src_fn, KC, rd_x, x_fn):
        for n in range(4):
            b = loadw(w_src_fn(n), KC, 512)
            for j in range(ST):
                k = mmc[0] % NMM
                mmc[0] += 1
                for kc in range(KC):
                    P.op("pe", lambda e, kc=kc, j=j, k=k, b=b: e.matmul(ps_mm[k][:, :], x_fn(j, kc), wg[b][:, kc, :],
                                                                      start=(kc == 0), stop=(kc == KC - 1)),
                         reads=rd_x + [t_wg[b]], writes=[t_ps_mm[k]])
                P.op("act", lambda e, j=j, k=k, n=n: e.copy(ybuf[j][:, n * 512:(n + 1) * 512], ps_mm[k][:, :]),
                     reads=[t_ps_mm[k]], writes=[t_y[j]])

    gk0 = gload(G_MKV)
    for kt in range(2):
        P.dma("sp", hres[0][:], mem[kt * 128:(kt + 1) * 128, :], writes=[t_hres[0]])
        norm_T(hres[0][:], t_hres[0], gk0, kt)
    for hh in range(4):
        if hh == 0:
            b = loadw(w_xkv[:, :, 0:512], 16, 512)
        for kc in range(16):
            P.op("pe", lambda e, kc=kc, hh=hh, b=b: e.matmul(ps_mm[0][:, 0:256], wg[b][:, kc, hh * 128:(hh + 1) * 128], xT[:, kc, 0:256],
                                                             start=(kc == 0), stop=(kc == 15)), reads=[t_xT, t_wg[b]], writes=[t_ps_mm[0]])
        P.op("act", lambda e, hh=hh: e.copy(kxT[:, hh, :], ps_mm[0][:, 0:256]), reads=[t_ps_mm[0]], writes=[t_kvx])
    P.op("pool", lambda e: e.memset(vx[:], 1.0), writes=[t_kvx])
    b = loadw(w_xkv[:, :, 512:1024], 16, 512)
    for kt in range(2):
        for kc in range(16):
            P.op("pe", lambda e, kc=kc, kt=kt, b=b: e.matmul(ps_mm[1][:, :], xT[:, kc, kt * 128:(kt + 1) * 128], wg[b][:, kc, :],
                                                             start=(kc == 0), stop=(kc == 15)), reads=[t_xT, t_wg[b]], writes=[t_ps_mm[1]])
        P.op("act", lambda e, kt=kt: e.copy(vx[:, kt, :, 0:128], ps_mm[1][:, :].rearrange("p (h d) -> p h d", d=128)),
             reads=[t_ps_mm[1]], writes=[t_kvx])

    for st in range(NSUP):
        for j in range(ST):
            tl = st * ST + j
            P.dma("sp", hres[j][:], h_in[tl * 128:(tl + 1) * 128, :], writes=[t_hres[j]])
            P.dma("pool", hn[:], O_in[tl * 128:(tl + 1) * 128, :], writes=[t_hn])
            for g4 in range(4):
                transposes([hn[:, (4 * g4 + i_) * 128:(4 * g4 + i_ + 1) * 128] for i_ in range(4)],
                           xT[:, 4 * g4:4 * g4 + 4, j * 128:(j + 1) * 128], [t_hn], [t_xT])
        linear_tm(lambda n: w_out[:, :, n * 512:(n + 1) * 512], 16, [t_xT], lambda j, kc: xT[:, kc, j * 128:(j + 1) * 128])
        ga, gb_ = gload(G_POST), gload(G_MPRE)
        for j in range(ST):
            norm_res(j, ga)
            norm_T(hres[j][:], t_hres[j], gb_, j)
        bq = loadw(w_xq[:, :, :], 16, 512)
        for j in range(ST):
            for hh in range(4):
                for kc in range(16):
                    P.op("pe", lambda e, kc=kc, hh=hh, j=j: e.matmul(ps_mm[0][:, hh * 128:(hh + 1) * 128], wg[bq][:, kc, hh * 128:(hh + 1) * 128],
                                                                    xT[:, kc, j * 128:(j + 1) * 128], start=(kc == 0), stop=(kc == 15),
                                                                    skip_group_check=True),
                         reads=[t_xT, t_wg[bq]], writes=[t_ps_mm[0]])
            P.op("act", lambda e: e.copy(qxT[:], ps_mm[0][:, :].rearrange("p (h q) -> p h q", h=4)), reads=[t_ps_mm[0]], writes=[t_qx])
            for half in range(2):
                for hi in range(2):
                    hh = 2 * half + hi
                    for kt in range(2):
                        cb_ = (hi * 2 + kt) * 128
                        P.op("pe", lambda e, hh=hh, kt=kt, cb_=cb_, half=half: e.matmul(ps_s[half][:, cb_:cb_ + 128], kxT[:, hh, kt * 128:(kt + 1) * 128],
                                                                                   qxT[:, hh, :], start=True, stop=True, skip_group_check=True),
                             reads=[t_kvx, t_qx], writes=[t_ps_s[half]])
                P.op("act", lambda e, half=half: e.activation(ptx[:, half * 512:(half + 1) * 512], ps_s[half][:, :], AF.Exp, scale=SC128),
                     reads=[t_ps_s[half]], writes=[t_ptx])
                for hi in range(2):
                    hh = 2 * half + hi
                    for kt in range(2):
                        cb_ = half * 512 + (hi * 2 + kt) * 128
                        P.op("pe", lambda e, hh=hh, kt=kt, cb_=cb_, hi=hi, half=half: e.matmul(
                            ps_o[half][:, hi * 129:(hi + 1) * 129], ptx[:, cb_:cb_ + 128], vx[:, kt, hh, :],
                            start=(kt == 0), stop=(kt == 1), skip_group_check=True), reads=[t_ptx, t_kvx], writes=[t_ps_o[half]])
                for hi in range(2):
                    hh = 2 * half + hi
                    P.op("dve", lambda e, hh=hh, hi=hi, half=half: e.reciprocal(dn[:, hh:hh + 1], ps_o[half][:, hi * 129 + 128:hi * 129 + 129]),
                         reads=[t_ps_o[half]], writes=[t_dn])
                    P.op("act", lambda e, hh=hh, hi=hi, half=half: e.activation(ox[:, hh * 128:(hh + 1) * 128], ps_o[half][:, hi * 129:hi * 129 + 128],
                                                                               AF.Copy, scale=dn[:, hh:hh + 1]),
                         reads=[t_ps_o[half], t_dn], writes=[t_ox])
            transposes([ox[:, i_ * 128:(i_ + 1) * 128] for i_ in range(4)], hidT[:, 0:4, j * 128:(j + 1) * 128], [t_ox], [t_hid])
        linear_tm(lambda n: w_xo[:, :, n * 512:(n + 1) * 512], 4, [t_hid], lambda j, kc: hidT[:, kc, j * 128:(j + 1) * 128])
        ga, gb_ = gload(G_MPOST), gload(G_FPRE)
        for j in range(ST):
            norm_res(j, ga)
            norm_T(hres[j][:], t_hres[j], gb_, j)
        for m in range(16):
            b = loadw(w_up[:, :, m * 512:(m + 1) * 512], 16, 512)
            for hc in range(4):
                k = mmc[0] % NMM
                mmc[0] += 1
                for kc in range(16):
                    P.op("pe", lambda e, kc=kc, hc=hc, k=k, b=b: e.matmul(ps_mm[k][:, 0:TW], wg[b][:, kc, hc * 128:(hc + 1) * 128], xT[:, kc, :],
                                                                        start=(kc == 0), stop=(kc == 15)),
                         reads=[t_xT, t_wg[b]], writes=[t_ps_mm[k]])
                P.op("act", lambda e, k=k: e.activation(rtmp[:], ps_mm[k][:, 0:TW], AF.Relu), reads=[t_ps_mm[k]], writes=[t_rt])
                P.op("dve", lambda e, m=m, hc=hc: e.tensor_tensor(hidT[:, m * 4 + hc, :], rtmp[:], rtmp[:], ALU.mult), reads=[t_rt], writes=[t_hid])
        for n in range(4):
            for kg in range(4):
                b = loadw(w_down[:, kg * 16:(kg + 1) * 16, n * 512:(n + 1) * 512], 16, 512)
                for j in range(ST):
                    for kc in range(16):
                        P.op("pe", lambda e, kc=kc, kg=kg, j=j, b=b: e.matmul(ps_mm[j][:, :], hidT[:, kg * 16 + kc, j * 128:(j + 1) * 128], wg[b][:, kc, :],
                                                                            start=(kg == 0 and kc == 0), stop=(kg == 3 and kc == 15),
                                                                            skip_group_check=True),
                             reads=[t_hid, t_wg[b]], writes=[t_ps_mm[j]])
            for j in range(ST):
                P.op("act", lambda e, j=j, n=n: e.copy(ybuf[j][:, n * 512:(n + 1) * 512], ps_mm[j][:, :]), reads=[t_ps_mm[j]], writes=[t_y[j]])
        ga = gload(G_FPOST)
        for j in range(ST):
            tl = st * ST + j
            norm_res(j, ga)
            P.dma("pool", hout[tl * 128:(tl + 1) * 128, :], hres[j][:], reads=[t_hres[j]], is_output=True)
    return P.finish()


def _klay(w):
    k, n = w.shape
    return np.ascontiguousarray(w.reshape(k // 128, 128, n).transpose(1, 0, 2))


_PROGS = {}


def _prog(name, fn):
    if name not in _PROGS:
        _PROGS[name] = fn()
    return _PROGS[name]


def kernel(x, mem, positions, g_mix_pre, w_in, cmp_pos_emb, w_cmp_k1, w_cmp_k2, w_cmp_v1, w_cmp_v2,
           g_q_lora, g_kv_lora, w_uq, w_ukv, w_out, g_mix_post, g_mem_pre, g_mem_kv, w_xq, w_xkv, w_xo,
           g_mem_post, g_mlp_pre, w_up, w_down, g_mlp_post):
    f32 = lambda a: np.asarray(a, dtype=np.float32)
    x = f32(x); mem = f32(mem); positions = np.asarray(positions).astype(np.int32)
    DEPTH = int(np.asarray(w_in).shape[0])
    names, arrs = [], []
    for l in range(DEPTH):
        lay = {
            "w_in": _klay(f32(w_in[l])[:, W_IN_PERM]), "w_uq": _klay(f32(w_uq[l])), "w_ukv": _klay(f32(w_ukv[l])),
            "w_ck1": np.ascontiguousarray(f32(w_cmp_k1[l]).reshape(32, 128, 128).transpose(1, 0, 2)), "w_ck2": f32(w_cmp_k2[l]),
            "w_cv1": np.ascontiguousarray(f32(w_cmp_v1[l]).reshape(32, 128, 128).transpose(1, 0, 2)), "w_cv2": f32(w_cmp_v2[l]),
            "pembT": np.ascontiguousarray(f32(cmp_pos_emb[l]).T),
            "w_out": _klay(f32(w_out[l])), "w_xq": _klay(f32(w_xq[l])), "w_xkv": _klay(f32(w_xkv[l])), "w_xo": _klay(f32(w_xo[l])),
            "w_up": _klay(f32(w_up[l])), "w_down": _klay(f32(w_down[l])),
        }
        for k_, v_ in lay.items():
            names.append((l, k_, v_.shape))
            arrs.append(v_.reshape(-1))
    flat = np.concatenate(arrs)
    del arrs
    flat_bf = cast_on_device(flat)
    del flat
    W = [dict() for _ in range(DEPTH)]
    off = 0
    for l, k_, shp in names:
        n = int(np.prod(shp))
        W[l][k_] = flat_bf[off:off + n].reshape(shp)
        off += n

    ident = np.eye(128, dtype=np.float32).astype(NPBF)
    inv128, inv64 = _rep(_inv_freq(128)), _rep(_inv_freq(64))
    NTC = S // 4
    hcur = x.copy()
    nc_p1 = _prog("p1", lambda: build_p1(NT=NTC))
    nc_a = _prog("a", lambda: build_A(S=S))
    nc_p3 = _prog("p3", lambda: build_p3(NT=NTC))
    cores = list(range(NCORES))
    for l in range(DEPTH):
        ims = []
        for c8 in cores:
            b, c = c8 // 4, c8 % 4
            sl = slice(c * NTC, (c + 1) * NTC)
            ims.append({"h": np.ascontiguousarray(hcur[b, sl]),
                        "posT": np.ascontiguousarray(positions[b, sl].reshape(NTC // 128, 128).T),
                        "inv128": inv128, "inv64": inv64, "g_pre": _rep(f32(g_mix_pre[l])), "w_in": W[l]["w_in"],
                        "g_q": _rep(f32(g_q_lora[l])), "g_kv": _rep(f32(g_kv_lora[l])), "w_uq": W[l]["w_uq"], "w_ukv": W[l]["w_ukv"],
                        "ident": ident})
        r1 = run_bass_kernel_spmd(nc_p1, ims, core_ids=cores).results
        del ims
        ims = []
        cw = {k_: W[l][k_] for k_ in ("w_ck1", "w_ck2", "w_cv1", "w_cv2", "pembT")}
        for b in range(B):
            FTb = np.concatenate([np.asarray(r1[4 * b + c]["FT"]) for c in range(4)], axis=2)
            VTb = np.concatenate([np.asarray(r1[4 * b + c]["VT"]) for c in range(4)], axis=0)
            GTb = np.concatenate([np.asarray(r1[4 * b + c]["GT"]) for c in range(4)], axis=0)
            for c in range(4):
                ims.append(a_inputs(FTb, VTb, GTb, positions[b], c, cw, S))
        del r1
        ra = run_bass_kernel_spmd(nc_a, ims, core_ids=cores).results
        del ims
        Ob = np.zeros((B, S, D), NPBF)
        for c8 in cores:
            b, c = c8 // 4, c8 % 4
            idx = np.concatenate([np.arange(2048 * i + 512 * c, 2048 * i + 512 * c + 512) for i in range(S // 2048)])
            Ob[b, idx] = np.asarray(ra[c8]["O"])
        del ra
        gl = [g_mix_post, g_mem_pre, g_mem_kv, g_mem_post, g_mlp_pre, g_mlp_post]
        gains = np.ascontiguousarray(np.stack([_rep(f32(g_[l])) for g_ in gl], 1))
        ims = []
        for c8 in cores:
            b, c = c8 // 4, c8 % 4
            sl = slice(c * NTC, (c + 1) * NTC)
            ims.append({"h": np.ascontiguousarray(hcur[b, sl]), "O": np.ascontiguousarray(Ob[b, sl]), "mem": np.ascontiguousarray(mem[b]),
                        "w_out": W[l]["w_out"], "w_xq": W[l]["w_xq"], "w_xkv": W[l]["w_xkv"], "w_xo": W[l]["w_xo"],
                        "w_up": W[l]["w_up"], "w_down": W[l]["w_down"], "gains": gains, "ident": ident})
        r3 = run_bass_kernel_spmd(nc_p3, ims, core_ids=cores).results
        del ims
        for c8 in cores:
            b, c = c8 // 4, c8 % 4
            hcur[b, c * NTC:(c + 1) * NTC] = np.asarray(r3[c8]["hout"])
        del r3
    return hcur
```
